# Optimizing a Trainium2 kernel written in Bass

```python
import math
import jax
import jax.numpy as jnp
from jax import lax
import numpy as np

D_MODEL = 1024
BATCH = 8
SEQ = 4096
DEPTH = 2

CHUNK = 64
Q_BLOCK = 128
EPS = 1e-6
NEG_BIG = -1e30
EXP_CLIP = 60.0

D_MIX = D_MODEL
N_MIXERS = 4
GROUP_W = D_MIX // N_MIXERS

S5_CH = 16
S5_GROUPS = GROUP_W // S5_CH
S5_STATE = 64
S5_DT_MIN = 1e-3
S5_DT_MAX = 1e-1

HG_HEADS = 4
HG_DH = GROUP_W // HG_HEADS

FOX_HEADS = 4
FOX_DH = GROUP_W // FOX_HEADS

ML_HEADS = 4
ML_DH = GROUP_W // ML_HEADS
ML_CONV = 4

D_FF = int(math.ceil(8 * D_MODEL / 3 / 256)) * 256

COL_WIDTHS = (GROUP_W,) * 8 + (FOX_HEADS,) + (GROUP_W,) * 4 + (ML_HEADS, ML_HEADS)
D_IN = sum(COL_WIDTHS)
SPLIT_IDX = tuple(int(c) for c in np.cumsum(COL_WIDTHS)[:-1])

kernel_name = 'hybrid_s5_hgrn2_fox_mlstm_block'

F32 = jnp.float32


def rms_norm(x, gain):
    xf = x.astype(F32)
    y = xf * lax.rsqrt(jnp.mean(xf * xf, axis=-1, keepdims=True) + EPS)
    return (y * gain.astype(F32)).astype(x.dtype)


def headwise_rms_norm(x, gain, head_dim):
    shp = x.shape
    xf = x.astype(F32).reshape(shp[:-1] + (shp[-1] // head_dim, head_dim))
    y = xf * lax.rsqrt(jnp.mean(xf * xf, axis=-1, keepdims=True) + EPS)
    return (y.reshape(shp) * gain.astype(F32)).astype(x.dtype)


def causal_depthwise_conv(x, w):
    k_w, ch = w.shape
    return lax.conv_general_dilated(
        x, w[:, None, :].astype(x.dtype), window_strides=(1,), padding=[(k_w - 1, 0)],
        dimension_numbers=('NWC', 'WIO', 'NWC'), feature_group_count=ch)


def to_chunks(t):
    b, s, h, d = t.shape
    return t.reshape(b, s // CHUNK, CHUNK, h, d).transpose(1, 0, 3, 2, 4)


def from_chunks(t):
    nc, b, h, l, d = t.shape
    return t.transpose(1, 0, 3, 2, 4).reshape(b, nc * l, h, d)


def _complex_affine_combine(e1, e2):
    a1r, a1i, b1r, b1i = e1
    a2r, a2i, b2r, b2i = e2
    return (a2r * a1r - a2i * a1i, a2r * a1i + a2i * a1r,
            a2r * b1r - a2i * b1i + b2r, a2r * b1i + a2i * b1r + b2i)


def s5_mixer(u, lam_re, lam_im, b_re, b_im, c_re, c_im, d_skip, log_dt, w_glu, gain):
    bsz, seq, _ = u.shape
    uf = u.astype(F32)
    ug = uf.reshape(bsz, seq, S5_GROUPS, S5_CH)
    lr = jnp.minimum(lam_re.astype(F32), -1e-4)
    li = lam_im.astype(F32)
    dt = jnp.exp(log_dt.astype(F32))[:, None]
    mag = jnp.exp(lr * dt)
    ab_re = mag * jnp.cos(li * dt)
    ab_im = mag * jnp.sin(li * dt)
    den = lr * lr + li * li
    cf_re = ((ab_re - 1.0) * lr + ab_im * li) / den
    cf_im = (ab_im * lr - (ab_re - 1.0) * li) / den
    bre = b_re.astype(F32)
    bim = b_im.astype(F32)
    bb_re = cf_re[..., None] * bre - cf_im[..., None] * bim
    bb_im = cf_re[..., None] * bim + cf_im[..., None] * bre
    bu_re = jnp.einsum('bsgp,gnp->bsgn', ug, bb_re)
    bu_im = jnp.einsum('bsgp,gnp->bsgn', ug, bb_im)
    a_re = jnp.broadcast_to(ab_re, bu_re.shape)
    a_im = jnp.broadcast_to(ab_im, bu_im.shape)
    _, _, x_re, x_im = lax.associative_scan(
        _complex_affine_combine, (a_re, a_im, bu_re, bu_im), axis=1)
    y = (jnp.einsum('bsgn,gpn->bsgp', x_re, c_re.astype(F32))
         - jnp.einsum('bsgn,gpn->bsgp', x_im, c_im.astype(F32))).reshape(bsz, seq, GROUP_W)
    y = y + d_skip.astype(F32) * uf
    g = jax.nn.gelu(y)
    y = g * jax.nn.sigmoid(g @ w_glu.astype(F32))
    return rms_norm(y, gain).astype(u.dtype)


def _hgrn2_chunk(state, inp):
    q, k, v, lf = inp
    l = q.shape[2]
    b = jnp.cumsum(lf, axis=2)
    o_inter = jnp.einsum('bhtk,bhkv->bhtv', q * jnp.exp(b), state)
    causal = jnp.tril(jnp.ones((l, l), dtype=bool))
    diff = b[:, :, :, None, :] - b[:, :, None, :, :]
    decay = jnp.exp(jnp.where(causal[:, :, None], diff, NEG_BIG))
    scores = jnp.einsum('bhtk,bhsk,bhtsk->bhts', q, k, decay)
    o_intra = jnp.einsum('bhts,bhsv->bhtv', scores, v)
    b_last = b[:, :, -1:, :]
    new_state = (jnp.exp(b_last[:, :, 0, :])[..., None] * state
                 + jnp.einsum('bhsk,bhsv->bhkv', k * jnp.exp(b_last - b), v))
    return new_state, o_inter + o_intra


def hgrn2_mixer(q, fz, i, g, lb, gain):
    bsz, seq, _ = q.shape
    z = fz.astype(F32)
    logf = jax.nn.log_sigmoid(z) + jnp.log1p(lb * jnp.exp(jnp.minimum(-z, EXP_CLIP)))
    k = (1.0 - lb) * jax.nn.sigmoid(-z)

    def heads(t):
        return to_chunks(t.astype(F32).reshape(bsz, seq, HG_HEADS, HG_DH))

    s0 = jnp.zeros((bsz, HG_HEADS, HG_DH, HG_DH), F32)
    _, o = lax.scan(_hgrn2_chunk, s0, (heads(q), heads(k), heads(i), heads(logf)))
    o = from_chunks(o).reshape(bsz, seq, GROUP_W)
    o = headwise_rms_norm(o, gain, HG_DH) * jax.nn.silu(g.astype(F32))
    return o.astype(q.dtype)


def fox_mixer(q, k, v, fz, gain):
    bsz, seq, _ = q.shape

    def heads(t):
        return t.reshape(bsz, seq, FOX_HEADS, FOX_DH).transpose(0, 2, 1, 3)

    qh, kh, vh = heads(q), heads(k), heads(v)
    cum_logf = jnp.cumsum(jax.nn.log_sigmoid(fz.astype(F32)), axis=1).transpose(0, 2, 1)
    n_blk = seq // Q_BLOCK
    q_blocks = qh.reshape(bsz, FOX_HEADS, n_blk, Q_BLOCK, FOX_DH).transpose(2, 0, 1, 3, 4)
    f_blocks = cum_logf.reshape(bsz, FOX_HEADS, n_blk, Q_BLOCK).transpose(2, 0, 1, 3)
    k_pos = jnp.arange(seq)
    scale = FOX_DH ** -0.5

    def attend(args):
        blk, qb, fq = args
        q_pos = blk * Q_BLOCK + jnp.arange(Q_BLOCK)
        s = (jnp.einsum('bhqd,bhkd->bhqk', qb, kh).astype(F32) * scale
             + fq[..., None] - cum_logf[:, :, None, :])
        s = jnp.where(k_pos[None, :] <= q_pos[:, None], s, NEG_BIG)
        p = jax.nn.softmax(s, axis=-1)
        return jnp.einsum('bhqk,bhkd->bhqd', p.astype(vh.dtype), vh)

    out = lax.map(attend, (jnp.arange(n_blk), q_blocks, f_blocks))
    out = out.transpose(1, 0, 3, 2, 4).reshape(bsz, seq, GROUP_W)
    return headwise_rms_norm(out, gain, FOX_DH)


def _mlstm_chunk(carry, inp):
    c_mat, n_vec, m_prev = carry
    q, k, v, ig, lf = inp
    l = q.shape[2]
    b = jnp.cumsum(lf, axis=-1)
    causal = jnp.tril(jnp.ones((l, l), dtype=bool))
    d_log = jnp.where(causal, b[..., :, None] - b[..., None, :] + ig[..., None, :], NEG_BIG)
    inter_log = b + m_prev[..., None]
    m_t = jnp.maximum(inter_log, jnp.max(d_log, axis=-1))
    w_inter = jnp.exp(inter_log - m_t)
    qk = jnp.einsum('bhtd,bhsd->bhts', q, k) * jnp.exp(d_log - m_t[..., None])
    num = (w_inter[..., None] * jnp.einsum('bhtk,bhkv->bhtv', q, c_mat)
           + jnp.einsum('bhts,bhsv->bhtv', qk, v))
    den = w_inter * jnp.einsum('bhtk,bhk->bht', q, n_vec) + jnp.sum(qk, axis=-1)
    h = num / jnp.maximum(jnp.abs(den), jnp.exp(-m_t))[..., None]
    b_last = b[..., -1]
    src_log = b_last[..., None] - b + ig
    m_new = jnp.maximum(b_last + m_prev, jnp.max(src_log, axis=-1))
    w_src = jnp.exp(src_log - m_new[..., None])
    decay = jnp.exp(b_last + m_prev - m_new)
    c_new = decay[..., None, None] * c_mat + jnp.einsum('bhs,bhsk,bhsv->bhkv', w_src, k, v)
    n_new = decay[..., None] * n_vec + jnp.einsum('bhs,bhsk->bhk', w_src, k)
    return (c_new, n_new, m_new), h


def mlstm_mixer(q, k, v, o, ig, fz, conv_w, gain):
    bsz, seq, _ = q.shape
    qk = jax.nn.silu(causal_depthwise_conv(jnp.concatenate([q, k], axis=-1), conv_w))
    q_c, k_c = qk[..., :GROUP_W], qk[..., GROUP_W:]

    def heads(t):
        return to_chunks(t.astype(F32).reshape(bsz, seq, ML_HEADS, ML_DH))

    def gates(t):
        return to_chunks(t.astype(F32)[..., None])[..., 0]

    lf = jax.nn.log_sigmoid(fz.astype(F32))
    init = (jnp.zeros((bsz, ML_HEADS, ML_DH, ML_DH), F32),
            jnp.zeros((bsz, ML_HEADS, ML_DH), F32),
            jnp.zeros((bsz, ML_HEADS), F32))
    _, h = lax.scan(_mlstm_chunk, init,
                    (heads(q_c), heads(k_c * ML_DH ** -0.5), heads(v), gates(ig), gates(lf)))
    h = from_chunks(h).reshape(bsz, seq, GROUP_W)
    h = headwise_rms_norm(h, gain, ML_DH) * jax.nn.sigmoid(o.astype(F32))
    return h.astype(q.dtype)


def setup_inputs(seed: int = 0) -> dict:
    key = jax.random.key(seed)
    ks = jax.random.split(key, 26)

    def nrm(k, shape, scale):
        return scale * jax.random.normal(k, shape, F32)

    x = nrm(ks[0], (BATCH, SEQ, D_MODEL), 1.0)
    w_in = nrm(ks[1], (DEPTH, D_MODEL, D_IN), D_MODEL ** -0.5)
    fox_fb = 2.0 + nrm(ks[2], (DEPTH, FOX_HEADS), 0.1)
    ml_ib = nrm(ks[3], (DEPTH, ML_HEADS), 0.1)
    ml_fb = jnp.linspace(3.0, 6.0, ML_HEADS, dtype=F32)[None, :] + nrm(ks[4], (DEPTH, ML_HEADS), 0.1)
    gate_bias = jnp.concatenate([fox_fb, ml_ib, ml_fb], axis=-1)
    n_idx = jnp.arange(S5_STATE, dtype=F32)
    s5_lambda_re = -0.5 + nrm(ks[5], (DEPTH, S5_GROUPS, S5_STATE), 0.01)
    s5_lambda_im = math.pi * n_idx + nrm(ks[6], (DEPTH, S5_GROUPS, S5_STATE), 0.01)
    s5_b_re = nrm(ks[7], (DEPTH, S5_GROUPS, S5_STATE, S5_CH), (2 * S5_CH) ** -0.5)
    s5_b_im = nrm(ks[8], (DEPTH, S5_GROUPS, S5_STATE, S5_CH), (2 * S5_CH) ** -0.5)
    s5_c_re = nrm(ks[9], (DEPTH, S5_GROUPS, S5_CH, S5_STATE), (2 * S5_STATE) ** -0.5)
    s5_c_im = nrm(ks[10], (DEPTH, S5_GROUPS, S5_CH, S5_STATE), (2 * S5_STATE) ** -0.5)
    s5_d = nrm(ks[11], (DEPTH, GROUP_W), 1.0)
    s5_log_dt = jax.random.uniform(ks[12], (DEPTH, S5_GROUPS), F32,
                                   math.log(S5_DT_MIN), math.log(S5_DT_MAX))
    s5_w_glu = nrm(ks[13], (DEPTH, GROUP_W, GROUP_W), GROUP_W ** -0.5)
    hgrn_lb_logits = nrm(ks[14], (DEPTH, GROUP_W), 0.5)
    mlstm_conv_w = nrm(ks[15], (DEPTH, ML_CONV, 2 * GROUP_W), ML_CONV ** -0.5)
    mix_gain = 1.0 + nrm(ks[16], (DEPTH, D_MIX), 0.02)
    w_out = nrm(ks[17], (DEPTH, D_MIX, D_MODEL), D_MIX ** -0.5)
    ln_mix_pre = 1.0 + nrm(ks[18], (DEPTH, D_MODEL), 0.02)
    ln_mix_post = 1.0 + nrm(ks[19], (DEPTH, D_MODEL), 0.02)
    ln_ffn_pre = 1.0 + nrm(ks[20], (DEPTH, D_MODEL), 0.02)
    ln_ffn_post = 1.0 + nrm(ks[21], (DEPTH, D_MODEL), 0.02)
    w_ffn_gate = nrm(ks[22], (DEPTH, D_MODEL, D_FF), D_MODEL ** -0.5)
    w_ffn_up = nrm(ks[23], (DEPTH, D_MODEL, D_FF), D_MODEL ** -0.5)
    w_ffn_down = nrm(ks[24], (DEPTH, D_FF, D_MODEL), D_FF ** -0.5)
    return {'x': x, 'w_in': w_in, 'gate_bias': gate_bias,
            's5_lambda_re': s5_lambda_re, 's5_lambda_im': s5_lambda_im,
            's5_b_re': s5_b_re, 's5_b_im': s5_b_im, 's5_c_re': s5_c_re, 's5_c_im': s5_c_im,
            's5_d': s5_d, 's5_log_dt': s5_log_dt, 's5_w_glu': s5_w_glu,
            'hgrn_lb_logits': hgrn_lb_logits, 'mlstm_conv_w': mlstm_conv_w,
            'mix_gain': mix_gain, 'w_out': w_out,
            'ln_mix_pre': ln_mix_pre, 'ln_mix_post': ln_mix_post,
            'ln_ffn_pre': ln_ffn_pre, 'ln_ffn_post': ln_ffn_post,
            'w_ffn_gate': w_ffn_gate, 'w_ffn_up': w_ffn_up, 'w_ffn_down': w_ffn_down}


def reference(x, w_in, gate_bias, s5_lambda_re, s5_lambda_im, s5_b_re, s5_b_im, s5_c_re, s5_c_im,
              s5_d, s5_log_dt, s5_w_glu, hgrn_lb_logits, mlstm_conv_w, mix_gain, w_out,
              ln_mix_pre, ln_mix_post, ln_ffn_pre, ln_ffn_post, w_ffn_gate, w_ffn_up, w_ffn_down):
    lb_prob = jax.nn.softmax(hgrn_lb_logits.astype(F32), axis=0)
    lb_all = jnp.maximum(jnp.cumsum(lb_prob, axis=0) - lb_prob[0:1], 0.0)

    h = x
    for l in range(DEPTH):
        a = rms_norm(h, ln_mix_pre[l])
        proj = a @ w_in[l]
        (s5_u, hg_q, hg_f, hg_i, hg_g, fox_q, fox_k, fox_v, fox_f,
         ml_q, ml_k, ml_v, ml_o, ml_i, ml_f) = jnp.split(proj, SPLIT_IDX, axis=-1)
        gb = gate_bias[l]
        gain = mix_gain[l]
        y_a = s5_mixer(s5_u, s5_lambda_re[l], s5_lambda_im[l], s5_b_re[l], s5_b_im[l],
                       s5_c_re[l], s5_c_im[l], s5_d[l], s5_log_dt[l], s5_w_glu[l],
                       gain[0:GROUP_W])
        y_b = hgrn2_mixer(hg_q, hg_f, hg_i, hg_g, lb_all[l], gain[GROUP_W:2 * GROUP_W])
        y_c = fox_mixer(fox_q, fox_k, fox_v, fox_f + gb[0:FOX_HEADS],
                        gain[2 * GROUP_W:3 * GROUP_W])
        y_d = mlstm_mixer(ml_q, ml_k, ml_v, ml_o,
                          ml_i + gb[FOX_HEADS:FOX_HEADS + ML_HEADS],
                          ml_f + gb[FOX_HEADS + ML_HEADS:FOX_HEADS + 2 * ML_HEADS],
                          mlstm_conv_w[l], gain[3 * GROUP_W:4 * GROUP_W])
        mix = jnp.concatenate([y_a, y_b, y_c, y_d], axis=-1) @ w_out[l]
        h = h + rms_norm(mix, ln_mix_post[l])

        a = rms_norm(h, ln_ffn_pre[l])
        ff = (jax.nn.silu(a @ w_ffn_gate[l]) * (a @ w_ffn_up[l])) @ w_ffn_down[l]
        h = h + rms_norm(ff, ln_ffn_post[l])
    return h
```

```python
import contextlib
import math
import numpy as np
import ml_dtypes
import concourse.bass as bass
import concourse.mybir as mybir
from concourse.bass_utils import run_bass_kernel_spmd

F32 = mybir.dt.float32
BF16 = mybir.dt.bfloat16
I32 = mybir.dt.int32
ALU = mybir.AluOpType
AF = mybir.ActivationFunctionType
AX = mybir.AxisListType

D = 1024
T = 512
NB = 4
DFF = 2816
NFC = 22
EPS = 1e-6
DIN = 3084
TWO_PI = 2.0 * math.pi
E60 = 1.1420073898156842e26


class Tl:
    def __init__(self, ap, name=""):
        self.ap0 = ap
        self.name = name
        self.lw = None
        self.rd = {}

    def __getitem__(self, k):
        return V(self, self.ap0[k])

    @property
    def v(self):
        return V(self, self.ap0)


class V:
    def __init__(self, tl, ap):
        self.tl = tl
        self.ap = ap

    def __getitem__(self, k):
        return V(self.tl, self.ap[k])

    def re(self, pat, **kw):
        return V(self.tl, self.ap.rearrange(pat, **kw))

    def bc(self, shape):
        return V(self.tl, self.ap.to_broadcast(list(shape)))

    @property
    def v(self):
        return self


def _tl(x):
    return x.tl if isinstance(x, V) else None


def _ap(x):
    return x.ap if isinstance(x, V) else x


class KB:
    def __init__(self, nc, es):
        self.nc = nc
        self.es = es
        self.eng = {'pe': nc.tensor, 'act': nc.scalar, 'dve': nc.vector, 'pool': nc.gpsimd, 'sp': nc.sync}
        self.sem = {}
        self.cnt = {}
        self.known = {}
        for e in self.eng:
            self.sem[e] = es.enter_context(nc.semaphore('s_' + e))
            self.cnt[e] = 0
            self.known[e] = {}
        import os
        self.limit = int(os.environ.get('KOPS', '100000000'))
        self.rings = {'sp': [], 'pool': []}
        self.rpos = {'sp': 0, 'pool': 0}
        for q, n in (('sp', 8), ('pool', 6)):
            for i in range(n):
                k = 'd_%s%d' % (q, i)
                self.sem[k] = es.enter_context(nc.semaphore('s_' + k))
                self.cnt[k] = 0
                self.rings[q].append(k)

    def sb(self, name, shape, dt):
        t = self.es.enter_context(self.nc.sbuf_tensor(name, list(shape), dt))
        return Tl(t[:], name)

    def _mult(self, e):
        return 16 if e.startswith('d_') else 1

    def _waits(self, eng, reads, writes):
        deps = {}
        for tl in reads:
            if tl is not None and tl.lw:
                e, q = tl.lw
                deps[e] = max(deps.get(e, 0), q)
        for tl in writes:
            if tl is None:
                continue
            if tl.lw:
                e, q = tl.lw
                deps[e] = max(deps.get(e, 0), q)
            for e, q in tl.rd.items():
                deps[e] = max(deps.get(e, 0), q)
        E = self.eng[eng]
        for e, q in deps.items():
            if e == 'pe' and eng == 'pe':
                continue
            if self.known[eng].get(e, 0) < q:
                E.wait_ge(self.sem[e], q * self._mult(e))
                self.known[eng][e] = q

    def count_ops(self, fn):
        self._counting = True
        self._ccount = 0
        try:
            fn()
        finally:
            self._counting = False
        return self._ccount

    def parallel(self, fns):
        import threading
        counts = [max(1, self.count_ops(f)) for f in fns]
        n = len(fns)
        st = {'turn': 0, 'alive': [True] * n, 'done': [0] * n, 'err': None}
        cv = threading.Condition()
        self._par = (st, cv, counts, threading.local())

        def pick_next():
            best, bf = None, None
            for i in range(n):
                if st['alive'][i]:
                    fr = st['done'][i] / counts[i]
                    if bf is None or fr < bf:
                        best, bf = i, fr
            st['turn'] = best

        def runner(i):
            self._par[3].idx = i
            with cv:
                while st['turn'] != i:
                    cv.wait()
            try:
                fns[i]()
            except BaseException as e:
                st['err'] = e
            finally:
                with cv:
                    st['alive'][i] = False
                    pick_next()
                    cv.notify_all()
        ths = [threading.Thread(target=runner, args=(i,)) for i in range(n)]
        for t in ths:
            t.start()
        for t in ths:
            t.join()
        self._par = None
        if st['err'] is not None:
            raise st['err']

    def _yield_point(self):
        par = getattr(self, '_par', None)
        if par is None:
            return
        st, cv, counts, tls = par
        i = getattr(tls, 'idx', None)
        if i is None:
            return
        st['done'][i] += 1
        if st['done'][i] % 6 == 0:
            with cv:
                best, bf = None, None
                for j in range(len(counts)):
                    if st['alive'][j]:
                        fr = st['done'][j] / counts[j]
                        if bf is None or fr < bf:
                            best, bf = j, fr
                if best != i:
                    st['turn'] = best
                    cv.notify_all()
                    while st['turn'] != i:
                        cv.wait()

    def op(self, eng, fn, reads, writes, inc=True):
        if getattr(self, '_counting', False):
            self._ccount += 1
            return
        self._yield_point()
        if getattr(self, '_par', None) is not None:
            inc = True
        self.n = getattr(self, 'n', 0) + 1
        if self.n > self.limit:
            return
        banks = []
        for tl in list(reads) + list(writes):
            bkk = getattr(tl, 'bank', None) if tl is not None else None
            if bkk is not None and bkk not in banks:
                banks.append(bkk)
        writes = list(writes) + banks
        self._waits(eng, reads, writes)
        ins = fn(self.eng[eng])
        if inc:
            self.cnt[eng] += 1
            ins.then_inc(self.sem[eng], 1)
            q = self.cnt[eng]
        else:
            q = self.cnt[eng] + 1
        for tl in writes:
            if tl is not None:
                tl.lw = (eng, q)
                tl.rd = {}
        for tl in reads:
            if tl is not None:
                tl.rd[eng] = max(tl.rd.get(eng, 0), q)

    def dma(self, q, out, in_, **kw):
        if getattr(self, '_counting', False):
            self._ccount += 1
            return
        self._yield_point()
        self.n = getattr(self, 'n', 0) + 1
        if self.n > self.limit:
            return
        ring = self.rings[q]
        k = ring[self.rpos[q] % len(ring)]
        self.rpos[q] += 1
        E = self.eng[q]
        if self.cnt[k] > 0 and self.known[q].get(k, 0) < self.cnt[k]:
            E.wait_ge(self.sem[k], 16 * self.cnt[k])
            self.known[q][k] = self.cnt[k]
        self._waits(q, [_tl(in_)], [_tl(out)])
        ins = E.dma_start(out=_ap(out), in_=_ap(in_), **kw)
        self.cnt[k] += 1
        ins.then_inc(self.sem[k], 16)
        qn = self.cnt[k]
        if _tl(out) is not None:
            out.tl.lw = (k, qn)
            out.tl.rd = {}
        if _tl(in_) is not None:
            in_.tl.rd[k] = qn

    def barrier(self):
        ce = ['pe', 'act', 'dve', 'pool']
        for e in ce:
            for f in ce:
                if e == f:
                    continue
                if self.known[e].get(f, 0) < self.cnt[f]:
                    self.eng[e].wait_ge(self.sem[f], self.cnt[f])
                    self.known[e][f] = self.cnt[f]
        for f in ce:
            if self.known['sp'].get(f, 0) < self.cnt[f]:
                self.eng['sp'].wait_ge(self.sem[f], self.cnt[f])
                self.known['sp'][f] = self.cnt[f]

    def finish(self):
        E = self.eng['sp']
        print("KB ops emitted:", getattr(self, 'n', 0), {e: self.cnt[e] for e in self.cnt})
        for f in ['pe', 'act', 'dve', 'pool']:
            if self.cnt[f] > 0:
                E.wait_ge(self.sem[f], self.cnt[f])
        for q in self.rings:
            for k in self.rings[q]:
                if self.cnt[k] > 0:
                    E.wait_ge(self.sem[k], 16 * self.cnt[k])

    def mm(self, out, lhsT, rhs, start=True, stop=True, inc=None):
        if inc is None:
            inc = stop
        self.op('pe', lambda e: e.matmul(out.ap, lhsT=lhsT.ap, rhs=rhs.ap, start=start, stop=stop),
                [lhsT.tl, rhs.tl], [out.tl], inc=inc)

    def tr(self, out, in_, ident):
        self.op('pe', lambda e: e.transpose(out.ap, in_.ap, ident.ap), [in_.tl, ident.tl], [out.tl])

    def act(self, out, in_, func, bias=None, scale=1.0, accum=None):
        reads = [in_.tl]
        kw = {}
        if bias is not None:
            kw['bias'] = _ap(bias)
            reads.append(_tl(bias))
        kw['scale'] = _ap(scale)
        reads.append(_tl(scale))
        writes = [out.tl]
        if accum is not None:
            kw['accum_out'] = accum.ap
            writes.append(accum.tl)
        self.op('act', lambda e: e.activation(out=out.ap, in_=in_.ap, func=func, **kw), reads, writes)

    def tt(self, eng, out, a, b, op):
        self.op(eng, lambda e: e.tensor_tensor(out=out.ap, in0=a.ap, in1=b.ap, op=op), [a.tl, b.tl], [out.tl])

    def ts(self, eng, out, a, s1, op0, s2, op1):
        self.op(eng, lambda e: e.tensor_scalar(out=out.ap, in0=a.ap, scalar1=_ap(s1), scalar2=_ap(s2), op0=op0, op1=op1),
                [a.tl, _tl(s1), _tl(s2)], [out.tl])

    def ts1(self, eng, out, a, s1, op):
        self.op(eng, lambda e: e.tensor_single_scalar(out=out.ap, in_=a.ap, scalar=_ap(s1), op=op),
                [a.tl, _tl(s1)], [out.tl])

    def stt(self, eng, out, a, sc, b, op0, op1):
        self.op(eng, lambda e: e.scalar_tensor_tensor(out=out.ap, in0=a.ap, scalar=_ap(sc), in1=b.ap, op0=op0, op1=op1),
                [a.tl, _tl(sc), b.tl], [out.tl])

    def cp(self, eng, out, a):
        if eng == 'act':
            self.op('act', lambda e: e.copy(out=out.ap, in_=a.ap), [a.tl], [out.tl])
        else:
            self.op(eng, lambda e: e.tensor_copy(out=out.ap, in_=a.ap), [a.tl], [out.tl])

    def recip(self, out, a):
        self.op('dve', lambda e: e.reciprocal(out=out.ap, in_=a.ap), [a.tl], [out.tl])

    def red(self, out, a, op=ALU.add):
        self.op('dve', lambda e: e.tensor_reduce(out=out.ap, in_=a.ap, axis=AX.X, op=op), [a.tl], [out.tl])

    def scan(self, out, d0, d1, init, op0, op1):
        self.op('dve', lambda e: e.tensor_tensor_scan(out=out.ap, data0=d0.ap, data1=d1.ap, initial=_ap(init), op0=op0, op1=op1),
                [d0.tl, d1.tl, _tl(init)], [out.tl])

    def memset(self, eng, out, val):
        self.op(eng, lambda e: e.memset(out.ap, val), [], [out.tl])


def host_consts():
    c = {}
    c['identf'] = np.eye(128, dtype=np.float32)
    c['identb'] = np.eye(128, dtype=np.float32).astype(ml_dtypes.bfloat16)
    s = np.arange(128)[:, None]
    t = np.arange(128)[None, :]
    bd = ((s // 64 == t // 64) & (s <= t)).astype(np.float32)
    c['maskbd'] = bd
    c['maskbd8'] = bd * 0.125
    c['maskneg'] = np.where(s <= t, 0.0, -1.0e5).astype(np.float32)
    rm = np.ones((128, 512), np.float32)
    rm[:, ::64] = 0.0
    c['resetmask'] = rm
    c['ramp'] = np.tile(np.arange(1, 65, dtype=np.float32)[None, :], (128, 1))
    r = np.arange(128)
    c['rowmask'] = np.stack([((r % 32) // 16 == 0), ((r % 32) // 16 == 1)], 1).astype(np.float32)
    sel = np.zeros((128, 128), np.float32)
    sel[127, :] = 1.0
    c['sel127'] = sel
    es = np.zeros((8, 2, 128), np.float32)
    for pr in range(2):
        for m in range(128):
            es[4 + 2 * pr + m // 64, pr, m] = 1.0
    c['esel'] = es
    c['onesb'] = np.ones((128, 128), np.float32).astype(ml_dtypes.bfloat16)
    return c


CONST_SPECS = [('identf', [128, 128], F32), ('identb', [128, 128], BF16), ('maskbd', [128, 128], F32),
               ('maskbd8', [128, 128], F32), ('maskneg', [128, 128], F32), ('resetmask', [128, 512], F32),
               ('ramp', [128, 64], F32), ('rowmask', [128, 2], F32), ('sel127', [128, 128], F32),
               ('esel', [8, 2, 128], F32), ('onesb', [128, 128], BF16)]

PARAM_SPECS = [('w_in', [2, 1024, DIN]), ('gate_bias', [2, 12]), ('s5_lambda_re', [2, 16, 64]),
               ('s5_lambda_im', [2, 16, 64]), ('s5_b_re', [2, 16, 64, 16]), ('s5_b_im', [2, 16, 64, 16]),
               ('s5_c_re', [2, 16, 16, 64]), ('s5_c_im', [2, 16, 16, 64]), ('s5_d', [2, 256]),
               ('s5_log_dt', [2, 16]), ('s5_w_glu', [2, 256, 256]), ('hgrn_lb_logits', [2, 256]),
               ('mlstm_conv_w', [2, 4, 512]), ('mix_gain', [2, 1024]), ('w_out', [2, 1024, 1024]),
               ('ln_mix_pre', [2, 1024]), ('ln_mix_post', [2, 1024]), ('ln_ffn_pre', [2, 1024]),
               ('ln_ffn_post', [2, 1024]), ('w_ffn_gate', [2, 1024, DFF]), ('w_ffn_up', [2, 1024, DFF]),
               ('w_ffn_down', [2, DFF, 1024])]


def DV(ap):
    return V(None, ap)


def build(NT, NL=2, dbg=False):
    nc = bass.Bass("TRN2", target_bir_lowering=False)
    SL = NT * T
    NBT = NT * NB
    dr = {}
    dr['x'] = nc.dram_tensor("x", [SL, D], F32, kind="ExternalInput").ap()
    for name, shp in PARAM_SPECS:
        dr[name] = nc.dram_tensor(name, shp, F32, kind="ExternalInput").ap()
    for name, shp, dt in CONST_SPECS:
        dr[name] = nc.dram_tensor("c_" + name, shp, dt, kind="ExternalInput").ap()
    out_d = nc.dram_tensor("out", [SL, D], F32, kind="ExternalOutput").ap()
    hsp = nc.dram_tensor("hspill", [SL, D], F32).ap()
    dbg_d = nc.dram_tensor("dbg", [128, 4096], F32, kind="ExternalOutput").ap() if dbg else None

    with contextlib.ExitStack() as es:
        es.enter_context(nc.allow_non_contiguous_dma(reason="small strided parameter loads"))
        kb = KB(nc, es)
        sb = kb.sb
        cst = {}
        for name, shp, dt in CONST_SPECS:
            cst[name] = sb("k_" + name, shp, dt)
            kb.dma('sp', cst[name].v, DV(dr[name]))
        identf, identb = cst['identf'], cst['identb']

        h = sb("h", [128, NB, D], F32)
        aT = sb("aT", [128, 8, T], BF16)
        NS = 4
        wslab = [sb("wslab%d" % i, [128, 8, 512], BF16) for i in range(NS)]
        gainb = sb("gainb", [128, D], F32)
        gainBCD = sb("gainBCD", [128, 768], F32)
        kTc = sb("kTc", [128, 2, SL], BF16)
        vaug = sb("vaug", [128, NBT, 4, 65], BF16)
        cftok = sb("cftok", [128, NBT, 8], F32)
        biasq = sb("biasq", [128, NBT, 4], F32)
        ctab = sb("ctab", [128, 8, 64], F32)
        stab = sb("stab", [128, 8, 64], F32)
        rhotab = sb("rhotab", [128, 8, 64], F32)
        BBTre = sb("BBTre", [128, 8, 128], BF16)
        BBTim = sb("BBTim", [128, 8, 128], BF16)
        Cre = sb("Cre", [128, 8, 128], BF16)
        Cnim = sb("Cnim", [128, 8, 128], BF16)
        rho = sb("rho", [128, 8], F32)
        d5 = sb("d5", [128, 2], F32)
        gainA = sb("gainA", [128, 2], F32)
        wglu = sb("wglu", [128, 2, 256], BF16)
        S32 = sb("S32", [128, 2, 64], F32)
        C32 = sb("C32", [128, 2, 65], F32)
        xrp = sb("xrp", [128, 8], F32)
        xip = sb("xip", [128, 8], F32)
        cumcar = sb("cumcar", [8, 1], F32)
        Gcar = sb("Gcar", [8, 1], F32)
        gpre_mix = sb("gpre_mix", [128, 8], F32)
        gpre_ffn = sb("gpre_ffn", [128, 8], F32)
        lb = sb("lb", [128, 2], F32)
        oml = sb("oml", [128, 2], F32)
        convw = sb("convw", [128, 4, 4], F32)
        gbias = sb("gbias", [8, 2], F32)
        ngA = sb("ngA", [8, 1], F32)
        wgA = sb("wgA", [128, 8, 8], BF16)
        wgB = sb("wgB", [128, 8, 8], BF16)
        gtok = sb("gtok", [128, NB, 16], F32)
        decb = sb("decb", [128, 2, 8], F32)
        vhat = sb("vhat", [128, NB, 4, 65], BF16)
        qkraw = sb("qkraw", [128, 4, 3 + T], F32)
        junk = sb("junk", [128, D], BF16)
        itile = V(junk, junk.ap0.bitcast(I32))
        ss = sb("ss", [128, 4], F32)
        rt = sb("rt", [128, 4], F32)
        rr = sb("rr", [128, 4], F32)
        sm = [sb("sm%d" % i, [128, 8], F32) for i in range(24)]
        sdec = sb("sdec", [128, 2, 8], F32)
        sinj = sb("sinj", [128, 2, 8], F32)
        ser = sb("ser", [128, 2, 8], F32)

        fa_t = es.enter_context(nc.sbuf_tensor("fa", [128, 10240], F32))
        ba_t = es.enter_context(nc.sbuf_tensor("ba", [128, 15360], BF16))
        F2 = [Tl(fa_t[:, i * 1024:(i + 1) * 1024]) for i in range(4)]
        F1 = [Tl(fa_t[:, 4096 + i * 512:4096 + (i + 1) * 512]) for i in range(12)]
        ffo = Tl(fa_t[:, 0:4096].rearrange("p (b d) -> p b d", d=D))
        silt = Tl(fa_t[:, 4096:4608])
        yT = Tl(ba_t[:, 0:4096].rearrange("p (k t) -> p k t", t=T))
        ytok = Tl(ba_t[:, 4096:7168].rearrange("p (b c) -> p b c", c=768))
        B2 = [Tl(ba_t[:, 7168 + i * 1024:7168 + (i + 1) * 1024]) for i in range(4)]
        xn = Tl(ba_t[:, 11264:15360].rearrange("p (b d) -> p b d", d=D))
        hid = Tl(ba_t[:, 0:11264].rearrange("p (f t) -> p f t", t=T))
        XA = [Tl(ba_t[:, 11264 + i * 1024:11264 + (i + 1) * 1024]) for i in range(4)]

        pb = [es.enter_context(nc.psum_tensor("pb%d" % i, [128, 512], F32)) for i in range(7)]
        pbfA = es.enter_context(nc.psum_tensor("pbfA", [128, 1024], BF16))
        bigs = [Tl(pb[i][:]) for i in range(3)]
        bigpos = [0]

        def big():
            t = bigs[bigpos[0] % 3]
            bigpos[0] += 1
            return t
        psT = [Tl(pb[3][:, 0:128]), Tl(pb[4][:, 0:128]), Tl(pb[3][:, 128:256]), Tl(pb[4][:, 128:256])]
        po = [Tl(pb[5][:, 0:260]), Tl(pb[6][:, 0:260])]
        pm = [Tl(pb[5][:, 260:390]), Tl(pb[5][:, 390:512]), Tl(pb[6][:, 260:390]), Tl(pb[6][:, 390:512])]
        ptr = [Tl(pbfA[:, 0:512]), Tl(pb[4][:].bitcast(BF16)[:, 0:512])]
        po_f = Tl(pb[0][:, 0:260])
        pc0_f = Tl(pb[0][:, 260:268])
        py_c = Tl(pb[2][:, 0:128])
        psG = [Tl(pb[3][:]), Tl(pb[4][:])]
        psG[0].bank = None
        psG[1].bank = None
        s5pr = Tl(pb[5][:])
        s5pi = Tl(pb[6][:])
        s5pi2 = Tl(pbfA[:].bitcast(F32))
        ptr3 = Tl(pb[3][:].bitcast(BF16)[:, 0:512])
        hpo = Tl(pb[5][:, 0:256])
        hpd = [Tl(pb[5][:, 256:384]), Tl(pb[5][:, 384:512])]
        bk = [Tl(None, "bank%d" % i) for i in range(8)]
        for i in range(3):
            bigs[i].bank = bk[i]
        psT[0].bank = bk[3]
        psT[2].bank = bk[3]
        psT[1].bank = bk[4]
        psT[3].bank = bk[4]
        ptr[1].bank = bk[4]
        for t_ in (po[0], pm[0], pm[1], s5pr):
            t_.bank = bk[5]
        for t_ in (po[1], pm[2], pm[3], s5pi):
            t_.bank = bk[6]
        ptr[0].bank = bk[7]
        s5pi2.bank = bk[7]
        ptr3.bank = bk[3]
        hpo.bank = bk[5]
        hpd[0].bank = bk[5]
        hpd[1].bank = bk[5]
        psG[0].bank = bk[3]
        psG[1].bank = bk[4]
        po_f.bank = bk[0]
        pc0_f.bank = bk[0]
        py_c.bank = bk[2]

        plan = []
        for l in range(NL):
            for ti in range(NT):
                W = dr['w_in'][l]
                for (c0, c1) in [(0, 512), (512, 1024), (1024, 1536), (1536, 2048), (2052, 2564), (2564, 3076)]:
                    plan.append((W[:, c0:c1], 8, c1 - c0))
                for dg in range(2):
                    plan.append((dr['w_out'][l][:, dg * 512:(dg + 1) * 512], 8, 512))
                for fg in range(6):
                    c0 = fg * 512
                    ncl = min(512, DFF - c0)
                    plan.append((dr['w_ffn_gate'][l][:, c0:c0 + ncl], 8, ncl))
                    plan.append((dr['w_ffn_up'][l][:, c0:c0 + ncl], 8, ncl))
                for dg in range(2):
                    for (f0, nf) in ((0, 8), (8, 8), (16, 6)):
                        plan.append((dr['w_ffn_down'][l][f0 * 128:(f0 + nf) * 128, dg * 512:(dg + 1) * 512], nf, 512))
        sstate = {'ptr': 0, 'issued': 0}

        def slab_issue(i):
            ap, nk, ncl = plan[i]
            sl = wslab[i % NS]
            kb.dma('pool', sl[:, 0:nk, 0:ncl], DV(ap.rearrange("(kc p) c -> p kc c", p=128)))

        def get_slab():
            idx = sstate['ptr']
            lim = min(len(plan), idx + 2)
            while sstate['issued'] < lim:
                slab_issue(sstate['issued'])
                sstate['issued'] += 1
            sstate['ptr'] += 1
            return wslab[idx % NS]

        def proj_fm(outv, slab, c0, ncl, t0=0, nt=T):
            for kc in range(8):
                kb.mm(outv, slab[:, kc, c0:c0 + ncl], aT[:, kc, t0:t0 + nt], start=(kc == 0), stop=(kc == 7))

        def proj_tm(outv, slab, c0, ncl, blk):
            for kc in range(8):
                kb.mm(outv, aT[:, kc, blk * 128:(blk + 1) * 128], slab[:, kc, c0:c0 + ncl], start=(kc == 0), stop=(kc == 7))

        def sincos(src, osin, ocos, N):
            a = F1[8][:, 0:N]
            b = F1[9][:, 0:N]
            ii = itile[:, 0:N]
            for (shift, outv) in ((0.0, osin), (math.pi / 2, ocos)):
                kb.ts('dve', a, src, shift, ALU.add, 1.0 / TWO_PI, ALU.mult)
                kb.cp('dve', ii, a)
                kb.cp('dve', b, ii)
                kb.stt('dve', a, b, -TWO_PI, src, ALU.mult, ALU.add)
                kb.ts('dve', a, a, shift, ALU.add, 3.1415925, ALU.min)
                kb.ts1('dve', a, a, -3.1415925, ALU.max)
                kb.act(outv, a, AF.Sin)

        def colsplit(ap_1d):
            return DV(ap_1d.rearrange("(kc p) -> p kc", p=128))

        def load_params(l):
            kb.dma('sp', gpre_mix.v, colsplit(dr['ln_mix_pre'][l]))
            kb.dma('sp', gpre_ffn.v, colsplit(dr['ln_ffn_pre'][l]))
            kb.dma('sp', d5.v, colsplit(dr['s5_d'][l]))
            kb.dma('sp', gainA.v, colsplit(dr['mix_gain'][l][0:256]))
            kb.dma('sp', gainBCD.v, DV(dr['mix_gain'][l][256:1024].partition_broadcast(128)))
            kb.dma('pool', wglu.v, DV(dr['s5_w_glu'][l].rearrange("(kc p) c -> p kc c", p=128)))
            for ctt in range(4):
                kb.dma('sp', convw[:, ctt, :], DV(dr['mlstm_conv_w'][l][:, ctt * 128:(ctt + 1) * 128].rearrange("j p -> p j")))
            gb = dr['gate_bias'][l]

            def col(a):
                return DV(a.rearrange("(p o) -> p o", o=1))
            kb.dma('sp', gbias[0:4, 0:1], col(gb[0:4]))
            kb.dma('sp', gbias[4:8, 0:1], col(gb[8:12]))
            kb.dma('sp', gbias[0:4, 1:2], col(gb[0:4]))
            kb.dma('sp', gbias[4:8, 1:2], col(gb[4:8]))
            kb.ts1('dve', ngA.v, gbias[:, 0:1], -1.0, ALU.mult)
            W = dr['w_in'][l]

            def gcols(c0):
                return DV(W[:, c0:c0 + 4].rearrange("(kc p) c -> p kc c", p=128))
            kb.dma('pool', wgA[:, :, 0:4], gcols(2048))
            kb.dma('pool', wgA[:, :, 4:8], gcols(3080))
            kb.dma('pool', wgB[:, :, 0:4], gcols(2048))
            kb.dma('pool', wgB[:, :, 4:8], gcols(3076))
            if l == 0:
                kb.memset('dve', lb.v, 0.0)
                kb.memset('dve', oml.v, 1.0)
            else:
                x0, x1 = sm[20], sm[21]
                kb.dma('sp', x0[:, 0:2], colsplit(dr['hgrn_lb_logits'][0]))
                kb.dma('sp', x1[:, 0:2], colsplit(dr['hgrn_lb_logits'][1]))
                kb.tt('dve', x0[:, 0:2], x0[:, 0:2], x1[:, 0:2], ALU.subtract)
                kb.act(x0[:, 0:2], x0[:, 0:2], AF.Exp)
                kb.ts1('dve', x0[:, 0:2], x0[:, 0:2], 1.0, ALU.add)
                kb.recip(lb.v, x0[:, 0:2])
                kb.ts('dve', oml.v, lb.v, -1.0, ALU.mult, 1.0, ALU.add)
            kb.memset('dve', S32.v, 0.0)
            kb.memset('dve', C32.v, 0.0)
            kb.memset('dve', xrp.v, 0.0)
            kb.memset('dve', xip.v, 0.0)
            kb.memset('dve', cumcar.v, 0.0)
            kb.memset('dve', Gcar.v, 0.0)
            kb.memset('dve', qkraw[:, :, 0:3], 0.0)
            if l == 0:
                kb.memset('dve', vaug[:, :, :, 64:65], 1.0)
            s5_prep(l)

        def s5_prep(l):
            ldt, lre, lim, dtt, lr, mag, th, sn, cs = sm[0:9]
            abre, abim, den, rden, am1, cfre, cfim, t0, t1 = sm[9:18]
            for half in range(2):
                rows = slice(half * 64, (half + 1) * 64)
                kb.dma('sp', ldt[rows, :], DV(dr['s5_log_dt'][l].rearrange("(j h) -> h j", h=2)[half].partition_broadcast(64)))
                kb.dma('sp', lre[rows, :], DV(dr['s5_lambda_re'][l].rearrange("(j h) n -> h n j", h=2)[half]))
                kb.dma('sp', lim[rows, :], DV(dr['s5_lambda_im'][l].rearrange("(j h) n -> h n j", h=2)[half]))
            bre = V(F1[0], F1[0].ap0[:, 0:128].rearrange("p (j q) -> p j q", q=16))
            bim = V(F1[1], F1[1].ap0[:, 0:128].rearrange("p (j q) -> p j q", q=16))
            bbre = V(F1[2], F1[2].ap0[:, 0:128].rearrange("p (j q) -> p j q", q=16))
            bbim = V(F1[3], F1[3].ap0[:, 0:128].rearrange("p (j q) -> p j q", q=16))
            tmpb = V(F1[4], F1[4].ap0[:, 0:128].rearrange("p (j q) -> p j q", q=16))
            for half in range(2):
                rows = slice(half * 64, (half + 1) * 64)
                kb.dma('sp', bre[rows, :, :], DV(dr['s5_b_re'][l].rearrange("(j h) n q -> h n j q", h=2)[half]))
                kb.dma('sp', bim[rows, :, :], DV(dr['s5_b_im'][l].rearrange("(j h) n q -> h n j q", h=2)[half]))
            kb.act(dtt.v, ldt.v, AF.Exp)
            kb.ts1('dve', lr.v, lre.v, -1e-4, ALU.min)
            kb.tt('dve', t0.v, lr.v, dtt.v, ALU.mult)
            kb.act(mag.v, t0.v, AF.Exp)
            kb.tt('dve', th.v, lim.v, dtt.v, ALU.mult)
            sincos(th.v, sn.v, cs.v, 8)
            kb.tt('dve', abre.v, mag.v, cs.v, ALU.mult)
            kb.tt('dve', abim.v, mag.v, sn.v, ALU.mult)
            kb.tt('dve', t0.v, lr.v, lr.v, ALU.mult)
            kb.tt('dve', t1.v, lim.v, lim.v, ALU.mult)
            kb.tt('dve', den.v, t0.v, t1.v, ALU.add)
            kb.recip(rden.v, den.v)
            kb.ts1('dve', am1.v, abre.v, -1.0, ALU.add)
            kb.tt('dve', t0.v, am1.v, lr.v, ALU.mult)
            kb.tt('dve', t1.v, abim.v, lim.v, ALU.mult)
            kb.tt('dve', t0.v, t0.v, t1.v, ALU.add)
            kb.tt('dve', cfre.v, t0.v, rden.v, ALU.mult)
            kb.tt('dve', t0.v, abim.v, lr.v, ALU.mult)
            kb.tt('dve', t1.v, am1.v, lim.v, ALU.mult)
            kb.tt('dve', t0.v, t0.v, t1.v, ALU.subtract)
            kb.tt('dve', cfim.v, t0.v, rden.v, ALU.mult)
            cfre_b = cfre.v.re("p (j o) -> p j o", o=1).bc([128, 8, 16])
            cfim_b = cfim.v.re("p (j o) -> p j o", o=1).bc([128, 8, 16])
            kb.tt('dve', bbre.v, cfre_b, bre.v, ALU.mult)
            kb.tt('dve', tmpb.v, cfim_b, bim.v, ALU.mult)
            kb.tt('dve', bbre.v, bbre.v, tmpb.v, ALU.subtract)
            kb.tt('dve', bbim.v, cfre_b, bim.v, ALU.mult)
            kb.tt('dve', tmpb.v, cfim_b, bre.v, ALU.mult)
            kb.tt('dve', bbim.v, bbim.v, tmpb.v, ALU.add)
            Xf = V(B2[0], B2[0].ap0.rearrange("p (j c) -> p j c", c=128))
            for (bb, BBT) in ((bbre, BBTre), (bbim, BBTim)):
                kb.memset('dve', Xf.v, 0.0)
                Xf4 = Xf.v.re("p (a b) c -> p a b c", b=4)
                bb4 = bb.v.re("p (a b) q -> p a b q", b=4)
                for j4 in range(4):
                    for half in range(2):
                        rows = slice(half * 64, (half + 1) * 64)
                        c0 = 32 * j4 + 16 * half
                        kb.cp('dve', Xf4[rows, :, j4, c0:c0 + 16], bb4[rows, :, j4, :])
                for g in range(2):
                    for k4 in range(4):
                        kb.tr(ptr[g][:, k4 * 128:(k4 + 1) * 128], Xf[:, g * 4 + k4, :], identb.v)
                    kb.cp('dve', BBT[:, g * 4:(g + 1) * 4, :], ptr[g].v.re("p (k t) -> p k t", t=128))
            ph = F1[10]
            th_b = th.v.re("p (j o) -> p j o", o=1).bc([128, 8, 64])
            ramp_b = cst['ramp'].v.re("p (o t) -> p o t", o=1).bc([128, 8, 64])
            kb.tt('dve', ph.v.re("p (j t) -> p j t", t=64), th_b, ramp_b, ALU.mult)
            sincos(ph.v, stab.v.re("p j t -> p (j t)"), ctab.v.re("p j t -> p (j t)"), 512)
            kb.cp('dve', rhotab.v, mag.v.re("p (j o) -> p j o", o=1).bc([128, 8, 64]))
            kb.memset('dve', rhotab[:, :, 0:1], 0.0)
            kb.cp('dve', rho.v, mag.v)
            cstt = V(F1[5], F1[5].ap0[:, 0:128].rearrange("p (t n) -> p t n", n=64))
            cst2 = V(B2[1], B2[1].ap0[:, 0:256].rearrange("p (t n) -> p t n", n=128))
            for (cname, Cm, sign) in (('s5_c_re', Cre, 1.0), ('s5_c_im', Cnim, -1.0)):
                kb.dma('sp', cstt.v, DV(dr[cname][l].rearrange("(t g) p n -> (g p) t n", t=2)))
                for half in range(2):
                    kb.ts('dve', cst2[:, :, half * 64:(half + 1) * 64], cstt.v, cst['rowmask'][:, half:half + 1], ALU.mult, sign, ALU.mult)
                kb.memset('dve', Cm.v, 0.0)
                for ti in range(2):
                    kb.tr(ptr[1][:, ti * 128:(ti + 1) * 128], cst2[:, ti, :], identb.v)
                for j in range(8):
                    c0 = 32 * (j % 4)
                    kb.cp('dve', Cm[:, j, c0:c0 + 32], ptr[1][:, (j // 4) * 128 + c0:(j // 4) * 128 + c0 + 32])

        PTt = [sb("PT%d" % i, [128, 128], BF16) for i in range(8)]
        Sbt = [sb("Sb%d" % i, [128, 64], BF16) for i in range(4)]
        Cbt = [sb("Cb%d" % i, [128, 65], BF16) for i in range(4)]

        def c3(v):
            return v.re("p (c t) -> p c t", t=64)

        def k3(v):
            return v.re("p (k t) -> p k t", t=T)

        def norm_to_aT(gpre):
            kb.memset('dve', ss.v, 0.0)
            for b in range(NB):
                kb.act(junk.v, h[:, b, :], AF.Square, accum=ss[:, b:b + 1])
            kb.act(rt.v, ss.v, AF.Sqrt, bias=EPS, scale=1.0 / D)
            kb.recip(rr.v, rt.v)
            for b in range(NB):
                kb.ts1('dve', xn[:, b, :], h[:, b, :], rr[:, b:b + 1], ALU.mult)
                for half in range(2):
                    pt = ptr[half]
                    for k4 in range(4):
                        kc = half * 4 + k4
                        kb.tr(pt[:, k4 * 128:(k4 + 1) * 128], xn[:, b, kc * 128:(kc + 1) * 128], identb.v)
                    kb.tt('dve', aT[:, half * 4:(half + 1) * 4, b * 128:(b + 1) * 128],
                          pt.v.re("p (k t) -> p k t", t=128),
                          gpre[:, half * 4:(half + 1) * 4].re("p (k o) -> p k o", o=1).bc([128, 4, 128]), ALU.mult)

        def gates(ti):
            pgA = big()
            pgB = big()
            for (pg, wg) in ((pgA, wgA), (pgB, wgB)):
                for kc in range(8):
                    kb.mm(pg[0:8, :], wg[:, kc, :], aT[:, kc, :], start=(kc == 0), stop=(kc == 7))
            eA, l1, cumA, gS, G, e2, clv, tmp = [F1[i][0:8, :] for i in range(8)]
            kb.act(eA, pgA[0:8, :], AF.Exp, bias=ngA.v, scale=-1.0)
            kb.act(l1, eA, AF.Ln, bias=1.0)
            ones8 = F1[9][0:8, :]
            kb.memset('dve', ones8, 1.0)
            kb.scan(cumA, ones8, l1, cumcar.v, ALU.mult, ALU.subtract)
            kb.cp('dve', cumcar.v, cumA[:, 511:512])
            for b in range(NB):
                kb.tr(pm[2][:, b * 8:(b + 1) * 8], cumA[:, b * 128:(b + 1) * 128], identf[0:8, 0:8])
            kb.cp('dve', cftok[:, ti * NB:(ti + 1) * NB, :], pm[2][:, 0:32].re("p (b g) -> p b g", g=8))
            kb.stt('dve', gS, pgB[0:8, :], gbias[:, 1:2], cumA, ALU.add, ALU.subtract)
            negb8 = F1[10][0:8, :]
            kb.memset('dve', negb8, -1.0e30)
            kb.scan(G, negb8, gS, Gcar.v, ALU.max, ALU.max)
            Gend_b = c3(G)[:, :, 63:64].bc([8, 8, 64])
            kb.tt('dve', c3(tmp), c3(gS), Gend_b, ALU.subtract)
            kb.act(e2, tmp, AF.Exp)
            kb.tt('dve', c3(tmp), c3(cumA), Gend_b, ALU.add)
            kb.act(clv, tmp, AF.Exp, scale=-1.0)
            Gpv = sm[22][0:8, 0:8]
            dd = sm[23][0:8, 0:8]
            dec = sm[19][0:8, 0:8]
            kb.cp('dve', Gpv[:, 0:1], Gcar.v)
            kb.cp('dve', Gpv[:, 1:8], c3(G)[:, 0:7, 63])
            kb.tt('dve', dd, Gpv, c3(G)[:, :, 63], ALU.subtract)
            kb.act(dec, dd, AF.Exp)
            kb.cp('dve', Gcar.v, c3(G)[:, 7, 63:64])
            for b in range(NB):
                kb.tr(pm[3][:, b * 16:b * 16 + 8], e2[:, b * 128:(b + 1) * 128], identf[0:8, 0:8])
                kb.tr(pm[3][:, b * 16 + 8:b * 16 + 16], clv[:, b * 128:(b + 1) * 128], identf[0:8, 0:8])
            kb.cp('dve', gtok.v, pm[3][:, 0:64].re("p (b g) -> p b g", g=16))
            for pr in range(2):
                kb.mm(pm[1][:, pr * 8:(pr + 1) * 8], cst['esel'][:, pr, :], dec)
            kb.cp('dve', decb.v, pm[1][:, 0:16].re("p (a c) -> p a c", c=8))

        def finish_tm(blk, num3, d2eps, gain2d, gate2d, c0, scr=None):
            if scr is None:
                scr = (F1[10], F1[11], sm[16], sm[17], ptr[1])
            sq = scr[0][:, 0:256].re("p (h v) -> p h v", v=64)
            s4 = scr[2][:, 0:4]
            r4 = scr[3][:, 0:4]
            y2d = scr[1][:, 0:256]
            ptrx = scr[4]
            y3d = y2d.re("p (h v) -> p h v", v=64)
            kb.tt('dve', sq, num3, num3, ALU.mult)
            kb.red(s4, sq)
            if d2eps is None:
                kb.act(r4, s4, AF.Sqrt, bias=EPS, scale=1.0 / 64)
            else:
                kb.stt('dve', s4, s4, 1.0 / 64, d2eps, ALU.mult, ALU.add)
                kb.act(r4, s4, AF.Sqrt)
            kb.recip(r4, r4)
            kb.tt('dve', y3d, num3, r4.re("p (h o) -> p h o", o=1).bc([128, 4, 64]), ALU.mult)
            if gate2d is None:
                kb.tt('dve', ytok[:, blk, c0:c0 + 256], y2d, gain2d, ALU.mult)
            else:
                kb.tt('dve', y2d, y2d, gain2d, ALU.mult)
                kb.tt('dve', ytok[:, blk, c0:c0 + 256], y2d, gate2d, ALU.mult)
            kc0 = 2 + c0 // 128
            for k in range(2):
                kb.tr(ptrx[:, k * 128:(k + 1) * 128], ytok[:, blk, c0 + k * 128:c0 + (k + 1) * 128], identb.v)
            kb.cp('act', yT[:, kc0:kc0 + 2, blk * 128:(blk + 1) * 128], ptrx[:, 0:256].re("p (k t) -> p k t", t=128))

        def s5_proj(slabA):
            uT = k3(F2[0].v)
            uTb = k3(XA[0].v)
            for kc in range(2):
                p = big()
                proj_fm(p.v, slabA, kc * 128, 128)
                kb.cp('act', uT[:, kc, :], p.v)
                kb.cp('dve', uTb[:, kc, :], p.v)

        def s5_main():
            uT = k3(F2[0].v)
            uTb = k3(XA[0].v)
            yv = k3(F2[1].v)
            prpi = [(s5pr, s5pi), (bigs[1], s5pi2)]
            xbs = [B2[1].v, XA[2].v]
            t1, t2, t3, t4, bmr, bmi, zr, zi = [F1[i].v for i in range(8)]
            ct = ctab.v.re("p j t -> p (j t)")
            st = stab.v.re("p j t -> p (j t)")
            rtb = rhotab.v.re("p j t -> p (j t)")

            def emit_bu(c):
                pr_, pi_ = prpi[c % 2]
                tok = slice(c * 64, (c + 1) * 64)
                for j in range(8):
                    kb.mm(c3(pr_.v)[:, j, :], BBTre[:, j, :], uTb[:, j // 4, tok], inc=False)
                    kb.mm(c3(pi_.v)[:, j, :], BBTim[:, j, :], uTb[:, j // 4, tok], inc=(j == 7))

            def emit_y(c):
                tok = slice(c * 64, (c + 1) * 64)
                xb = xbs[c % 2]
                xrb = c3(xb[:, 0:512])
                xib = c3(xb[:, 512:1024])
                for t2i in range(2):
                    for j4 in range(4):
                        j = t2i * 4 + j4
                        kb.mm(py_c[:, t2i * 64:(t2i + 1) * 64], Cre[:, j, :], xrb[:, j, :], start=(j4 == 0), stop=False, inc=False)
                        kb.mm(py_c[:, t2i * 64:(t2i + 1) * 64], Cnim[:, j, :], xib[:, j, :], start=False, stop=(j4 == 3), inc=(j4 == 3))
                for t2i in range(2):
                    kb.stt('dve', yv[:, t2i, tok], uT[:, t2i, tok], d5[:, t2i:t2i + 1], py_c[:, t2i * 64:(t2i + 1) * 64], ALU.mult, ALU.add)

            emit_bu(0)
            for c in range(8):
                if c + 1 < 8:
                    emit_bu(c + 1)
                pr_, pi_ = prpi[c % 2]
                xb = xbs[c % 2]
                kb.tt('dve', t1, ct, pr_.v, ALU.mult)
                kb.tt('dve', t2, st, pi_.v, ALU.mult)
                kb.tt('dve', bmr, t1, t2, ALU.add)
                kb.tt('dve', t3, ct, pi_.v, ALU.mult)
                kb.tt('dve', t4, st, pr_.v, ALU.mult)
                kb.tt('dve', bmi, t3, t4, ALU.subtract)
                kb.tt('dve', sm[18].v, rho.v, xrp.v, ALU.mult)
                kb.tt('dve', c3(bmr)[:, :, 0], c3(bmr)[:, :, 0], sm[18].v, ALU.add)
                kb.tt('dve', sm[13].v, rho.v, xip.v, ALU.mult)
                kb.tt('dve', c3(bmi)[:, :, 0], c3(bmi)[:, :, 0], sm[13].v, ALU.add)
                kb.scan(zr, rtb, bmr, 0.0, ALU.mult, ALU.add)
                kb.scan(zi, rtb, bmi, 0.0, ALU.mult, ALU.add)
                kb.tt('dve', t1, ct, zr, ALU.mult)
                kb.tt('dve', t2, st, zi, ALU.mult)
                kb.tt('dve', xb[:, 0:512], t1, t2, ALU.subtract)
                kb.tt('dve', t3, st, zr, ALU.mult)
                kb.tt('dve', t4, ct, zi, ALU.mult)
                kb.tt('dve', xb[:, 512:1024], t3, t4, ALU.add)
                kb.tt('dve', xrp.v, c3(t1)[:, :, 63], c3(t2)[:, :, 63], ALU.subtract)
                kb.tt('dve', xip.v, c3(t3)[:, :, 63], c3(t4)[:, :, 63], ALU.add)
                if c >= 1:
                    emit_y(c - 1)
            emit_y(7)
            gel = k3(F2[2].v)
            y3 = k3(F2[3].v)
            gelb = k3(B2[2].v)
            sq = k3(B2[3].v)
            for kc in range(2):
                y = yv[:, kc, :]
                a = F1[0].v
                b = F1[1].v
                kb.tt('dve', a, y, y, ALU.mult)
                kb.ts('dve', a, a, 0.044715, ALU.mult, 1.0, ALU.add)
                kb.tt('dve', a, a, y, ALU.mult)
                kb.act(b, a, AF.Sigmoid, scale=1.5957691216057308)
                kb.tt('dve', gel[:, kc, :], y, b, ALU.mult)
                kb.cp('dve', gelb[:, kc, :], gel[:, kc, :])
            for co in range(2):
                p = bigs[1]
                for kc in range(2):
                    kb.mm(p.v, wglu[:, kc, co * 128:(co + 1) * 128], gelb[:, kc, :], start=(kc == 0), stop=(kc == 1))
                b = F1[1].v
                kb.act(b, p.v, AF.Sigmoid)
                kb.tt('dve', y3[:, co, :], gel[:, co, :], b, ALU.mult)
                kb.act(sq[:, co, :], y3[:, co, :], AF.Square)
            pn = bigs[1]
            for kc in range(2):
                kb.mm(pn.v, cst['onesb'].v, sq[:, kc, :], start=(kc == 0), stop=(kc == 1))
            r = F1[2].v
            kb.act(r, pn.v, AF.Sqrt, bias=EPS, scale=1.0 / 256)
            kb.recip(r, r)
            for kc in range(2):
                kb.stt('dve', yT[:, kc, :], y3[:, kc, :], gainA[:, kc:kc + 1], r, ALU.mult, ALU.mult)

        def hgrn_prep(slabA, slabB, slabC, ti):
            qhT = k3(B2[0].v)
            khT = k3(B2[1].v)
            for pr in range(2):
                pq = big()
                proj_fm(pq.v, slabA, 256 + pr * 128, 128)
                pz = big()
                proj_fm(pz.v, slabB, pr * 128, 128)
                e, A, t1_, f, kk, b, d1, E1 = [F1[i].v for i in range(8)]
                kb.act(e, pz.v, AF.Exp, scale=-1.0)
                kb.ts1('dve', A, e, 1.0, ALU.add)
                kb.recip(A, A)
                kb.ts1('dve', t1_, e, E60, ALU.min)
                kb.ts1('dve', t1_, t1_, lb[:, pr:pr + 1], ALU.mult)
                kb.stt('dve', f, t1_, 1.0, A, ALU.add, ALU.mult)
                kb.act(f, f, AF.Ln)
                kb.stt('dve', kk, e, oml[:, pr:pr + 1], A, ALU.mult, ALU.mult)
                kb.scan(b, cst['resetmask'].v, f, 0.0, ALU.mult, ALU.add)
                b3 = c3(b)
                kb.tt('dve', c3(d1), b3, b3[:, :, 31:32].bc([128, 8, 64]), ALU.subtract)
                kb.act(E1, d1, AF.Exp)
                kb.tt('dve', qhT[:, pr, :], pq.v, E1, ALU.mult)
                kb.act(E1, d1, AF.Exp, scale=-1.0)
                kb.tt('dve', khT[:, pr, :], kk, E1, ALU.mult)
                kb.act(sdec[:, pr, :], b3[:, :, 63], AF.Exp)
                kb.tt('dve', sm[18].v, b3[:, :, 63], b3[:, :, 31], ALU.subtract)
                kb.act(sinj[:, pr, :], sm[18].v, AF.Exp)
                kb.act(ser[:, pr, :], b3[:, :, 31], AF.Exp)
            vb = B2[2].v.re("p (b c) -> p b c", c=256)
            sgate = F2[1].v.re("p (b c) -> p b c", c=256)
            for blk in range(NB):
                p = big()
                proj_tm(p[:, 0:256], slabB, 256, 256, blk)
                kb.cp('act', vb[:, blk, :], p[:, 0:256])
                p = big()
                proj_tm(p[:, 0:256], slabC, 0, 256, blk)
                kb.act(sgate[:, blk, :], p[:, 0:256], AF.Silu)
            khtok = B2[3].v.re("p (b r c) -> p b r c", r=2, c=128)
            for blk in range(NB):
                for pr in range(2):
                    kb.tr(ptr[0][:, pr * 128:(pr + 1) * 128], khT[:, pr, blk * 128:(blk + 1) * 128], identb.v)
                kb.cp('dve', khtok[:, blk, :, :], ptr[0][:, 0:256].re("p (r c) -> p r c", c=128))

        def hgrn_attn(ti):
            qhT = k3(B2[0].v)
            khT = k3(B2[1].v)
            vb = B2[2].v.re("p (b c) -> p b c", c=256)
            sgate = F2[1].v.re("p (b c) -> p b c", c=256)
            khtok = B2[3].v.re("p (b r c) -> p b r c", r=2, c=128)
            hpsT = [psT[0], psT[2]]
            for blk in range(NB):
                pob = hpo
                tk = slice(blk * 128, (blk + 1) * 128)
                for cc in range(2):
                    c = 2 * blk + cc
                    rows = slice(cc * 64, (cc + 1) * 64)
                    for pr in range(2):
                        kb.ts1('dve', Sbt[cc * 2 + pr].v, S32[:, pr, :], ser[:, pr, c:c + 1], ALU.mult)
                        pd = hpd[pr]
                        kb.mm(pd[:, 0:128], khtok[rows, blk, pr, :], vb[rows, blk, pr * 128:(pr + 1) * 128])
                        for half in range(2):
                            hs = slice(half * 64, (half + 1) * 64)
                            kb.ts1('dve', S32[hs, pr, :], S32[hs, pr, :], sdec[hs, pr, c:c + 1], ALU.mult)
                            kb.stt('dve', S32[hs, pr, :], pd[hs, half * 64:(half + 1) * 64], sinj[hs, pr, c:c + 1],
                                   S32[hs, pr, :], ALU.mult, ALU.add)
                for hh in range(4):
                    pr, half = hh // 2, hh % 2
                    hs = slice(half * 64, (half + 1) * 64)
                    ps = hpsT[hh % 2]
                    PT = PTt[hh]
                    kb.mm(ps.v, khT[hs, pr, tk], qhT[hs, pr, tk])
                    kb.tt('dve', PT.v, ps.v, cst['maskbd'].v, ALU.mult)
                    oc = slice(hh * 64, (hh + 1) * 64)
                    kb.mm(pob[:, oc], PT.v, vb[:, blk, oc], start=True, stop=False, inc=False)
                    kb.mm(pob[0:64, oc], qhT[hs, pr, blk * 128:blk * 128 + 64], Sbt[0 + pr][hs, :], start=False, stop=True, inc=False)
                    kb.mm(pob[64:128, oc], qhT[hs, pr, blk * 128 + 64:(blk + 1) * 128], Sbt[2 + pr][hs, :], start=False, stop=True)
                ob = F1[9][:, 0:256]
                kb.cp('act', ob, pob[:, 0:256])
                finish_tm(blk, ob.re("p (h v) -> p h v", v=64), None, gainBCD[:, 0:256], sgate[:, blk, :], 0,
                          scr=(F1[10], F1[11], sm[16], sm[17], ptr3))

        def fox_proj(slabC, slabD, ti):
            qT = k3(XA[1].v)
            for pr in range(2):
                p = big()
                proj_fm(p.v, slabC, 256 + pr * 128, 128)
                kb.cp('act', qT[:, pr, :], p.v)
                p = big()
                proj_fm(p.v, slabD, pr * 128, 128)
                kb.cp('act', kTc[:, pr, ti * T:(ti + 1) * T], p.v)
            for blk in range(NB):
                p = big()
                proj_tm(p[:, 0:256], slabD, 256, 256, blk)
                kb.cp('dve', vaug[:, ti * NB + blk, :, 0:64], p[:, 0:256].re("p (h v) -> p h v", v=64))

        def fox_attn(ti):
            qT = k3(XA[1].v)
            fpsG = [bigs[1], bigs[2]]
            fPT = [V(F1[0], F1[0].ap0.bitcast(BF16)), V(F1[1], F1[1].ap0.bitcast(BF16))]
            for qb in range(NB):
                gq = ti * NB + qb
                nk = gq + 1
                kb.mm(pc0_f.v, cst['sel127'].v, cftok[:, gq, :])
                kb.tt('dve', biasq[:, 0:nk, :], pc0_f[:, 0:4].re("p (o h) -> p o h", o=1).bc([128, nk, 4]),
                      cftok[:, 0:nk, 0:4], ALU.subtract)
                pob = po_f
                for hh in range(4):
                    pr, half = hh // 2, hh % 2
                    hs = slice(half * 64, (half + 1) * 64)
                    qv = qT[hs, pr, qb * 128:(qb + 1) * 128]
                    ngrp = (nk + 3) // 4

                    def scores(g):
                        for i in range(g * 4, min(nk, g * 4 + 4)):
                            kb.mm(fpsG[g % 2][:, (i % 4) * 128:(i % 4 + 1) * 128], kTc[hs, pr, i * 128:(i + 1) * 128], qv)
                    scores(0)
                    for g in range(ngrp):
                        if g + 1 < ngrp:
                            scores(g + 1)
                        blks = list(range(g * 4, min(nk, g * 4 + 4)))
                        for kbi in blks:
                            ps = fpsG[g % 2][:, (kbi % 4) * 128:(kbi % 4 + 1) * 128]
                            PT = fPT[g % 2][:, (kbi % 4) * 128:(kbi % 4 + 1) * 128]
                            if kbi == gq:
                                tmp = F1[8][:, 0:128]
                                kb.tt('dve', tmp, ps, cst['maskneg'].v, ALU.add)
                                kb.act(PT, tmp, AF.Exp, bias=biasq[:, kbi, hh:hh + 1], scale=0.125)
                            else:
                                kb.act(PT, ps, AF.Exp, bias=biasq[:, kbi, hh:hh + 1], scale=0.125)
                        for kbi in blks:
                            PT = fPT[g % 2][:, (kbi % 4) * 128:(kbi % 4 + 1) * 128]
                            kb.mm(pob[:, hh * 65:(hh + 1) * 65], PT, vaug[:, kbi, hh, :], start=(kbi == 0), stop=(kbi == nk - 1), inc=True)
                ob = F1[7][:, 0:260]
                kb.cp('act', ob, pob[:, 0:260])
                ob3 = ob.re("p (h v) -> p h v", v=65)
                d2 = sm[7][:, 0:4]
                kb.tt('dve', d2, ob3[:, :, 64], ob3[:, :, 64], ALU.mult)
                kb.ts1('dve', d2, d2, EPS, ALU.mult)
                finish_tm(qb, ob3[:, :, 0:64], d2, gainBCD[:, 256:512], None, 256,
                          scr=(F1[2], F1[3], sm[8], sm[9], ptr[0]))

        def mlstm_prep(slabE, slabF, ti):
            for ctt in range(4):
                p = big()
                proj_fm(p.v, slabE, ctt * 128, 128)
                kb.cp('act', qkraw[:, ctt, 3:3 + T], p.v)
            qkc = [k3(XA[2].v), k3(XA[3].v)]
            for ctt in range(4):
                acc = F1[ctt].v
                kb.ts1('dve', acc, qkraw[:, ctt, 3:3 + T], convw[:, ctt, 3:4], ALU.mult)
                for j in range(3):
                    kb.stt('dve', acc, qkraw[:, ctt, j:j + T], convw[:, ctt, j:j + 1], acc, ALU.mult, ALU.add)
                kb.cp('pool', qkraw[:, ctt, 0:3], qkraw[:, ctt, T:T + 3])
                kb.act(qkc[ctt // 2][:, ctt % 2, :], acc, AF.Silu)
            sig_o = F2[2].v.re("p (b c) -> p b c", c=256)
            for blk in range(NB):
                p = big()
                proj_tm(p.v, slabF, 0, 512, blk)
                e2h = gtok[:, blk, 4:8]
                kb.tt('dve', vhat[:, blk, :, 0:64], p[:, 0:256].re("p (h v) -> p h v", v=64),
                      e2h.re("p (h o) -> p h o", o=1).bc([128, 4, 64]), ALU.mult)
                kb.cp('dve', vhat[:, blk, :, 64], e2h)
                kb.act(sig_o[:, blk, :], p[:, 256:512], AF.Sigmoid)
            ktok = junk.v.re("p (b r c) -> p b r c", r=2, c=128)
            for blk in range(NB):
                for pr in range(2):
                    kb.tr(ptr[0][:, pr * 128:(pr + 1) * 128], qkc[1][:, pr, blk * 128:(blk + 1) * 128], identb.v)
                kb.ts1('dve', ktok[:, blk, :, :], ptr[0][:, 0:256].re("p (r c) -> p r c", c=128), 0.125, ALU.mult)

        def mlstm_attn(ti):
            qkc = [k3(XA[2].v), k3(XA[3].v)]
            sig_o = F2[2].v.re("p (b c) -> p b c", c=256)
            ktok = junk.v.re("p (b r c) -> p b r c", r=2, c=128)
            mpsT = [psT[1], psT[3]]
            for blk in range(NB):
                pob = po[1]
                tk = slice(blk * 128, (blk + 1) * 128)
                for cc in range(2):
                    c = 2 * blk + cc
                    rows = slice(cc * 64, (cc + 1) * 64)
                    for pr in range(2):
                        kb.ts1('dve', C32[:, pr, :], C32[:, pr, :], decb[:, pr, c:c + 1], ALU.mult)
                        kb.cp('dve', Cbt[cc * 2 + pr].v, C32[:, pr, :])
                        pd = pm[2]
                        kb.mm(pd[:, 0:130], ktok[rows, blk, pr, :], vhat[rows, blk, 2 * pr:2 * pr + 2, :].re("p h v -> p (h v)"))
                        for half in range(2):
                            hs = slice(half * 64, (half + 1) * 64)
                            kb.tt('dve', C32[hs, pr, :], C32[hs, pr, :], pd[hs, half * 65:(half + 1) * 65], ALU.add)
                for hh in range(4):
                    pr, half = hh // 2, hh % 2
                    hs = slice(half * 64, (half + 1) * 64)
                    ps = mpsT[hh % 2]
                    PT = PTt[4 + hh]
                    kb.mm(ps.v, qkc[1][hs, pr, tk], qkc[0][hs, pr, tk])
                    kb.tt('dve', PT.v, ps.v, cst['maskbd8'].v, ALU.mult)
                    oc = slice(hh * 65, (hh + 1) * 65)
                    kb.mm(pob[:, oc], PT.v, vhat[:, blk, hh, :], start=True, stop=False, inc=False)
                    kb.mm(pob[0:64, oc], qkc[0][hs, pr, blk * 128:blk * 128 + 64], Cbt[0 + pr][hs, :], start=False, stop=True, inc=False)
                    kb.mm(pob[64:128, oc], qkc[0][hs, pr, blk * 128 + 64:(blk + 1) * 128], Cbt[2 + pr][hs, :], start=False, stop=True)
                ob = F1[4][:, 0:260]
                kb.cp('act', ob, pob[:, 0:260])
                ob3 = ob.re("p (h v) -> p h v", v=65)
                den = ob3[:, :, 64]
                Dn = sm[14][:, 0:4]
                d2 = sm[15][:, 0:4]
                kb.stt('dve', Dn, den, -1.0, den, ALU.mult, ALU.max)
                kb.tt('dve', Dn, Dn, gtok[:, blk, 12:16], ALU.max)
                kb.tt('dve', d2, Dn, Dn, ALU.mult)
                kb.ts1('dve', d2, d2, EPS, ALU.mult)
                finish_tm(blk, ob3[:, :, 0:64], d2, gainBCD[:, 512:768], sig_o[:, blk, :], 512,
                          scr=(F1[5], F1[6], sm[10], sm[11], ptr[1]))

        def post_norm(name, l):
            kb.dma('sp', gainb.v, DV(dr[name][l].partition_broadcast(128)))
            kb.memset('dve', ss.v, 0.0)
            for b in range(NB):
                kb.act(junk.v, ffo[:, b, :], AF.Square, accum=ss[:, b:b + 1])
            kb.act(rt.v, ss.v, AF.Sqrt, bias=EPS, scale=1.0 / D)
            kb.recip(rr.v, rt.v)
            for b in range(NB):
                kb.stt('dve', ffo[:, b, :], ffo[:, b, :], rr[:, b:b + 1], gainb.v, ALU.mult, ALU.mult)
                kb.tt('pool', h[:, b, :], h[:, b, :], ffo[:, b, :], ALU.add)

        def wout_stage(l):
            for dg in range(2):
                slab = get_slab()
                for blk in range(NB):
                    p = big()
                    for kc in range(8):
                        kb.mm(p.v, yT[:, kc, blk * 128:(blk + 1) * 128], slab[:, kc, 0:512], start=(kc == 0), stop=(kc == 7))
                    kb.cp('act', ffo[:, blk, dg * 512:(dg + 1) * 512], p.v)
            post_norm('ln_mix_post', l)

        def ffn_stage(l):
            norm_to_aT(gpre_ffn)
            for fg in range(6):
                sg_ = get_slab()
                su_ = get_slab()
                ncl = min(512, DFF - fg * 512)
                for j in range(ncl // 128):
                    fc = fg * 4 + j
                    pg = big()
                    proj_fm(pg.v, sg_, j * 128, 128)
                    pu = big()
                    proj_fm(pu.v, su_, j * 128, 128)
                    kb.act(silt.v, pg.v, AF.Silu)
                    kb.tt('dve', hid[:, fc, :], silt.v, pu.v, ALU.mult)
            for dg in range(2):
                s3 = [get_slab(), get_slab(), get_slab()]
                for blk in range(NB):
                    p = big()
                    for fc in range(NFC):
                        kb.mm(p.v, hid[:, fc, blk * 128:(blk + 1) * 128], s3[fc // 8][:, fc % 8, 0:512], start=(fc == 0), stop=(fc == NFC - 1))
                    kb.cp('act', ffo[:, blk, dg * 512:(dg + 1) * 512], p.v)
            post_norm('ln_ffn_post', l)

        hspT = Tl(hsp)
        outT = Tl(out_d)
        import os
        KSTOP = int(os.environ.get('KSTOP', '99'))
        for l in range(NL):
            load_params(l)
            kb.barrier()
            if KSTOP <= 1:
                break
            for ti in range(NT):
                rowsl = slice(ti * T, (ti + 1) * T)
                if l == 0:
                    kb.dma('sp', h.v, DV(dr['x'][rowsl, :].rearrange("(b p) d -> p b d", p=128)))
                else:
                    kb.dma('sp', h.v, hspT[rowsl, :].re("(b p) d -> p b d", p=128))
                norm_to_aT(gpre_mix)
                gates(ti)
                if KSTOP <= 2:
                    break
                sA = get_slab()
                s5_proj(sA)
                sB = get_slab()
                sC = get_slab()
                hgrn_prep(sA, sB, sC, ti)
                sD = get_slab()
                fox_proj(sC, sD, ti)
                sE = get_slab()
                sF = get_slab()
                mlstm_prep(sE, sF, ti)
                kb.parallel([lambda: hgrn_attn(ti), lambda: mlstm_attn(ti), lambda: fox_attn(ti)])
                s5_main()
                if dbg and l == 0 and ti == 0:
                    kb.dma('pool', DV(dbg_d.rearrange("p (k t) -> p k t", t=T)), yT.v)
                kb.barrier()
                wout_stage(l)
                ffn_stage(l)
                dst = hspT if l < NL - 1 else outT
                kb.dma('sp', dst[rowsl, :].re("(b p) d -> p b d", p=128), h.v)
                kb.barrier()
        kb.finish()
    return nc


def kernel(**inputs):
    NT = 8
    nc = build(NT, 2)
    consts = host_consts()
    params = {name: np.ascontiguousarray(np.asarray(inputs[name], dtype=np.float32)) for name, _ in PARAM_SPECS}
    x = np.asarray(inputs['x'], dtype=np.float32)
    in_maps = []
    for b in range(8):
        m = {'x': np.ascontiguousarray(x[b])}
        m.update(params)
        for name in consts:
            m['c_' + name] = consts[name]
        in_maps.append(m)
    res = run_bass_kernel_spmd(nc, in_maps, core_ids=list(range(8)))
    return np.stack([np.asarray(r['out'], dtype=np.float32) for r in res.results], 0)
```

```python
import contextlib
import math
import numpy as np
import ml_dtypes
import concourse.bass as bass
import concourse.mybir as mybir
from concourse.bass_utils import run_bass_kernel_spmd

F32 = mybir.dt.float32
BF16 = mybir.dt.bfloat16
I32 = mybir.dt.int32
ALU = mybir.AluOpType
AF = mybir.ActivationFunctionType
AX = mybir.AxisListType

D = 1024
T = 512
NB = 4
DFF = 2816
NFC = 22
EPS = 1e-6
DIN = 3084
TWO_PI = 2.0 * math.pi
E60 = 1.1420073898156842e26


class Tl:
    def __init__(self, ap, name=""):
        self.ap0 = ap
        self.name = name
        self.lw = None
        self.rd = {}

    def __getitem__(self, k):
        return V(self, self.ap0[k])

    @property
    def v(self):
        return V(self, self.ap0)


class V:
    def __init__(self, tl, ap):
        self.tl = tl
        self.ap = ap

    def __getitem__(self, k):
        return V(self.tl, self.ap[k])

    def re(self, pat, **kw):
        return V(self.tl, self.ap.rearrange(pat, **kw))

    def bc(self, shape):
        return V(self.tl, self.ap.to_broadcast(list(shape)))

    @property
    def v(self):
        return self


def _tl(x):
    return x.tl if isinstance(x, V) else None


def _ap(x):
    return x.ap if isinstance(x, V) else x


class KB:
    def __init__(self, nc, es):
        self.nc = nc
        self.es = es
        self.eng = {'pe': nc.tensor, 'act': nc.scalar, 'dve': nc.vector, 'pool': nc.gpsimd, 'sp': nc.sync}
        self.sem = {}
        self.cnt = {}
        self.known = {}
        for e in self.eng:
            self.sem[e] = es.enter_context(nc.semaphore('s_' + e))
            self.cnt[e] = 0
            self.known[e] = {}
        import os
        self.limit = int(os.environ.get('KOPS', '100000000'))
        self.rings = {'sp': [], 'pool': []}
        self.rpos = {'sp': 0, 'pool': 0}
        for q, n in (('sp', 8), ('pool', 6)):
            for i in range(n):
                k = 'd_%s%d' % (q, i)
                self.sem[k] = es.enter_context(nc.semaphore('s_' + k))
                self.cnt[k] = 0
                self.rings[q].append(k)

    def sb(self, name, shape, dt):
        t = self.es.enter_context(self.nc.sbuf_tensor(name, list(shape), dt))
        return Tl(t[:], name)

    def _mult(self, e):
        return 16 if e.startswith('d_') else 1

    def _waits(self, eng, reads, writes):
        deps = {}
        for tl in reads:
            if tl is not None and tl.lw:
                e, q = tl.lw
                deps[e] = max(deps.get(e, 0), q)
        for tl in writes:
            if tl is None:
                continue
            if tl.lw:
                e, q = tl.lw
                deps[e] = max(deps.get(e, 0), q)
            for e, q in tl.rd.items():
                deps[e] = max(deps.get(e, 0), q)
        E = self.eng[eng]
        for e, q in deps.items():
            if e == 'pe' and eng == 'pe':
                continue
            if self.known[eng].get(e, 0) < q:
                E.wait_ge(self.sem[e], q * self._mult(e))
                self.known[eng][e] = q

    def count_ops(self, fn):
        self._counting = True
        self._ccount = 0
        try:
            fn()
        finally:
            self._counting = False
        return self._ccount

    def parallel(self, fns):
        import threading
        counts = [max(1, self.count_ops(f)) for f in fns]
        n = len(fns)
        st = {'turn': 0, 'alive': [True] * n, 'done': [0] * n, 'err': None}
        cv = threading.Condition()
        self._par = (st, cv, counts, threading.local())

        def pick_next():
            best, bf = None, None
            for i in range(n):
                if st['alive'][i]:
                    fr = st['done'][i] / counts[i]
                    if bf is None or fr < bf:
                        best, bf = i, fr
            st['turn'] = best

        def runner(i):
            self._par[3].idx = i
            with cv:
                while st['turn'] != i:
                    cv.wait()
            try:
                fns[i]()
            except BaseException as e:
                st['err'] = e
            finally:
                with cv:
                    st['alive'][i] = False
                    pick_next()
                    cv.notify_all()
        ths = [threading.Thread(target=runner, args=(i,)) for i in range(n)]
        for t in ths:
            t.start()
        for t in ths:
            t.join()
        self._par = None
        if st['err'] is not None:
            raise st['err']

    def _yield_point(self):
        par = getattr(self, '_par', None)
        if par is None:
            return
        st, cv, counts, tls = par
        i = getattr(tls, 'idx', None)
        if i is None:
            return
        st['done'][i] += 1
        if st['done'][i] % 6 == 0:
            with cv:
                best, bf = None, None
                for j in range(len(counts)):
                    if st['alive'][j]:
                        fr = st['done'][j] / counts[j]
                        if bf is None or fr < bf:
                            best, bf = j, fr
                if best != i:
                    st['turn'] = best
                    cv.notify_all()
                    while st['turn'] != i:
                        cv.wait()

    def op(self, eng, fn, reads, writes, inc=True):
        if getattr(self, '_counting', False):
            self._ccount += 1
            return
        self._yield_point()
        if getattr(self, '_par', None) is not None:
            inc = True
        self.n = getattr(self, 'n', 0) + 1
        if self.n > self.limit:
            return
        banks = []
        for tl in list(reads) + list(writes):
            bkk = getattr(tl, 'bank', None) if tl is not None else None
            if bkk is not None and bkk not in banks:
                banks.append(bkk)
        writes = list(writes) + banks
        self._waits(eng, reads, writes)
        ins = fn(self.eng[eng])
        if inc:
            self.cnt[eng] += 1
            ins.then_inc(self.sem[eng], 1)
            q = self.cnt[eng]
        else:
            q = self.cnt[eng] + 1
        for tl in writes:
            if tl is not None:
                tl.lw = (eng, q)
                tl.rd = {}
        for tl in reads:
            if tl is not None:
                tl.rd[eng] = max(tl.rd.get(eng, 0), q)

    def dma(self, q, out, in_, **kw):
        if getattr(self, '_counting', False):
            self._ccount += 1
            return
        self._yield_point()
        self.n = getattr(self, 'n', 0) + 1
        if self.n > self.limit:
            return
        ring = self.rings[q]
        k = ring[self.rpos[q] % len(ring)]
        self.rpos[q] += 1
        E = self.eng[q]
        if self.cnt[k] > 0 and self.known[q].get(k, 0) < self.cnt[k]:
            E.wait_ge(self.sem[k], 16 * self.cnt[k])
            self.known[q][k] = self.cnt[k]
        self._waits(q, [_tl(in_)], [_tl(out)])
        ins = E.dma_start(out=_ap(out), in_=_ap(in_), **kw)
        self.cnt[k] += 1
        ins.then_inc(self.sem[k], 16)
        qn = self.cnt[k]
        if _tl(out) is not None:
            out.tl.lw = (k, qn)
            out.tl.rd = {}
        if _tl(in_) is not None:
            in_.tl.rd[k] = qn

    def barrier(self):
        ce = ['pe', 'act', 'dve', 'pool']
        for e in ce:
            for f in ce:
                if e == f:
                    continue
                if self.known[e].get(f, 0) < self.cnt[f]:
                    self.eng[e].wait_ge(self.sem[f], self.cnt[f])
                    self.known[e][f] = self.cnt[f]
        for f in ce:
            if self.known['sp'].get(f, 0) < self.cnt[f]:
                self.eng['sp'].wait_ge(self.sem[f], self.cnt[f])
                self.known['sp'][f] = self.cnt[f]

    def finish(self):
        E = self.eng['sp']
        print("KB ops emitted:", getattr(self, 'n', 0), {e: self.cnt[e] for e in self.cnt})
        for f in ['pe', 'act', 'dve', 'pool']:
            if self.cnt[f] > 0:
                E.wait_ge(self.sem[f], self.cnt[f])
        for q in self.rings:
            for k in self.rings[q]:
                if self.cnt[k] > 0:
                    E.wait_ge(self.sem[k], 16 * self.cnt[k])

    def mm(self, out, lhsT, rhs, start=True, stop=True, inc=None):
        if inc is None:
            inc = stop
        self.op('pe', lambda e: e.matmul(out.ap, lhsT=lhsT.ap, rhs=rhs.ap, start=start, stop=stop),
                [lhsT.tl, rhs.tl], [out.tl], inc=inc)

    def tr(self, out, in_, ident):
        self.op('pe', lambda e: e.transpose(out.ap, in_.ap, ident.ap), [in_.tl, ident.tl], [out.tl])

    def act(self, out, in_, func, bias=None, scale=1.0, accum=None):
        reads = [in_.tl]
        kw = {}
        if bias is not None:
            kw['bias'] = _ap(bias)
            reads.append(_tl(bias))
        kw['scale'] = _ap(scale)
        reads.append(_tl(scale))
        writes = [out.tl]
        if accum is not None:
            kw['accum_out'] = accum.ap
            writes.append(accum.tl)
        self.op('act', lambda e: e.activation(out=out.ap, in_=in_.ap, func=func, **kw), reads, writes)

    def tt(self, eng, out, a, b, op):
        self.op(eng, lambda e: e.tensor_tensor(out=out.ap, in0=a.ap, in1=b.ap, op=op), [a.tl, b.tl], [out.tl])

    def ts(self, eng, out, a, s1, op0, s2, op1):
        self.op(eng, lambda e: e.tensor_scalar(out=out.ap, in0=a.ap, scalar1=_ap(s1), scalar2=_ap(s2), op0=op0, op1=op1),
                [a.tl, _tl(s1), _tl(s2)], [out.tl])

    def ts1(self, eng, out, a, s1, op):
        self.op(eng, lambda e: e.tensor_single_scalar(out=out.ap, in_=a.ap, scalar=_ap(s1), op=op),
                [a.tl, _tl(s1)], [out.tl])

    def stt(self, eng, out, a, sc, b, op0, op1):
        self.op(eng, lambda e: e.scalar_tensor_tensor(out=out.ap, in0=a.ap, scalar=_ap(sc), in1=b.ap, op0=op0, op1=op1),
                [a.tl, _tl(sc), b.tl], [out.tl])

    def cp(self, eng, out, a):
        if eng == 'act':
            self.op('act', lambda e: e.copy(out=out.ap, in_=a.ap), [a.tl], [out.tl])
        else:
            self.op(eng, lambda e: e.tensor_copy(out=out.ap, in_=a.ap), [a.tl], [out.tl])

    def recip(self, out, a):
        self.op('dve', lambda e: e.reciprocal(out=out.ap, in_=a.ap), [a.tl], [out.tl])

    def red(self, out, a, op=ALU.add):
        self.op('dve', lambda e: e.tensor_reduce(out=out.ap, in_=a.ap, axis=AX.X, op=op), [a.tl], [out.tl])

    def scan(self, out, d0, d1, init, op0, op1):
        self.op('dve', lambda e: e.tensor_tensor_scan(out=out.ap, data0=d0.ap, data1=d1.ap, initial=_ap(init), op0=op0, op1=op1),
                [d0.tl, d1.tl, _tl(init)], [out.tl])

    def memset(self, eng, out, val):
        self.op(eng, lambda e: e.memset(out.ap, val), [], [out.tl])


def host_consts():
    c = {}
    c['identf'] = np.eye(128, dtype=np.float32)
    c['identb'] = np.eye(128, dtype=np.float32).astype(ml_dtypes.bfloat16)
    s = np.arange(128)[:, None]
    t = np.arange(128)[None, :]
    bd = ((s // 64 == t // 64) & (s <= t)).astype(np.float32)
    c['maskbd'] = bd
    c['maskbd8'] = bd * 0.125
    c['maskneg'] = np.where(s <= t, 0.0, -1.0e5).astype(np.float32)
    rm = np.ones((128, 512), np.float32)
    rm[:, ::64] = 0.0
    c['resetmask'] = rm
    c['ramp'] = np.tile(np.arange(1, 65, dtype=np.float32)[None, :], (128, 1))
    r = np.arange(128)
    c['rowmask'] = np.stack([((r % 32) // 16 == 0), ((r % 32) // 16 == 1)], 1).astype(np.float32)
    sel = np.zeros((128, 128), np.float32)
    sel[127, :] = 1.0
    c['sel127'] = sel
    es = np.zeros((8, 2, 128), np.float32)
    for pr in range(2):
        for m in range(128):
            es[4 + 2 * pr + m // 64, pr, m] = 1.0
    c['esel'] = es
    c['onesb'] = np.ones((128, 128), np.float32).astype(ml_dtypes.bfloat16)
    return c


CONST_SPECS = [('identf', [128, 128], F32), ('identb', [128, 128], BF16), ('maskbd', [128, 128], F32),
               ('maskbd8', [128, 128], F32), ('maskneg', [128, 128], F32), ('resetmask', [128, 512], F32),
               ('ramp', [128, 64], F32), ('rowmask', [128, 2], F32), ('sel127', [128, 128], F32),
               ('esel', [8, 2, 128], F32), ('onesb', [128, 128], BF16)]

PARAM_SPECS = [('w_in', [2, 1024, DIN]), ('gate_bias', [2, 12]), ('s5_lambda_re', [2, 16, 64]),
               ('s5_lambda_im', [2, 16, 64]), ('s5_b_re', [2, 16, 64, 16]), ('s5_b_im', [2, 16, 64, 16]),
               ('s5_c_re', [2, 16, 16, 64]), ('s5_c_im', [2, 16, 16, 64]), ('s5_d', [2, 256]),
               ('s5_log_dt', [2, 16]), ('s5_w_glu', [2, 256, 256]), ('hgrn_lb_logits', [2, 256]),
               ('mlstm_conv_w', [2, 4, 512]), ('mix_gain', [2, 1024]), ('w_out', [2, 1024, 1024]),
               ('ln_mix_pre', [2, 1024]), ('ln_mix_post', [2, 1024]), ('ln_ffn_pre', [2, 1024]),
               ('ln_ffn_post', [2, 1024]), ('w_ffn_gate', [2, 1024, DFF]), ('w_ffn_up', [2, 1024, DFF]),
               ('w_ffn_down', [2, DFF, 1024])]


def DV(ap):
    return V(None, ap)


def build(NT, NL=2, dbg=False):
    nc = bass.Bass("TRN2", target_bir_lowering=False)
    SL = NT * T
    NBT = NT * NB
    dr = {}
    dr['x'] = nc.dram_tensor("x", [SL, D], F32, kind="ExternalInput").ap()
    for name, shp in PARAM_SPECS:
        dr[name] = nc.dram_tensor(name, shp, F32, kind="ExternalInput").ap()
    for name, shp, dt in CONST_SPECS:
        dr[name] = nc.dram_tensor("c_" + name, shp, dt, kind="ExternalInput").ap()
    out_d = nc.dram_tensor("out", [SL, D], F32, kind="ExternalOutput").ap()
    hsp = nc.dram_tensor("hspill", [SL, D], F32).ap()
    dbg_d = nc.dram_tensor("dbg", [128, 4096], F32, kind="ExternalOutput").ap() if dbg else None

    with contextlib.ExitStack() as es:
        es.enter_context(nc.allow_non_contiguous_dma(reason="small strided parameter loads"))
        kb = KB(nc, es)
        sb = kb.sb
        cst = {}
        for name, shp, dt in CONST_SPECS:
            cst[name] = sb("k_" + name, shp, dt)
            kb.dma('sp', cst[name].v, DV(dr[name]))
        identf, identb = cst['identf'], cst['identb']

        h = sb("h", [128, NB, D], F32)
        aT = sb("aT", [128, 8, T], BF16)
        NS = 4
        wslab = [sb("wslab%d" % i, [128, 8, 512], BF16) for i in range(NS)]
        gainb = sb("gainb", [128, D], F32)
        gainBCD = sb("gainBCD", [128, 768], F32)
        kTc = sb("kTc", [128, 2, SL], BF16)
        vaug = sb("vaug", [128, NBT, 4, 65], BF16)
        cftok = sb("cftok", [128, NBT, 8], F32)
        biasq = sb("biasq", [128, NBT, 4], F32)
        ctab = sb("ctab", [128, 8, 64], F32)
        stab = sb("stab", [128, 8, 64], F32)
        rhotab = sb("rhotab", [128, 8, 64], F32)
        BBTre = sb("BBTre", [128, 8, 128], BF16)
        BBTim = sb("BBTim", [128, 8, 128], BF16)
        Cre = sb("Cre", [128, 8, 128], BF16)
        Cnim = sb("Cnim", [128, 8, 128], BF16)
        rho = sb("rho", [128, 8], F32)
        d5 = sb("d5", [128, 2], F32)
        gainA = sb("gainA", [128, 2], F32)
        wglu = sb("wglu", [128, 2, 256], BF16)
        S32 = sb("S32", [128, 2, 64], F32)
        C32 = sb("C32", [128, 2, 65], F32)
        xrp = sb("xrp", [128, 8], F32)
        xip = sb("xip", [128, 8], F32)
        cumcar = sb("cumcar", [8, 1], F32)
        Gcar = sb("Gcar", [8, 1], F32)
        gpre_mix = sb("gpre_mix", [128, 8], F32)
        gpre_ffn = sb("gpre_ffn", [128, 8], F32)
        lb = sb("lb", [128, 2], F32)
        oml = sb("oml", [128, 2], F32)
        convw = sb("convw", [128, 4, 4], F32)
        gbias = sb("gbias", [8, 2], F32)
        ngA = sb("ngA", [8, 1], F32)
        wgA = sb("wgA", [128, 8, 8], BF16)
        wgB = sb("wgB", [128, 8, 8], BF16)
        gtok = sb("gtok", [128, NB, 16], F32)
        decb = sb("decb", [128, 2, 8], F32)
        vhat = sb("vhat", [128, NB, 4, 65], BF16)
        qkraw = sb("qkraw", [128, 4, 3 + T], F32)
        junk = sb("junk", [128, D], BF16)
        itile = V(junk, junk.ap0.bitcast(I32))
        ss = sb("ss", [128, 4], F32)
        rt = sb("rt", [128, 4], F32)
        rr = sb("rr", [128, 4], F32)
        sm = [sb("sm%d" % i, [128, 8], F32) for i in range(24)]
        sdec = sb("sdec", [128, 2, 8], F32)
        sinj = sb("sinj", [128, 2, 8], F32)
        ser = sb("ser", [128, 2, 8], F32)

        fa_t = es.enter_context(nc.sbuf_tensor("fa", [128, 10240], F32))
        ba_t = es.enter_context(nc.sbuf_tensor("ba", [128, 15360], BF16))
        F2 = [Tl(fa_t[:, i * 1024:(i + 1) * 1024]) for i in range(4)]
        F1 = [Tl(fa_t[:, 4096 + i * 512:4096 + (i + 1) * 512]) for i in range(12)]
        ffo = Tl(fa_t[:, 0:4096].rearrange("p (b d) -> p b d", d=D))
        silt = Tl(fa_t[:, 4096:4608])
        yT = Tl(ba_t[:, 0:4096].rearrange("p (k t) -> p k t", t=T))
        ytok = Tl(ba_t[:, 4096:7168].rearrange("p (b c) -> p b c", c=768))
        B2 = [Tl(ba_t[:, 7168 + i * 1024:7168 + (i + 1) * 1024]) for i in range(4)]
        xn = Tl(ba_t[:, 11264:15360].rearrange("p (b d) -> p b d", d=D))
        hid = Tl(ba_t[:, 0:11264].rearrange("p (f t) -> p f t", t=T))
        XA = [Tl(ba_t[:, 11264 + i * 1024:11264 + (i + 1) * 1024]) for i in range(4)]

        pb = [es.enter_context(nc.psum_tensor("pb%d" % i, [128, 512], F32)) for i in range(7)]
        pbfA = es.enter_context(nc.psum_tensor("pbfA", [128, 1024], BF16))
        bigs = [Tl(pb[i][:]) for i in range(3)]
        bigpos = [0]

        def big():
            t = bigs[bigpos[0] % 3]
            bigpos[0] += 1
            return t
        psT = [Tl(pb[3][:, 0:128]), Tl(pb[4][:, 0:128]), Tl(pb[3][:, 128:256]), Tl(pb[4][:, 128:256])]
        po = [Tl(pb[5][:, 0:260]), Tl(pb[6][:, 0:260])]
        pm = [Tl(pb[5][:, 260:390]), Tl(pb[5][:, 390:512]), Tl(pb[6][:, 260:390]), Tl(pb[6][:, 390:512])]
        ptr = [Tl(pbfA[:, 0:512]), Tl(pb[4][:].bitcast(BF16)[:, 0:512])]
        po_f = Tl(pb[0][:, 0:260])
        pc0_f = Tl(pb[0][:, 260:268])
        py_c = Tl(pb[2][:, 0:128])
        psG = [Tl(pb[3][:]), Tl(pb[4][:])]
        psG[0].bank = None
        psG[1].bank = None
        s5pr = Tl(pb[5][:])
        s5pi = Tl(pb[6][:])
        s5pi2 = Tl(pbfA[:].bitcast(F32))
        ptr3 = Tl(pb[3][:].bitcast(BF16)[:, 0:512])
        hpo = Tl(pb[5][:, 0:256])
        hpd = [Tl(pb[5][:, 256:384]), Tl(pb[5][:, 384:512])]
        bk = [Tl(None, "bank%d" % i) for i in range(8)]
        for i in range(3):
            bigs[i].bank = bk[i]
        psT[0].bank = bk[3]
        psT[2].bank = bk[3]
        psT[1].bank = bk[4]
        psT[3].bank = bk[4]
        ptr[1].bank = bk[4]
        for t_ in (po[0], pm[0], pm[1], s5pr):
            t_.bank = bk[5]
        for t_ in (po[1], pm[2], pm[3], s5pi):
            t_.bank = bk[6]
        ptr[0].bank = bk[7]
        s5pi2.bank = bk[7]
        ptr3.bank = bk[3]
        hpo.bank = bk[5]
        hpd[0].bank = bk[5]
        hpd[1].bank = bk[5]
        psG[0].bank = bk[3]
        psG[1].bank = bk[4]
        po_f.bank = bk[0]
        pc0_f.bank = bk[0]
        py_c.bank = bk[2]

        plan = []
        for l in range(NL):
            for ti in range(NT):
                W = dr['w_in'][l]
                for (c0, c1) in [(0, 512), (512, 1024), (1024, 1536), (1536, 2048), (2052, 2564), (2564, 3076)]:
                    plan.append((W[:, c0:c1], 8, c1 - c0))
                for dg in range(2):
                    plan.append((dr['w_out'][l][:, dg * 512:(dg + 1) * 512], 8, 512))
                for fg in range(6):
                    c0 = fg * 512
                    ncl = min(512, DFF - c0)
                    plan.append((dr['w_ffn_gate'][l][:, c0:c0 + ncl], 8, ncl))
                    plan.append((dr['w_ffn_up'][l][:, c0:c0 + ncl], 8, ncl))
                for dg in range(2):
                    for (f0, nf) in ((0, 8), (8, 8), (16, 6)):
                        plan.append((dr['w_ffn_down'][l][f0 * 128:(f0 + nf) * 128, dg * 512:(dg + 1) * 512], nf, 512))
        sstate = {'ptr': 0, 'issued': 0}

        def slab_issue(i):
            ap, nk, ncl = plan[i]
            sl = wslab[i % NS]
            kb.dma('pool', sl[:, 0:nk, 0:ncl], DV(ap.rearrange("(kc p) c -> p kc c", p=128)))

        def get_slab():
            idx = sstate['ptr']
            lim = min(len(plan), idx + 2)
            while sstate['issued'] < lim:
                slab_issue(sstate['issued'])
                sstate['issued'] += 1
            sstate['ptr'] += 1
            return wslab[idx % NS]

        def proj_fm(outv, slab, c0, ncl, t0=0, nt=T):
            for kc in range(8):
                kb.mm(outv, slab[:, kc, c0:c0 + ncl], aT[:, kc, t0:t0 + nt], start=(kc == 0), stop=(kc == 7))

        def proj_tm(outv, slab, c0, ncl, blk):
            for kc in range(8):
                kb.mm(outv, aT[:, kc, blk * 128:(blk + 1) * 128], slab[:, kc, c0:c0 + ncl], start=(kc == 0), stop=(kc == 7))

        def sincos(src, osin, ocos, N):
            a = F1[8][:, 0:N]
            b = F1[9][:, 0:N]
            ii = itile[:, 0:N]
            for (shift, outv) in ((0.0, osin), (math.pi / 2, ocos)):
                kb.ts('dve', a, src, shift, ALU.add, 1.0 / TWO_PI, ALU.mult)
                kb.cp('dve', ii, a)
                kb.cp('dve', b, ii)
                kb.stt('dve', a, b, -TWO_PI, src, ALU.mult, ALU.add)
                kb.ts('dve', a, a, shift, ALU.add, 3.1415925, ALU.min)
                kb.ts1('dve', a, a, -3.1415925, ALU.max)
                kb.act(outv, a, AF.Sin)

        def colsplit(ap_1d):
            return DV(ap_1d.rearrange("(kc p) -> p kc", p=128))

        def load_params(l):
            kb.dma('sp', gpre_mix.v, colsplit(dr['ln_mix_pre'][l]))
            kb.dma('sp', gpre_ffn.v, colsplit(dr['ln_ffn_pre'][l]))
            kb.dma('sp', d5.v, colsplit(dr['s5_d'][l]))
            kb.dma('sp', gainA.v, colsplit(dr['mix_gain'][l][0:256]))
            kb.dma('sp', gainBCD.v, DV(dr['mix_gain'][l][256:1024].partition_broadcast(128)))
            kb.dma('pool', wglu.v, DV(dr['s5_w_glu'][l].rearrange("(kc p) c -> p kc c", p=128)))
            for ctt in range(4):
                kb.dma('sp', convw[:, ctt, :], DV(dr['mlstm_conv_w'][l][:, ctt * 128:(ctt + 1) * 128].rearrange("j p -> p j")))
            gb = dr['gate_bias'][l]

            def col(a):
                return DV(a.rearrange("(p o) -> p o", o=1))
            kb.dma('sp', gbias[0:4, 0:1], col(gb[0:4]))
            kb.dma('sp', gbias[4:8, 0:1], col(gb[8:12]))
            kb.dma('sp', gbias[0:4, 1:2], col(gb[0:4]))
            kb.dma('sp', gbias[4:8, 1:2], col(gb[4:8]))
            kb.ts1('dve', ngA.v, gbias[:, 0:1], -1.0, ALU.mult)
            W = dr['w_in'][l]

            def gcols(c0):
                return DV(W[:, c0:c0 + 4].rearrange("(kc p) c -> p kc c", p=128))
            kb.dma('pool', wgA[:, :, 0:4], gcols(2048))
            kb.dma('pool', wgA[:, :, 4:8], gcols(3080))
            kb.dma('pool', wgB[:, :, 0:4], gcols(2048))
            kb.dma('pool', wgB[:, :, 4:8], gcols(3076))
            if l == 0:
                kb.memset('dve', lb.v, 0.0)
                kb.memset('dve', oml.v, 1.0)
            else:
                x0, x1 = sm[20], sm[21]
                kb.dma('sp', x0[:, 0:2], colsplit(dr['hgrn_lb_logits'][0]))
                kb.dma('sp', x1[:, 0:2], colsplit(dr['hgrn_lb_logits'][1]))
                kb.tt('dve', x0[:, 0:2], x0[:, 0:2], x1[:, 0:2], ALU.subtract)
                kb.act(x0[:, 0:2], x0[:, 0:2], AF.Exp)
                kb.ts1('dve', x0[:, 0:2], x0[:, 0:2], 1.0, ALU.add)
                kb.recip(lb.v, x0[:, 0:2])
                kb.ts('dve', oml.v, lb.v, -1.0, ALU.mult, 1.0, ALU.add)
            kb.memset('dve', S32.v, 0.0)
            kb.memset('dve', C32.v, 0.0)
            kb.memset('dve', xrp.v, 0.0)
            kb.memset('dve', xip.v, 0.0)
            kb.memset('dve', cumcar.v, 0.0)
            kb.memset('dve', Gcar.v, 0.0)
            kb.memset('dve', qkraw[:, :, 0:3], 0.0)
            if l == 0:
                kb.memset('dve', vaug[:, :, :, 64:65], 1.0)
            s5_prep(l)

        def s5_prep(l):
            ldt, lre, lim, dtt, lr, mag, th, sn, cs = sm[0:9]
            abre, abim, den, rden, am1, cfre, cfim, t0, t1 = sm[9:18]
            for half in range(2):
                rows = slice(half * 64, (half + 1) * 64)
                kb.dma('sp', ldt[rows, :], DV(dr['s5_log_dt'][l].rearrange("(j h) -> h j", h=2)[half].partition_broadcast(64)))
                kb.dma('sp', lre[rows, :], DV(dr['s5_lambda_re'][l].rearrange("(j h) n -> h n j", h=2)[half]))
                kb.dma('sp', lim[rows, :], DV(dr['s5_lambda_im'][l].rearrange("(j h) n -> h n j", h=2)[half]))
            bre = V(F1[0], F1[0].ap0[:, 0:128].rearrange("p (j q) -> p j q", q=16))
            bim = V(F1[1], F1[1].ap0[:, 0:128].rearrange("p (j q) -> p j q", q=16))
            bbre = V(F1[2], F1[2].ap0[:, 0:128].rearrange("p (j q) -> p j q", q=16))
            bbim = V(F1[3], F1[3].ap0[:, 0:128].rearrange("p (j q) -> p j q", q=16))
            tmpb = V(F1[4], F1[4].ap0[:, 0:128].rearrange("p (j q) -> p j q", q=16))
            for half in range(2):
                rows = slice(half * 64, (half + 1) * 64)
                kb.dma('sp', bre[rows, :, :], DV(dr['s5_b_re'][l].rearrange("(j h) n q -> h n j q", h=2)[half]))
                kb.dma('sp', bim[rows, :, :], DV(dr['s5_b_im'][l].rearrange("(j h) n q -> h n j q", h=2)[half]))
            kb.act(dtt.v, ldt.v, AF.Exp)
            kb.ts1('dve', lr.v, lre.v, -1e-4, ALU.min)
            kb.tt('dve', t0.v, lr.v, dtt.v, ALU.mult)
            kb.act(mag.v, t0.v, AF.Exp)
            kb.tt('dve', th.v, lim.v, dtt.v, ALU.mult)
            sincos(th.v, sn.v, cs.v, 8)
            kb.tt('dve', abre.v, mag.v, cs.v, ALU.mult)
            kb.tt('dve', abim.v, mag.v, sn.v, ALU.mult)
            kb.tt('dve', t0.v, lr.v, lr.v, ALU.mult)
            kb.tt('dve', t1.v, lim.v, lim.v, ALU.mult)
            kb.tt('dve', den.v, t0.v, t1.v, ALU.add)
            kb.recip(rden.v, den.v)
            kb.ts1('dve', am1.v, abre.v, -1.0, ALU.add)
            kb.tt('dve', t0.v, am1.v, lr.v, ALU.mult)
            kb.tt('dve', t1.v, abim.v, lim.v, ALU.mult)
            kb.tt('dve', t0.v, t0.v, t1.v, ALU.add)
            kb.tt('dve', cfre.v, t0.v, rden.v, ALU.mult)
            kb.tt('dve', t0.v, abim.v, lr.v, ALU.mult)
            kb.tt('dve', t1.v, am1.v, lim.v, ALU.mult)
            kb.tt('dve', t0.v, t0.v, t1.v, ALU.subtract)
            kb.tt('dve', cfim.v, t0.v, rden.v, ALU.mult)
            cfre_b = cfre.v.re("p (j o) -> p j o", o=1).bc([128, 8, 16])
            cfim_b = cfim.v.re("p (j o) -> p j o", o=1).bc([128, 8, 16])
            kb.tt('dve', bbre.v, cfre_b, bre.v, ALU.mult)
            kb.tt('dve', tmpb.v, cfim_b, bim.v, ALU.mult)
            kb.tt('dve', bbre.v, bbre.v, tmpb.v, ALU.subtract)
            kb.tt('dve', bbim.v, cfre_b, bim.v, ALU.mult)
            kb.tt('dve', tmpb.v, cfim_b, bre.v, ALU.mult)
            kb.tt('dve', bbim.v, bbim.v, tmpb.v, ALU.add)
            Xf = V(B2[0], B2[0].ap0.rearrange("p (j c) -> p j c", c=128))
            for (bb, BBT) in ((bbre, BBTre), (bbim, BBTim)):
                kb.memset('dve', Xf.v, 0.0)
                Xf4 = Xf.v.re("p (a b) c -> p a b c", b=4)
                bb4 = bb.v.re("p (a b) q -> p a b q", b=4)
                for j4 in range(4):
                    for half in range(2):
                        rows = slice(half * 64, (half + 1) * 64)
                        c0 = 32 * j4 + 16 * half
                        kb.cp('dve', Xf4[rows, :, j4, c0:c0 + 16], bb4[rows, :, j4, :])
                for g in range(2):
                    for k4 in range(4):
                        kb.tr(ptr[g][:, k4 * 128:(k4 + 1) * 128], Xf[:, g * 4 + k4, :], identb.v)
                    kb.cp('dve', BBT[:, g * 4:(g + 1) * 4, :], ptr[g].v.re("p (k t) -> p k t", t=128))
            ph = F1[10]
            th_b = th.v.re("p (j o) -> p j o", o=1).bc([128, 8, 64])
            ramp_b = cst['ramp'].v.re("p (o t) -> p o t", o=1).bc([128, 8, 64])
            kb.tt('dve', ph.v.re("p (j t) -> p j t", t=64), th_b, ramp_b, ALU.mult)
            sincos(ph.v, stab.v.re("p j t -> p (j t)"), ctab.v.re("p j t -> p (j t)"), 512)
            kb.cp('dve', rhotab.v, mag.v.re("p (j o) -> p j o", o=1).bc([128, 8, 64]))
            kb.memset('dve', rhotab[:, :, 0:1], 0.0)
            kb.cp('dve', rho.v, mag.v)
            cstt = V(F1[5], F1[5].ap0[:, 0:128].rearrange("p (t n) -> p t n", n=64))
            cst2 = V(B2[1], B2[1].ap0[:, 0:256].rearrange("p (t n) -> p t n", n=128))
            for (cname, Cm, sign) in (('s5_c_re', Cre, 1.0), ('s5_c_im', Cnim, -1.0)):
                kb.dma('sp', cstt.v, DV(dr[cname][l].rearrange("(t g) p n -> (g p) t n", t=2)))
                for half in range(2):
                    kb.ts('dve', cst2[:, :, half * 64:(half + 1) * 64], cstt.v, cst['rowmask'][:, half:half + 1], ALU.mult, sign, ALU.mult)
                kb.memset('dve', Cm.v, 0.0)
                for ti in range(2):
                    kb.tr(ptr[1][:, ti * 128:(ti + 1) * 128], cst2[:, ti, :], identb.v)
                for j in range(8):
                    c0 = 32 * (j % 4)
                    kb.cp('dve', Cm[:, j, c0:c0 + 32], ptr[1][:, (j // 4) * 128 + c0:(j // 4) * 128 + c0 + 32])

        PTt = [sb("PT%d" % i, [128, 128], BF16) for i in range(8)]
        Sbt = [sb("Sb%d" % i, [128, 64], BF16) for i in range(4)]
        Cbt = [sb("Cb%d" % i, [128, 65], BF16) for i in range(4)]

        def c3(v):
            return v.re("p (c t) -> p c t", t=64)

        def k3(v):
            return v.re("p (k t) -> p k t", t=T)

        def norm_to_aT(gpre):
            kb.memset('dve', ss.v, 0.0)
            for b in range(NB):
                kb.act(junk.v, h[:, b, :], AF.Square, accum=ss[:, b:b + 1])
            kb.act(rt.v, ss.v, AF.Sqrt, bias=EPS, scale=1.0 / D)
            kb.recip(rr.v, rt.v)
            for b in range(NB):
                kb.ts1('dve', xn[:, b, :], h[:, b, :], rr[:, b:b + 1], ALU.mult)
                for half in range(2):
                    pt = ptr[half]
                    for k4 in range(4):
                        kc = half * 4 + k4
                        kb.tr(pt[:, k4 * 128:(k4 + 1) * 128], xn[:, b, kc * 128:(kc + 1) * 128], identb.v)
                    kb.tt('dve', aT[:, half * 4:(half + 1) * 4, b * 128:(b + 1) * 128],
                          pt.v.re("p (k t) -> p k t", t=128),
                          gpre[:, half * 4:(half + 1) * 4].re("p (k o) -> p k o", o=1).bc([128, 4, 128]), ALU.mult)

        def gates(ti):
            pgA = big()
            pgB = big()
            for (pg, wg) in ((pgA, wgA), (pgB, wgB)):
                for kc in range(8):
                    kb.mm(pg[0:8, :], wg[:, kc, :], aT[:, kc, :], start=(kc == 0), stop=(kc == 7))
            eA, l1, cumA, gS, G, e2, clv, tmp = [F1[i][0:8, :] for i in range(8)]
            kb.act(eA, pgA[0:8, :], AF.Exp, bias=ngA.v, scale=-1.0)
            kb.act(l1, eA, AF.Ln, bias=1.0)
            ones8 = F1[9][0:8, :]
            kb.memset('dve', ones8, 1.0)
            kb.scan(cumA, ones8, l1, cumcar.v, ALU.mult, ALU.subtract)
            kb.cp('dve', cumcar.v, cumA[:, 511:512])
            for b in range(NB):
                kb.tr(pm[2][:, b * 8:(b + 1) * 8], cumA[:, b * 128:(b + 1) * 128], identf[0:8, 0:8])
            kb.cp('dve', cftok[:, ti * NB:(ti + 1) * NB, :], pm[2][:, 0:32].re("p (b g) -> p b g", g=8))
            kb.stt('dve', gS, pgB[0:8, :], gbias[:, 1:2], cumA, ALU.add, ALU.subtract)
            negb8 = F1[10][0:8, :]
            kb.memset('dve', negb8, -1.0e30)
            kb.scan(G, negb8, gS, Gcar.v, ALU.max, ALU.max)
            Gend_b = c3(G)[:, :, 63:64].bc([8, 8, 64])
            kb.tt('dve', c3(tmp), c3(gS), Gend_b, ALU.subtract)
            kb.act(e2, tmp, AF.Exp)
            kb.tt('dve', c3(tmp), c3(cumA), Gend_b, ALU.add)
            kb.act(clv, tmp, AF.Exp, scale=-1.0)
            Gpv = sm[22][0:8, 0:8]
            dd = sm[23][0:8, 0:8]
            dec = sm[19][0:8, 0:8]
            kb.cp('dve', Gpv[:, 0:1], Gcar.v)
            kb.cp('dve', Gpv[:, 1:8], c3(G)[:, 0:7, 63])
            kb.tt('dve', dd, Gpv, c3(G)[:, :, 63], ALU.subtract)
            kb.act(dec, dd, AF.Exp)
            kb.cp('dve', Gcar.v, c3(G)[:, 7, 63:64])
            for b in range(NB):
                kb.tr(pm[3][:, b * 16:b * 16 + 8], e2[:, b * 128:(b + 1) * 128], identf[0:8, 0:8])
                kb.tr(pm[3][:, b * 16 + 8:b * 16 + 16], clv[:, b * 128:(b + 1) * 128], identf[0:8, 0:8])
            kb.cp('dve', gtok.v, pm[3][:, 0:64].re("p (b g) -> p b g", g=16))
            for pr in range(2):
                kb.mm(pm[1][:, pr * 8:(pr + 1) * 8], cst['esel'][:, pr, :], dec)
            kb.cp('dve', decb.v, pm[1][:, 0:16].re("p (a c) -> p a c", c=8))

        def finish_tm(blk, num3, d2eps, gain2d, gate2d, c0, scr=None):
            if scr is None:
                scr = (F1[10], F1[11], sm[16], sm[17], ptr[1])
            sq = scr[0][:, 0:256].re("p (h v) -> p h v", v=64)
            s4 = scr[2][:, 0:4]
            r4 = scr[3][:, 0:4]
            y2d = scr[1][:, 0:256]
            ptrx = scr[4]
            y3d = y2d.re("p (h v) -> p h v", v=64)
            kb.tt('dve', sq, num3, num3, ALU.mult)
            kb.red(s4, sq)
            if d2eps is None:
                kb.act(r4, s4, AF.Sqrt, bias=EPS, scale=1.0 / 64)
            else:
                kb.stt('dve', s4, s4, 1.0 / 64, d2eps, ALU.mult, ALU.add)
                kb.act(r4, s4, AF.Sqrt)
            kb.recip(r4, r4)
            kb.tt('dve', y3d, num3, r4.re("p (h o) -> p h o", o=1).bc([128, 4, 64]), ALU.mult)
            if gate2d is None:
                kb.tt('dve', ytok[:, blk, c0:c0 + 256], y2d, gain2d, ALU.mult)
            else:
                kb.tt('dve', y2d, y2d, gain2d, ALU.mult)
                kb.tt('dve', ytok[:, blk, c0:c0 + 256], y2d, gate2d, ALU.mult)
            kc0 = 2 + c0 // 128
            for k in range(2):
                kb.tr(ptrx[:, k * 128:(k + 1) * 128], ytok[:, blk, c0 + k * 128:c0 + (k + 1) * 128], identb.v)
            kb.cp('act', yT[:, kc0:kc0 + 2, blk * 128:(blk + 1) * 128], ptrx[:, 0:256].re("p (k t) -> p k t", t=128))

        def s5_proj(slabA):
            uT = k3(F2[0].v)
            uTb = k3(XA[0].v)
            for kc in range(2):
                p = big()
                proj_fm(p.v, slabA, kc * 128, 128)
                kb.cp('act', uT[:, kc, :], p.v)
                kb.cp('dve', uTb[:, kc, :], p.v)

        def s5_main():
            uT = k3(F2[0].v)
            uTb = k3(XA[0].v)
            yv = k3(F2[1].v)
            prpi = [(s5pr, s5pi), (bigs[1], s5pi2)]
            xbs = [B2[1].v, XA[2].v]
            t1, t2, t3, t4, bmr, bmi, zr, zi = [F1[i].v for i in range(8)]
            ct = ctab.v.re("p j t -> p (j t)")
            st = stab.v.re("p j t -> p (j t)")
            rtb = rhotab.v.re("p j t -> p (j t)")

            def emit_bu(c):
                pr_, pi_ = prpi[c % 2]
                tok = slice(c * 64, (c + 1) * 64)
                for j in range(8):
                    kb.mm(c3(pr_.v)[:, j, :], BBTre[:, j, :], uTb[:, j // 4, tok], inc=False)
                    kb.mm(c3(pi_.v)[:, j, :], BBTim[:, j, :], uTb[:, j // 4, tok], inc=(j == 7))

            def emit_y(c):
                tok = slice(c * 64, (c + 1) * 64)
                xb = xbs[c % 2]
                xrb = c3(xb[:, 0:512])
                xib = c3(xb[:, 512:1024])
                for t2i in range(2):
                    for j4 in range(4):
                        j = t2i * 4 + j4
                        kb.mm(py_c[:, t2i * 64:(t2i + 1) * 64], Cre[:, j, :], xrb[:, j, :], start=(j4 == 0), stop=False, inc=False)
                        kb.mm(py_c[:, t2i * 64:(t2i + 1) * 64], Cnim[:, j, :], xib[:, j, :], start=False, stop=(j4 == 3), inc=(j4 == 3))
                for t2i in range(2):
                    kb.stt('dve', yv[:, t2i, tok], uT[:, t2i, tok], d5[:, t2i:t2i + 1], py_c[:, t2i * 64:(t2i + 1) * 64], ALU.mult, ALU.add)

            emit_bu(0)
            for c in range(8):
                if c + 1 < 8:
                    emit_bu(c + 1)
                pr_, pi_ = prpi[c % 2]
                xb = xbs[c % 2]
                kb.tt('dve', t1, ct, pr_.v, ALU.mult)
                kb.tt('dve', t2, st, pi_.v, ALU.mult)
                kb.tt('dve', bmr, t1, t2, ALU.add)
                kb.tt('dve', t3, ct, pi_.v, ALU.mult)
                kb.tt('dve', t4, st, pr_.v, ALU.mult)
                kb.tt('dve', bmi, t3, t4, ALU.subtract)
                kb.tt('dve', sm[18].v, rho.v, xrp.v, ALU.mult)
                kb.tt('dve', c3(bmr)[:, :, 0], c3(bmr)[:, :, 0], sm[18].v, ALU.add)
                kb.tt('dve', sm[13].v, rho.v, xip.v, ALU.mult)
                kb.tt('dve', c3(bmi)[:, :, 0], c3(bmi)[:, :, 0], sm[13].v, ALU.add)
                kb.scan(zr, rtb, bmr, 0.0, ALU.mult, ALU.add)
                kb.scan(zi, rtb, bmi, 0.0, ALU.mult, ALU.add)
                kb.tt('dve', t1, ct, zr, ALU.mult)
                kb.tt('dve', t2, st, zi, ALU.mult)
                kb.tt('dve', xb[:, 0:512], t1, t2, ALU.subtract)
                kb.tt('dve', t3, st, zr, ALU.mult)
                kb.tt('dve', t4, ct, zi, ALU.mult)
                kb.tt('dve', xb[:, 512:1024], t3, t4, ALU.add)
                kb.tt('dve', xrp.v, c3(t1)[:, :, 63], c3(t2)[:, :, 63], ALU.subtract)
                kb.tt('dve', xip.v, c3(t3)[:, :, 63], c3(t4)[:, :, 63], ALU.add)
                if c >= 1:
                    emit_y(c - 1)
            emit_y(7)
            gel = k3(F2[2].v)
            y3 = k3(F2[3].v)
            gelb = k3(B2[2].v)
            sq = k3(B2[3].v)
            for kc in range(2):
                y = yv[:, kc, :]
                a = F1[0].v
                b = F1[1].v
                kb.tt('dve', a, y, y, ALU.mult)
                kb.ts('dve', a, a, 0.044715, ALU.mult, 1.0, ALU.add)
                kb.tt('dve', a, a, y, ALU.mult)
                kb.act(b, a, AF.Sigmoid, scale=1.5957691216057308)
                kb.tt('dve', gel[:, kc, :], y, b, ALU.mult)
                kb.cp('dve', gelb[:, kc, :], gel[:, kc, :])
            for co in range(2):
                p = bigs[1]
                for kc in range(2):
                    kb.mm(p.v, wglu[:, kc, co * 128:(co + 1) * 128], gelb[:, kc, :], start=(kc == 0), stop=(kc == 1))
                b = F1[1].v
                kb.act(b, p.v, AF.Sigmoid)
                kb.tt('dve', y3[:, co, :], gel[:, co, :], b, ALU.mult)
                kb.act(sq[:, co, :], y3[:, co, :], AF.Square)
            pn = bigs[1]
            for kc in range(2):
                kb.mm(pn.v, cst['onesb'].v, sq[:, kc, :], start=(kc == 0), stop=(kc == 1))
            r = F1[2].v
            kb.act(r, pn.v, AF.Sqrt, bias=EPS, scale=1.0 / 256)
            kb.recip(r, r)
            for kc in range(2):
                kb.stt('dve', yT[:, kc, :], y3[:, kc, :], gainA[:, kc:kc + 1], r, ALU.mult, ALU.mult)

        def hgrn_prep(slabA, slabB, slabC, ti):
            qhT = k3(B2[0].v)
            khT = k3(B2[1].v)
            for pr in range(2):
                pq = big()
                proj_fm(pq.v, slabA, 256 + pr * 128, 128)
                pz = big()
                proj_fm(pz.v, slabB, pr * 128, 128)
                e, A, t1_, f, kk, b, d1, E1 = [F1[i].v for i in range(8)]
                kb.act(e, pz.v, AF.Exp, scale=-1.0)
                kb.ts1('dve', A, e, 1.0, ALU.add)
                kb.recip(A, A)
                kb.ts1('dve', t1_, e, E60, ALU.min)
                kb.ts1('dve', t1_, t1_, lb[:, pr:pr + 1], ALU.mult)
                kb.stt('dve', f, t1_, 1.0, A, ALU.add, ALU.mult)
                kb.act(f, f, AF.Ln)
                kb.stt('dve', kk, e, oml[:, pr:pr + 1], A, ALU.mult, ALU.mult)
                kb.scan(b, cst['resetmask'].v, f, 0.0, ALU.mult, ALU.add)
                b3 = c3(b)
                kb.tt('dve', c3(d1), b3, b3[:, :, 31:32].bc([128, 8, 64]), ALU.subtract)
                kb.act(E1, d1, AF.Exp)
                kb.tt('dve', qhT[:, pr, :], pq.v, E1, ALU.mult)
                kb.act(E1, d1, AF.Exp, scale=-1.0)
                kb.tt('dve', khT[:, pr, :], kk, E1, ALU.mult)
                kb.act(sdec[:, pr, :], b3[:, :, 63], AF.Exp)
                kb.tt('dve', sm[18].v, b3[:, :, 63], b3[:, :, 31], ALU.subtract)
                kb.act(sinj[:, pr, :], sm[18].v, AF.Exp)
                kb.act(ser[:, pr, :], b3[:, :, 31], AF.Exp)
            vb = B2[2].v.re("p (b c) -> p b c", c=256)
            sgate = F2[1].v.re("p (b c) -> p b c", c=256)
            for blk in range(NB):
                p = big()
                proj_tm(p[:, 0:256], slabB, 256, 256, blk)
                kb.cp('act', vb[:, blk, :], p[:, 0:256])
                p = big()
                proj_tm(p[:, 0:256], slabC, 0, 256, blk)
                kb.act(sgate[:, blk, :], p[:, 0:256], AF.Silu)
            khtok = B2[3].v.re("p (b r c) -> p b r c", r=2, c=128)
            for blk in range(NB):
                for pr in range(2):
                    kb.tr(ptr[0][:, pr * 128:(pr + 1) * 128], khT[:, pr, blk * 128:(blk + 1) * 128], identb.v)
                kb.cp('dve', khtok[:, blk, :, :], ptr[0][:, 0:256].re("p (r c) -> p r c", c=128))

        def hgrn_attn(ti):
            qhT = k3(B2[0].v)
            khT = k3(B2[1].v)
            vb = B2[2].v.re("p (b c) -> p b c", c=256)
            sgate = F2[1].v.re("p (b c) -> p b c", c=256)
            khtok = B2[3].v.re("p (b r c) -> p b r c", r=2, c=128)
            hpsT = [psT[0], psT[2]]
            for blk in range(NB):
                pob = hpo
                tk = slice(blk * 128, (blk + 1) * 128)
                for cc in range(2):
                    c = 2 * blk + cc
                    rows = slice(cc * 64, (cc + 1) * 64)
                    for pr in range(2):
                        kb.ts1('dve', Sbt[cc * 2 + pr].v, S32[:, pr, :], ser[:, pr, c:c + 1], ALU.mult)
                        pd = hpd[pr]
                        kb.mm(pd[:, 0:128], khtok[rows, blk, pr, :], vb[rows, blk, pr * 128:(pr + 1) * 128])
                        for half in range(2):
                            hs = slice(half * 64, (half + 1) * 64)
                            kb.ts1('dve', S32[hs, pr, :], S32[hs, pr, :], sdec[hs, pr, c:c + 1], ALU.mult)
                            kb.stt('dve', S32[hs, pr, :], pd[hs, half * 64:(half + 1) * 64], sinj[hs, pr, c:c + 1],
                                   S32[hs, pr, :], ALU.mult, ALU.add)
                for hh in range(4):
                    pr, half = hh // 2, hh % 2
                    hs = slice(half * 64, (half + 1) * 64)
                    ps = hpsT[hh % 2]
                    PT = PTt[hh]
                    kb.mm(ps.v, khT[hs, pr, tk], qhT[hs, pr, tk])
                    kb.tt('dve', PT.v, ps.v, cst['maskbd'].v, ALU.mult)
                    oc = slice(hh * 64, (hh + 1) * 64)
                    kb.mm(pob[:, oc], PT.v, vb[:, blk, oc], start=True, stop=False, inc=False)
                    kb.mm(pob[0:64, oc], qhT[hs, pr, blk * 128:blk * 128 + 64], Sbt[0 + pr][hs, :], start=False, stop=True, inc=False)
                    kb.mm(pob[64:128, oc], qhT[hs, pr, blk * 128 + 64:(blk + 1) * 128], Sbt[2 + pr][hs, :], start=False, stop=True)
                ob = F1[9][:, 0:256]
                kb.cp('act', ob, pob[:, 0:256])
                finish_tm(blk, ob.re("p (h v) -> p h v", v=64), None, gainBCD[:, 0:256], sgate[:, blk, :], 0,
                          scr=(F1[10], F1[11], sm[16], sm[17], ptr3))

        def fox_proj(slabC, slabD, ti):
            qT = k3(XA[1].v)
            for pr in range(2):
                p = big()
                proj_fm(p.v, slabC, 256 + pr * 128, 128)
                kb.cp('act', qT[:, pr, :], p.v)
                p = big()
                proj_fm(p.v, slabD, pr * 128, 128)
                kb.cp('act', kTc[:, pr, ti * T:(ti + 1) * T], p.v)
            for blk in range(NB):
                p = big()
                proj_tm(p[:, 0:256], slabD, 256, 256, blk)
                kb.cp('dve', vaug[:, ti * NB + blk, :, 0:64], p[:, 0:256].re("p (h v) -> p h v", v=64))

        def fox_attn(ti):
            qT = k3(XA[1].v)
            for qb in range(NB):
                gq = ti * NB + qb
                nk = gq + 1
                kb.mm(pc0_f.v, cst['sel127'].v, cftok[:, gq, :])
                kb.tt('dve', biasq[:, 0:nk, :], pc0_f[:, 0:4].re("p (o h) -> p o h", o=1).bc([128, nk, 4]),
                      cftok[:, 0:nk, 0:4], ALU.subtract)
                pob = po_f
                ngrp = (nk + 3) // 4
                glist = [(hh, g) for hh in range(4) for g in range(ngrp)]

                def scores(idx):
                    hh, g = glist[idx]
                    pr, half = hh // 2, hh % 2
                    hs = slice(half * 64, (half + 1) * 64)
                    qv = qT[hs, pr, qb * 128:(qb + 1) * 128]
                    for i in range(g * 4, min(nk, g * 4 + 4)):
                        kb.mm(psG[idx % 2][:, (i % 4) * 128:(i % 4 + 1) * 128], kTc[hs, pr, i * 128:(i + 1) * 128], qv)
                scores(0)
                for idx in range(len(glist)):
                    if idx + 1 < len(glist):
                        scores(idx + 1)
                    hh, g = glist[idx]
                    blks = list(range(g * 4, min(nk, g * 4 + 4)))
                    for kbi in blks:
                        ps = psG[idx % 2][:, (kbi % 4) * 128:(kbi % 4 + 1) * 128]
                        PT = PTt[(idx % 2) * 4 + kbi % 4]
                        if kbi == gq:
                            tmp = F1[8][:, 0:128]
                            kb.tt('dve', tmp, ps, cst['maskneg'].v, ALU.add)
                            kb.act(PT.v, tmp, AF.Exp, bias=biasq[:, kbi, hh:hh + 1], scale=0.125)
                        else:
                            kb.act(PT.v, ps, AF.Exp, bias=biasq[:, kbi, hh:hh + 1], scale=0.125)
                    for kbi in blks:
                        PT = PTt[(idx % 2) * 4 + kbi % 4]
                        kb.mm(pob[:, hh * 65:(hh + 1) * 65], PT.v, vaug[:, kbi, hh, :], start=(kbi == 0), stop=(kbi == nk - 1), inc=True)
                ob = F1[9][:, 0:260]
                kb.cp('act', ob, pob[:, 0:260])
                ob3 = ob.re("p (h v) -> p h v", v=65)
                d2 = sm[15][:, 0:4]
                kb.tt('dve', d2, ob3[:, :, 64], ob3[:, :, 64], ALU.mult)
                kb.ts1('dve', d2, d2, EPS, ALU.mult)
                finish_tm(qb, ob3[:, :, 0:64], d2, gainBCD[:, 256:512], None, 256)

        def mlstm_prep(slabE, slabF, ti):
            for ctt in range(4):
                p = big()
                proj_fm(p.v, slabE, ctt * 128, 128)
                kb.cp('act', qkraw[:, ctt, 3:3 + T], p.v)
            qkc = [k3(XA[2].v), k3(XA[3].v)]
            for ctt in range(4):
                acc = F1[ctt].v
                kb.ts1('dve', acc, qkraw[:, ctt, 3:3 + T], convw[:, ctt, 3:4], ALU.mult)
                for j in range(3):
                    kb.stt('dve', acc, qkraw[:, ctt, j:j + T], convw[:, ctt, j:j + 1], acc, ALU.mult, ALU.add)
                kb.cp('pool', qkraw[:, ctt, 0:3], qkraw[:, ctt, T:T + 3])
                kb.act(qkc[ctt // 2][:, ctt % 2, :], acc, AF.Silu)
            sig_o = F2[2].v.re("p (b c) -> p b c", c=256)
            for blk in range(NB):
                p = big()
                proj_tm(p.v, slabF, 0, 512, blk)
                e2h = gtok[:, blk, 4:8]
                kb.tt('dve', vhat[:, blk, :, 0:64], p[:, 0:256].re("p (h v) -> p h v", v=64),
                      e2h.re("p (h o) -> p h o", o=1).bc([128, 4, 64]), ALU.mult)
                kb.cp('dve', vhat[:, blk, :, 64], e2h)
                kb.act(sig_o[:, blk, :], p[:, 256:512], AF.Sigmoid)
            ktok = junk.v.re("p (b r c) -> p b r c", r=2, c=128)
            for blk in range(NB):
                for pr in range(2):
                    kb.tr(ptr[0][:, pr * 128:(pr + 1) * 128], qkc[1][:, pr, blk * 128:(blk + 1) * 128], identb.v)
                kb.ts1('dve', ktok[:, blk, :, :], ptr[0][:, 0:256].re("p (r c) -> p r c", c=128), 0.125, ALU.mult)

        def mlstm_attn(ti):
            qkc = [k3(XA[2].v), k3(XA[3].v)]
            sig_o = F2[2].v.re("p (b c) -> p b c", c=256)
            ktok = junk.v.re("p (b r c) -> p b r c", r=2, c=128)
            mpsT = [psT[1], psT[3]]
            for blk in range(NB):
                pob = po[1]
                tk = slice(blk * 128, (blk + 1) * 128)
                for cc in range(2):
                    c = 2 * blk + cc
                    rows = slice(cc * 64, (cc + 1) * 64)
                    for pr in range(2):
                        kb.ts1('dve', C32[:, pr, :], C32[:, pr, :], decb[:, pr, c:c + 1], ALU.mult)
                        kb.cp('dve', Cbt[cc * 2 + pr].v, C32[:, pr, :])
                        pd = pm[2]
                        kb.mm(pd[:, 0:130], ktok[rows, blk, pr, :], vhat[rows, blk, 2 * pr:2 * pr + 2, :].re("p h v -> p (h v)"))
                        for half in range(2):
                            hs = slice(half * 64, (half + 1) * 64)
                            kb.tt('dve', C32[hs, pr, :], C32[hs, pr, :], pd[hs, half * 65:(half + 1) * 65], ALU.add)
                for hh in range(4):
                    pr, half = hh // 2, hh % 2
                    hs = slice(half * 64, (half + 1) * 64)
                    ps = mpsT[hh % 2]
                    PT = PTt[4 + hh]
                    kb.mm(ps.v, qkc[1][hs, pr, tk], qkc[0][hs, pr, tk])
                    kb.tt('dve', PT.v, ps.v, cst['maskbd8'].v, ALU.mult)
                    oc = slice(hh * 65, (hh + 1) * 65)
                    kb.mm(pob[:, oc], PT.v, vhat[:, blk, hh, :], start=True, stop=False, inc=False)
                    kb.mm(pob[0:64, oc], qkc[0][hs, pr, blk * 128:blk * 128 + 64], Cbt[0 + pr][hs, :], start=False, stop=True, inc=False)
                    kb.mm(pob[64:128, oc], qkc[0][hs, pr, blk * 128 + 64:(blk + 1) * 128], Cbt[2 + pr][hs, :], start=False, stop=True)
                ob = F1[4][:, 0:260]
                kb.cp('act', ob, pob[:, 0:260])
                ob3 = ob.re("p (h v) -> p h v", v=65)
                den = ob3[:, :, 64]
                Dn = sm[14][:, 0:4]
                d2 = sm[15][:, 0:4]
                kb.stt('dve', Dn, den, -1.0, den, ALU.mult, ALU.max)
                kb.tt('dve', Dn, Dn, gtok[:, blk, 12:16], ALU.max)
                kb.tt('dve', d2, Dn, Dn, ALU.mult)
                kb.ts1('dve', d2, d2, EPS, ALU.mult)
                finish_tm(blk, ob3[:, :, 0:64], d2, gainBCD[:, 512:768], sig_o[:, blk, :], 512,
                          scr=(F1[5], F1[6], sm[10], sm[11], ptr[1]))

        def post_norm(name, l):
            kb.dma('sp', gainb.v, DV(dr[name][l].partition_broadcast(128)))
            kb.memset('dve', ss.v, 0.0)
            for b in range(NB):
                kb.act(junk.v, ffo[:, b, :], AF.Square, accum=ss[:, b:b + 1])
            kb.act(rt.v, ss.v, AF.Sqrt, bias=EPS, scale=1.0 / D)
            kb.recip(rr.v, rt.v)
            for b in range(NB):
                kb.stt('dve', ffo[:, b, :], ffo[:, b, :], rr[:, b:b + 1], gainb.v, ALU.mult, ALU.mult)
                kb.tt('pool', h[:, b, :], h[:, b, :], ffo[:, b, :], ALU.add)

        def wout_stage(l):
            for dg in range(2):
                slab = get_slab()
                for blk in range(NB):
                    p = big()
                    for kc in range(8):
                        kb.mm(p.v, yT[:, kc, blk * 128:(blk + 1) * 128], slab[:, kc, 0:512], start=(kc == 0), stop=(kc == 7))
                    kb.cp('act', ffo[:, blk, dg * 512:(dg + 1) * 512], p.v)
            post_norm('ln_mix_post', l)

        def ffn_stage(l):
            norm_to_aT(gpre_ffn)
            for fg in range(6):
                sg_ = get_slab()
                su_ = get_slab()
                ncl = min(512, DFF - fg * 512)
                for j in range(ncl // 128):
                    fc = fg * 4 + j
                    pg = big()
                    proj_fm(pg.v, sg_, j * 128, 128)
                    pu = big()
                    proj_fm(pu.v, su_, j * 128, 128)
                    kb.act(silt.v, pg.v, AF.Silu)
                    kb.tt('dve', hid[:, fc, :], silt.v, pu.v, ALU.mult)
            for dg in range(2):
                s3 = [get_slab(), get_slab(), get_slab()]
                for blk in range(NB):
                    p = big()
                    for fc in range(NFC):
                        kb.mm(p.v, hid[:, fc, blk * 128:(blk + 1) * 128], s3[fc // 8][:, fc % 8, 0:512], start=(fc == 0), stop=(fc == NFC - 1))
                    kb.cp('act', ffo[:, blk, dg * 512:(dg + 1) * 512], p.v)
            post_norm('ln_ffn_post', l)

        hspT = Tl(hsp)
        outT = Tl(out_d)
        import os
        KSTOP = int(os.environ.get('KSTOP', '99'))
        for l in range(NL):
            load_params(l)
            kb.barrier()
            if KSTOP <= 1:
                break
            for ti in range(NT):
                rowsl = slice(ti * T, (ti + 1) * T)
                if l == 0:
                    kb.dma('sp', h.v, DV(dr['x'][rowsl, :].rearrange("(b p) d -> p b d", p=128)))
                else:
                    kb.dma('sp', h.v, hspT[rowsl, :].re("(b p) d -> p b d", p=128))
                norm_to_aT(gpre_mix)
                gates(ti)
                if KSTOP <= 2:
                    break
                sA = get_slab()
                s5_proj(sA)
                sB = get_slab()
                sC = get_slab()
                hgrn_prep(sA, sB, sC, ti)
                sD = get_slab()
                fox_proj(sC, sD, ti)
                sE = get_slab()
                sF = get_slab()
                mlstm_prep(sE, sF, ti)
                kb.parallel([lambda: hgrn_attn(ti), lambda: mlstm_attn(ti)])
                kb.parallel([s5_main, lambda: fox_attn(ti)])
                if dbg and l == 0 and ti == 0:
                    kb.dma('pool', DV(dbg_d.rearrange("p (k t) -> p k t", t=T)), yT.v)
                kb.barrier()
                wout_stage(l)
                ffn_stage(l)
                dst = hspT if l < NL - 1 else outT
                kb.dma('sp', dst[rowsl, :].re("(b p) d -> p b d", p=128), h.v)
                kb.barrier()
        kb.finish()
    return nc


def kernel(**inputs):
    NT = 8
    nc = build(NT, 2)
    consts = host_consts()
    params = {name: np.ascontiguousarray(np.asarray(inputs[name], dtype=np.float32)) for name, _ in PARAM_SPECS}
    x = np.asarray(inputs['x'], dtype=np.float32)
    in_maps = []
    for b in range(8):
        m = {'x': np.ascontiguousarray(x[b])}
        m.update(params)
        for name in consts:
            m['c_' + name] = consts[name]
        in_maps.append(m)
    res = run_bass_kernel_spmd(nc, in_maps, core_ids=list(range(8)))
    return np.stack([np.asarray(r['out'], dtype=np.float32) for r in res.results], 0)
```

```python
import contextlib
import math
import numpy as np
import ml_dtypes
import concourse.bass as bass
import concourse.mybir as mybir
from concourse.bass_utils import run_bass_kernel_spmd

F32 = mybir.dt.float32
BF16 = mybir.dt.bfloat16
I32 = mybir.dt.int32
ALU = mybir.AluOpType
AF = mybir.ActivationFunctionType
AX = mybir.AxisListType

D = 1024
T = 512
NB = 4
DFF = 2816
NFC = 22
EPS = 1e-6
DIN = 3084
TWO_PI = 2.0 * math.pi
E60 = 1.1420073898156842e26


class Tl:
    def __init__(self, ap, name=""):
        self.ap0 = ap
        self.name = name
        self.lw = None
        self.rd = {}

    def __getitem__(self, k):
        return V(self, self.ap0[k])

    @property
    def v(self):
        return V(self, self.ap0)


class V:
    def __init__(self, tl, ap):
        self.tl = tl
        self.ap = ap

    def __getitem__(self, k):
        return V(self.tl, self.ap[k])

    def re(self, pat, **kw):
        return V(self.tl, self.ap.rearrange(pat, **kw))

    def bc(self, shape):
        return V(self.tl, self.ap.to_broadcast(list(shape)))

    @property
    def v(self):
        return self


def _tl(x):
    return x.tl if isinstance(x, V) else None


def _ap(x):
    return x.ap if isinstance(x, V) else x


class KB:
    def __init__(self, nc, es):
        self.nc = nc
        self.es = es
        self.eng = {'pe': nc.tensor, 'act': nc.scalar, 'dve': nc.vector, 'pool': nc.gpsimd, 'sp': nc.sync}
        self.sem = {}
        self.cnt = {}
        self.known = {}
        for e in self.eng:
            self.sem[e] = es.enter_context(nc.semaphore('s_' + e))
            self.cnt[e] = 0
            self.known[e] = {}
        import os
        self.limit = int(os.environ.get('KOPS', '100000000'))
        self.rings = {'sp': [], 'pool': []}
        self.rpos = {'sp': 0, 'pool': 0}
        for q, n in (('sp', 8), ('pool', 6)):
            for i in range(n):
                k = 'd_%s%d' % (q, i)
                self.sem[k] = es.enter_context(nc.semaphore('s_' + k))
                self.cnt[k] = 0
                self.rings[q].append(k)

    def sb(self, name, shape, dt):
        t = self.es.enter_context(self.nc.sbuf_tensor(name, list(shape), dt))
        return Tl(t[:], name)

    def _mult(self, e):
        return 16 if e.startswith('d_') else 1

    def _waits(self, eng, reads, writes):
        deps = {}
        for tl in reads:
            if tl is not None and tl.lw:
                e, q = tl.lw
                deps[e] = max(deps.get(e, 0), q)
        for tl in writes:
            if tl is None:
                continue
            if tl.lw:
                e, q = tl.lw
                deps[e] = max(deps.get(e, 0), q)
            for e, q in tl.rd.items():
                deps[e] = max(deps.get(e, 0), q)
        E = self.eng[eng]
        for e, q in deps.items():
            if e == 'pe' and eng == 'pe':
                continue
            if self.known[eng].get(e, 0) < q:
                E.wait_ge(self.sem[e], q * self._mult(e))
                self.known[eng][e] = q

    def count_ops(self, fn):
        self._counting = True
        self._ccount = 0
        try:
            fn()
        finally:
            self._counting = False
        return self._ccount

    def parallel(self, fns):
        import threading
        counts = [max(1, self.count_ops(f)) for f in fns]
        n = len(fns)
        st = {'turn': 0, 'alive': [True] * n, 'done': [0] * n, 'err': None}
        cv = threading.Condition()
        self._par = (st, cv, counts, threading.local())

        def pick_next():
            best, bf = None, None
            for i in range(n):
                if st['alive'][i]:
                    fr = st['done'][i] / counts[i]
                    if bf is None or fr < bf:
                        best, bf = i, fr
            st['turn'] = best

        def runner(i):
            self._par[3].idx = i
            with cv:
                while st['turn'] != i:
                    cv.wait()
            try:
                fns[i]()
            except BaseException as e:
                st['err'] = e
            finally:
                with cv:
                    st['alive'][i] = False
                    pick_next()
                    cv.notify_all()
        ths = [threading.Thread(target=runner, args=(i,)) for i in range(n)]
        for t in ths:
            t.start()
        for t in ths:
            t.join()
        self._par = None
        if st['err'] is not None:
            raise st['err']

    def _yield_point(self):
        par = getattr(self, '_par', None)
        if par is None:
            return
        st, cv, counts, tls = par
        i = getattr(tls, 'idx', None)
        if i is None:
            return
        st['done'][i] += 1
        if st['done'][i] % 6 == 0:
            with cv:
                best, bf = None, None
                for j in range(len(counts)):
                    if st['alive'][j]:
                        fr = st['done'][j] / counts[j]
                        if bf is None or fr < bf:
                            best, bf = j, fr
                if best != i:
                    st['turn'] = best
                    cv.notify_all()
                    while st['turn'] != i:
                        cv.wait()

    def op(self, eng, fn, reads, writes, inc=True):
        if getattr(self, '_counting', False):
            self._ccount += 1
            return
        self._yield_point()
        if getattr(self, '_par', None) is not None:
            inc = True
        self.n = getattr(self, 'n', 0) + 1
        if self.n > self.limit:
            return
        banks = []
        for tl in list(reads) + list(writes):
            bkk = getattr(tl, 'bank', None) if tl is not None else None
            if bkk is not None and bkk not in banks:
                banks.append(bkk)
        writes = list(writes) + banks
        self._waits(eng, reads, writes)
        ins = fn(self.eng[eng])
        if inc:
            self.cnt[eng] += 1
            ins.then_inc(self.sem[eng], 1)
            q = self.cnt[eng]
        else:
            q = self.cnt[eng] + 1
        for tl in writes:
            if tl is not None:
                tl.lw = (eng, q)
                tl.rd = {}
        for tl in reads:
            if tl is not None:
                tl.rd[eng] = max(tl.rd.get(eng, 0), q)

    def dma(self, q, out, in_, **kw):
        if getattr(self, '_counting', False):
            self._ccount += 1
            return
        self._yield_point()
        self.n = getattr(self, 'n', 0) + 1
        if self.n > self.limit:
            return
        ring = self.rings[q]
        k = ring[self.rpos[q] % len(ring)]
        self.rpos[q] += 1
        E = self.eng[q]
        if self.cnt[k] > 0 and self.known[q].get(k, 0) < self.cnt[k]:
            E.wait_ge(self.sem[k], 16 * self.cnt[k])
            self.known[q][k] = self.cnt[k]
        self._waits(q, [_tl(in_)], [_tl(out)])
        ins = E.dma_start(out=_ap(out), in_=_ap(in_), **kw)
        self.cnt[k] += 1
        ins.then_inc(self.sem[k], 16)
        qn = self.cnt[k]
        if _tl(out) is not None:
            out.tl.lw = (k, qn)
            out.tl.rd = {}
        if _tl(in_) is not None:
            in_.tl.rd[k] = qn

    def barrier(self):
        ce = ['pe', 'act', 'dve', 'pool']
        for e in ce:
            for f in ce:
                if e == f:
                    continue
                if self.known[e].get(f, 0) < self.cnt[f]:
                    self.eng[e].wait_ge(self.sem[f], self.cnt[f])
                    self.known[e][f] = self.cnt[f]
        for f in ce:
            if self.known['sp'].get(f, 0) < self.cnt[f]:
                self.eng['sp'].wait_ge(self.sem[f], self.cnt[f])
                self.known['sp'][f] = self.cnt[f]

    def finish(self):
        E = self.eng['sp']
        print("KB ops emitted:", getattr(self, 'n', 0), {e: self.cnt[e] for e in self.cnt})
        for f in ['pe', 'act', 'dve', 'pool']:
            if self.cnt[f] > 0:
                E.wait_ge(self.sem[f], self.cnt[f])
        for q in self.rings:
            for k in self.rings[q]:
                if self.cnt[k] > 0:
                    E.wait_ge(self.sem[k], 16 * self.cnt[k])

    def mm(self, out, lhsT, rhs, start=True, stop=True, inc=None):
        if inc is None:
            inc = stop
        self.op('pe', lambda e: e.matmul(out.ap, lhsT=lhsT.ap, rhs=rhs.ap, start=start, stop=stop),
                [lhsT.tl, rhs.tl], [out.tl], inc=inc)

    def tr(self, out, in_, ident):
        self.op('pe', lambda e: e.transpose(out.ap, in_.ap, ident.ap), [in_.tl, ident.tl], [out.tl])

    def act(self, out, in_, func, bias=None, scale=1.0, accum=None):
        reads = [in_.tl]
        kw = {}
        if bias is not None:
            kw['bias'] = _ap(bias)
            reads.append(_tl(bias))
        kw['scale'] = _ap(scale)
        reads.append(_tl(scale))
        writes = [out.tl]
        if accum is not None:
            kw['accum_out'] = accum.ap
            writes.append(accum.tl)
        self.op('act', lambda e: e.activation(out=out.ap, in_=in_.ap, func=func, **kw), reads, writes)

    def tt(self, eng, out, a, b, op):
        self.op(eng, lambda e: e.tensor_tensor(out=out.ap, in0=a.ap, in1=b.ap, op=op), [a.tl, b.tl], [out.tl])

    def ts(self, eng, out, a, s1, op0, s2, op1):
        self.op(eng, lambda e: e.tensor_scalar(out=out.ap, in0=a.ap, scalar1=_ap(s1), scalar2=_ap(s2), op0=op0, op1=op1),
                [a.tl, _tl(s1), _tl(s2)], [out.tl])

    def ts1(self, eng, out, a, s1, op):
        self.op(eng, lambda e: e.tensor_single_scalar(out=out.ap, in_=a.ap, scalar=_ap(s1), op=op),
                [a.tl, _tl(s1)], [out.tl])

    def stt(self, eng, out, a, sc, b, op0, op1):
        self.op(eng, lambda e: e.scalar_tensor_tensor(out=out.ap, in0=a.ap, scalar=_ap(sc), in1=b.ap, op0=op0, op1=op1),
                [a.tl, _tl(sc), b.tl], [out.tl])

    def cp(self, eng, out, a):
        if eng == 'act':
            self.op('act', lambda e: e.copy(out=out.ap, in_=a.ap), [a.tl], [out.tl])
        else:
            self.op(eng, lambda e: e.tensor_copy(out=out.ap, in_=a.ap), [a.tl], [out.tl])

    def recip(self, out, a):
        self.op('dve', lambda e: e.reciprocal(out=out.ap, in_=a.ap), [a.tl], [out.tl])

    def red(self, out, a, op=ALU.add):
        self.op('dve', lambda e: e.tensor_reduce(out=out.ap, in_=a.ap, axis=AX.X, op=op), [a.tl], [out.tl])

    def scan(self, out, d0, d1, init, op0, op1):
        self.op('dve', lambda e: e.tensor_tensor_scan(out=out.ap, data0=d0.ap, data1=d1.ap, initial=_ap(init), op0=op0, op1=op1),
                [d0.tl, d1.tl, _tl(init)], [out.tl])

    def memset(self, eng, out, val):
        self.op(eng, lambda e: e.memset(out.ap, val), [], [out.tl])


def host_consts():
    c = {}
    c['identf'] = np.eye(128, dtype=np.float32)
    c['identb'] = np.eye(128, dtype=np.float32).astype(ml_dtypes.bfloat16)
    s = np.arange(128)[:, None]
    t = np.arange(128)[None, :]
    bd = ((s // 64 == t // 64) & (s <= t)).astype(np.float32)
    c['maskbd'] = bd
    c['maskbd8'] = bd * 0.125
    c['maskneg'] = np.where(s <= t, 0.0, -1.0e5).astype(np.float32)
    c['masknb'] = np.where(s <= t, 0.0, -1.0e5).astype(np.float32).astype(ml_dtypes.bfloat16)
    rm = np.ones((128, 512), np.float32)
    rm[:, ::64] = 0.0
    c['resetmask'] = rm
    c['ramp'] = np.tile(np.arange(1, 65, dtype=np.float32)[None, :], (128, 1))
    r = np.arange(128)
    c['rowmask'] = np.stack([((r % 32) // 16 == 0), ((r % 32) // 16 == 1)], 1).astype(np.float32)
    sel = np.zeros((128, 128), np.float32)
    sel[127, :] = 1.0
    c['sel127'] = sel
    es = np.zeros((8, 2, 128), np.float32)
    for pr in range(2):
        for m in range(128):
            es[4 + 2 * pr + m // 64, pr, m] = 1.0
    c['esel'] = es
    c['onesb'] = np.ones((128, 128), np.float32).astype(ml_dtypes.bfloat16)
    return c


CONST_SPECS = [('identf', [128, 128], F32), ('identb', [128, 128], BF16), ('maskbd', [128, 128], F32),
               ('maskbd8', [128, 128], F32), ('maskneg', [128, 128], F32), ('masknb', [128, 128], BF16), ('resetmask', [128, 512], F32),
               ('ramp', [128, 64], F32), ('rowmask', [128, 2], F32), ('sel127', [128, 128], F32),
               ('esel', [8, 2, 128], F32), ('onesb', [128, 128], BF16)]

PARAM_SPECS = [('w_in', [2, 1024, DIN]), ('gate_bias', [2, 12]), ('s5_lambda_re', [2, 16, 64]),
               ('s5_lambda_im', [2, 16, 64]), ('s5_b_re', [2, 16, 64, 16]), ('s5_b_im', [2, 16, 64, 16]),
               ('s5_c_re', [2, 16, 16, 64]), ('s5_c_im', [2, 16, 16, 64]), ('s5_d', [2, 256]),
               ('s5_log_dt', [2, 16]), ('s5_w_glu', [2, 256, 256]), ('hgrn_lb_logits', [2, 256]),
               ('mlstm_conv_w', [2, 4, 512]), ('mix_gain', [2, 1024]), ('w_out', [2, 1024, 1024]),
               ('ln_mix_pre', [2, 1024]), ('ln_mix_post', [2, 1024]), ('ln_ffn_pre', [2, 1024]),
               ('ln_ffn_post', [2, 1024]), ('w_ffn_gate', [2, 1024, DFF]), ('w_ffn_up', [2, 1024, DFF]),
               ('w_ffn_down', [2, DFF, 1024])]


def DV(ap):
    return V(None, ap)


def build(NT, NL=2, dbg=False):
    nc = bass.Bass("TRN2", target_bir_lowering=False)
    SL = NT * T
    NBT = NT * NB
    dr = {}
    dr['x'] = nc.dram_tensor("x", [SL, D], F32, kind="ExternalInput").ap()
    for name, shp in PARAM_SPECS:
        dr[name] = nc.dram_tensor(name, shp, F32, kind="ExternalInput").ap()
    for name, shp, dt in CONST_SPECS:
        dr[name] = nc.dram_tensor("c_" + name, shp, dt, kind="ExternalInput").ap()
    out_d = nc.dram_tensor("out", [SL, D], F32, kind="ExternalOutput").ap()
    hsp = nc.dram_tensor("hspill", [SL, D], F32).ap()
    dbg_d = nc.dram_tensor("dbg", [128, 4096], F32, kind="ExternalOutput").ap() if dbg else None

    with contextlib.ExitStack() as es:
        es.enter_context(nc.allow_non_contiguous_dma(reason="small strided parameter loads"))
        kb = KB(nc, es)
        sb = kb.sb
        cst = {}
        for name, shp, dt in CONST_SPECS:
            cst[name] = sb("k_" + name, shp, dt)
            kb.dma('sp', cst[name].v, DV(dr[name]))
        identf, identb = cst['identf'], cst['identb']

        h = sb("h", [128, NB, D], F32)
        aT = sb("aT", [128, 8, T], BF16)
        NS = 4
        wslab = [sb("wslab%d" % i, [128, 8, 512], BF16) for i in range(NS)]
        gainb = sb("gainb", [128, D], F32)
        gainBCD = sb("gainBCD", [128, 768], F32)
        kTc = sb("kTc", [128, 2, SL], BF16)
        vaug = sb("vaug", [128, NBT, 4, 65], BF16)
        cftok = sb("cftok", [128, NBT, 8], F32)
        biasq = sb("biasq", [128, NBT, 4], F32)
        ctab = sb("ctab", [128, 8, 64], F32)
        stab = sb("stab", [128, 8, 64], F32)
        rhotab = sb("rhotab", [128, 8, 64], F32)
        BBTre = sb("BBTre", [128, 8, 128], BF16)
        BBTim = sb("BBTim", [128, 8, 128], BF16)
        Cre = sb("Cre", [128, 8, 128], BF16)
        Cnim = sb("Cnim", [128, 8, 128], BF16)
        rho = sb("rho", [128, 8], F32)
        d5 = sb("d5", [128, 2], F32)
        gainA = sb("gainA", [128, 2], F32)
        wglu = sb("wglu", [128, 2, 256], BF16)
        S32 = sb("S32", [128, 2, 64], F32)
        C32 = sb("C32", [128, 2, 65], F32)
        xrp = sb("xrp", [128, 8], F32)
        xip = sb("xip", [128, 8], F32)
        cumcar = sb("cumcar", [8, 1], F32)
        Gcar = sb("Gcar", [8, 1], F32)
        gpre_mix = sb("gpre_mix", [128, 8], F32)
        gpre_ffn = sb("gpre_ffn", [128, 8], F32)
        lb = sb("lb", [128, 2], F32)
        oml = sb("oml", [128, 2], F32)
        convw = sb("convw", [128, 4, 4], F32)
        gbias = sb("gbias", [8, 2], F32)
        ngA = sb("ngA", [8, 1], F32)
        wgA = sb("wgA", [128, 8, 8], BF16)
        wgB = sb("wgB", [128, 8, 8], BF16)
        gtok = sb("gtok", [128, NB, 16], F32)
        decb = sb("decb", [128, 2, 8], F32)
        vhat = sb("vhat", [128, NB, 4, 65], BF16)
        qkraw = sb("qkraw", [128, 4, 3 + T], F32)
        junk = sb("junk", [128, D], BF16)
        itile = V(junk, junk.ap0.bitcast(I32))
        ss = sb("ss", [128, 4], F32)
        rt = sb("rt", [128, 4], F32)
        rr = sb("rr", [128, 4], F32)
        sm = [sb("sm%d" % i, [128, 8], F32) for i in range(24)]
        sdec = sb("sdec", [128, 2, 8], F32)
        sinj = sb("sinj", [128, 2, 8], F32)
        ser = sb("ser", [128, 2, 8], F32)

        fa_t = es.enter_context(nc.sbuf_tensor("fa", [128, 10240], F32))
        ba_t = es.enter_context(nc.sbuf_tensor("ba", [128, 15360], BF16))
        F2 = [Tl(fa_t[:, i * 1024:(i + 1) * 1024]) for i in range(4)]
        F1 = [Tl(fa_t[:, 4096 + i * 512:4096 + (i + 1) * 512]) for i in range(12)]
        ffo = Tl(fa_t[:, 0:4096].rearrange("p (b d) -> p b d", d=D))
        silt = Tl(fa_t[:, 4096:4608])
        yT = Tl(ba_t[:, 0:4096].rearrange("p (k t) -> p k t", t=T))
        ytok = Tl(ba_t[:, 4096:7168].rearrange("p (b c) -> p b c", c=768))
        B2 = [Tl(ba_t[:, 7168 + i * 1024:7168 + (i + 1) * 1024]) for i in range(4)]
        xn = Tl(ba_t[:, 11264:15360].rearrange("p (b d) -> p b d", d=D))
        hid = Tl(ba_t[:, 0:11264].rearrange("p (f t) -> p f t", t=T))
        XA = [Tl(ba_t[:, 11264 + i * 1024:11264 + (i + 1) * 1024]) for i in range(4)]

        pb = [es.enter_context(nc.psum_tensor("pb%d" % i, [128, 512], F32)) for i in range(7)]
        pbfA = es.enter_context(nc.psum_tensor("pbfA", [128, 1024], BF16))
        bigs = [Tl(pb[i][:]) for i in range(3)]
        bigpos = [0]

        def big():
            t = bigs[bigpos[0] % 3]
            bigpos[0] += 1
            return t
        psT = [Tl(pb[3][:, 0:128]), Tl(pb[4][:, 0:128]), Tl(pb[3][:, 128:256]), Tl(pb[4][:, 128:256])]
        po = [Tl(pb[5][:, 0:260]), Tl(pb[6][:, 0:260])]
        pm = [Tl(pb[5][:, 260:390]), Tl(pb[5][:, 390:512]), Tl(pb[6][:, 260:390]), Tl(pb[6][:, 390:512])]
        ptr = [Tl(pbfA[:, 0:512]), Tl(pb[4][:].bitcast(BF16)[:, 0:512])]
        po_f = Tl(pb[0][:, 0:260])
        pc0_f = Tl(pb[0][:, 260:268])
        py_c = Tl(pb[2][:, 0:128])
        psG = [Tl(pb[3][:]), Tl(pb[4][:])]
        psG[0].bank = None
        psG[1].bank = None
        s5pr = Tl(pb[5][:])
        s5pi = Tl(pb[6][:])
        s5pi2 = Tl(pbfA[:].bitcast(F32))
        ptr3 = Tl(pb[3][:].bitcast(BF16)[:, 0:512])
        hpo = Tl(pb[5][:, 0:256])
        hpd = [Tl(pb[5][:, 256:384]), Tl(pb[5][:, 384:512])]
        bk = [Tl(None, "bank%d" % i) for i in range(8)]
        for i in range(3):
            bigs[i].bank = bk[i]
        psT[0].bank = bk[3]
        psT[2].bank = bk[3]
        psT[1].bank = bk[4]
        psT[3].bank = bk[4]
        ptr[1].bank = bk[4]
        for t_ in (po[0], pm[0], pm[1], s5pr):
            t_.bank = bk[5]
        for t_ in (po[1], pm[2], pm[3], s5pi):
            t_.bank = bk[6]
        ptr[0].bank = bk[7]
        s5pi2.bank = bk[7]
        ptr3.bank = bk[3]
        hpo.bank = bk[5]
        hpd[0].bank = bk[5]
        hpd[1].bank = bk[5]
        psG[0].bank = bk[3]
        psG[1].bank = bk[4]
        po_f.bank = bk[0]
        pc0_f.bank = bk[0]
        py_c.bank = bk[2]

        plan = []
        for l in range(NL):
            for ti in range(NT):
                W = dr['w_in'][l]
                for (c0, c1) in [(0, 512), (512, 1024), (1024, 1536), (1536, 2048), (2052, 2564), (2564, 3076)]:
                    plan.append((W[:, c0:c1], 8, c1 - c0))
                for dg in range(2):
                    plan.append((dr['w_out'][l][:, dg * 512:(dg + 1) * 512], 8, 512))
                for fg in range(6):
                    c0 = fg * 512
                    ncl = min(512, DFF - c0)
                    plan.append((dr['w_ffn_gate'][l][:, c0:c0 + ncl], 8, ncl))
                    plan.append((dr['w_ffn_up'][l][:, c0:c0 + ncl], 8, ncl))
                for dg in range(2):
                    for (f0, nf) in ((0, 8), (8, 8), (16, 6)):
                        plan.append((dr['w_ffn_down'][l][f0 * 128:(f0 + nf) * 128, dg * 512:(dg + 1) * 512], nf, 512))
        sstate = {'ptr': 0, 'issued': 0}

        def slab_issue(i):
            ap, nk, ncl = plan[i]
            sl = wslab[i % NS]
            kb.dma('pool', sl[:, 0:nk, 0:ncl], DV(ap.rearrange("(kc p) c -> p kc c", p=128)))

        def get_slab():
            idx = sstate['ptr']
            lim = min(len(plan), idx + 2)
            while sstate['issued'] < lim:
                slab_issue(sstate['issued'])
                sstate['issued'] += 1
            sstate['ptr'] += 1
            return wslab[idx % NS]

        def proj_fm(outv, slab, c0, ncl, t0=0, nt=T):
            for kc in range(8):
                kb.mm(outv, slab[:, kc, c0:c0 + ncl], aT[:, kc, t0:t0 + nt], start=(kc == 0), stop=(kc == 7))

        def proj_tm(outv, slab, c0, ncl, blk):
            for kc in range(8):
                kb.mm(outv, aT[:, kc, blk * 128:(blk + 1) * 128], slab[:, kc, c0:c0 + ncl], start=(kc == 0), stop=(kc == 7))

        def sincos(src, osin, ocos, N):
            a = F1[8][:, 0:N]
            b = F1[9][:, 0:N]
            ii = itile[:, 0:N]
            for (shift, outv) in ((0.0, osin), (math.pi / 2, ocos)):
                kb.ts('dve', a, src, shift, ALU.add, 1.0 / TWO_PI, ALU.mult)
                kb.cp('dve', ii, a)
                kb.cp('dve', b, ii)
                kb.stt('dve', a, b, -TWO_PI, src, ALU.mult, ALU.add)
                kb.ts('dve', a, a, shift, ALU.add, 3.1415925, ALU.min)
                kb.ts1('dve', a, a, -3.1415925, ALU.max)
                kb.act(outv, a, AF.Sin)

        def colsplit(ap_1d):
            return DV(ap_1d.rearrange("(kc p) -> p kc", p=128))

        def load_params(l):
            kb.dma('sp', gpre_mix.v, colsplit(dr['ln_mix_pre'][l]))
            kb.dma('sp', gpre_ffn.v, colsplit(dr['ln_ffn_pre'][l]))
            kb.dma('sp', d5.v, colsplit(dr['s5_d'][l]))
            kb.dma('sp', gainA.v, colsplit(dr['mix_gain'][l][0:256]))
            kb.dma('sp', gainBCD.v, DV(dr['mix_gain'][l][256:1024].partition_broadcast(128)))
            kb.dma('pool', wglu.v, DV(dr['s5_w_glu'][l].rearrange("(kc p) c -> p kc c", p=128)))
            for ctt in range(4):
                kb.dma('sp', convw[:, ctt, :], DV(dr['mlstm_conv_w'][l][:, ctt * 128:(ctt + 1) * 128].rearrange("j p -> p j")))
            gb = dr['gate_bias'][l]

            def col(a):
                return DV(a.rearrange("(p o) -> p o", o=1))
            kb.dma('sp', gbias[0:4, 0:1], col(gb[0:4]))
            kb.dma('sp', gbias[4:8, 0:1], col(gb[8:12]))
            kb.dma('sp', gbias[0:4, 1:2], col(gb[0:4]))
            kb.dma('sp', gbias[4:8, 1:2], col(gb[4:8]))
            kb.ts1('dve', ngA.v, gbias[:, 0:1], -1.0, ALU.mult)
            W = dr['w_in'][l]

            def gcols(c0):
                return DV(W[:, c0:c0 + 4].rearrange("(kc p) c -> p kc c", p=128))
            kb.dma('pool', wgA[:, :, 0:4], gcols(2048))
            kb.dma('pool', wgA[:, :, 4:8], gcols(3080))
            kb.dma('pool', wgB[:, :, 0:4], gcols(2048))
            kb.dma('pool', wgB[:, :, 4:8], gcols(3076))
            if l == 0:
                kb.memset('dve', lb.v, 0.0)
                kb.memset('dve', oml.v, 1.0)
            else:
                x0, x1 = sm[20], sm[21]
                kb.dma('sp', x0[:, 0:2], colsplit(dr['hgrn_lb_logits'][0]))
                kb.dma('sp', x1[:, 0:2], colsplit(dr['hgrn_lb_logits'][1]))
                kb.tt('dve', x0[:, 0:2], x0[:, 0:2], x1[:, 0:2], ALU.subtract)
                kb.act(x0[:, 0:2], x0[:, 0:2], AF.Exp)
                kb.ts1('dve', x0[:, 0:2], x0[:, 0:2], 1.0, ALU.add)
                kb.recip(lb.v, x0[:, 0:2])
                kb.ts('dve', oml.v, lb.v, -1.0, ALU.mult, 1.0, ALU.add)
            kb.memset('dve', S32.v, 0.0)
            kb.memset('dve', C32.v, 0.0)
            kb.memset('dve', xrp.v, 0.0)
            kb.memset('dve', xip.v, 0.0)
            kb.memset('dve', cumcar.v, 0.0)
            kb.memset('dve', Gcar.v, 0.0)
            kb.memset('dve', qkraw[:, :, 0:3], 0.0)
            if l == 0:
                kb.memset('dve', vaug[:, :, :, 64:65], 1.0)
            s5_prep(l)

        def s5_prep(l):
            ldt, lre, lim, dtt, lr, mag, th, sn, cs = sm[0:9]
            abre, abim, den, rden, am1, cfre, cfim, t0, t1 = sm[9:18]
            for half in range(2):
                rows = slice(half * 64, (half + 1) * 64)
                kb.dma('sp', ldt[rows, :], DV(dr['s5_log_dt'][l].rearrange("(j h) -> h j", h=2)[half].partition_broadcast(64)))
                kb.dma('sp', lre[rows, :], DV(dr['s5_lambda_re'][l].rearrange("(j h) n -> h n j", h=2)[half]))
                kb.dma('sp', lim[rows, :], DV(dr['s5_lambda_im'][l].rearrange("(j h) n -> h n j", h=2)[half]))
            bre = V(F1[0], F1[0].ap0[:, 0:128].rearrange("p (j q) -> p j q", q=16))
            bim = V(F1[1], F1[1].ap0[:, 0:128].rearrange("p (j q) -> p j q", q=16))
            bbre = V(F1[2], F1[2].ap0[:, 0:128].rearrange("p (j q) -> p j q", q=16))
            bbim = V(F1[3], F1[3].ap0[:, 0:128].rearrange("p (j q) -> p j q", q=16))
            tmpb = V(F1[4], F1[4].ap0[:, 0:128].rearrange("p (j q) -> p j q", q=16))
            for half in range(2):
                rows = slice(half * 64, (half + 1) * 64)
                kb.dma('sp', bre[rows, :, :], DV(dr['s5_b_re'][l].rearrange("(j h) n q -> h n j q", h=2)[half]))
                kb.dma('sp', bim[rows, :, :], DV(dr['s5_b_im'][l].rearrange("(j h) n q -> h n j q", h=2)[half]))
            kb.act(dtt.v, ldt.v, AF.Exp)
            kb.ts1('dve', lr.v, lre.v, -1e-4, ALU.min)
            kb.tt('dve', t0.v, lr.v, dtt.v, ALU.mult)
            kb.act(mag.v, t0.v, AF.Exp)
            kb.tt('dve', th.v, lim.v, dtt.v, ALU.mult)
            sincos(th.v, sn.v, cs.v, 8)
            kb.tt('dve', abre.v, mag.v, cs.v, ALU.mult)
            kb.tt('dve', abim.v, mag.v, sn.v, ALU.mult)
            kb.tt('dve', t0.v, lr.v, lr.v, ALU.mult)
            kb.tt('dve', t1.v, lim.v, lim.v, ALU.mult)
            kb.tt('dve', den.v, t0.v, t1.v, ALU.add)
            kb.recip(rden.v, den.v)
            kb.ts1('dve', am1.v, abre.v, -1.0, ALU.add)
            kb.tt('dve', t0.v, am1.v, lr.v, ALU.mult)
            kb.tt('dve', t1.v, abim.v, lim.v, ALU.mult)
            kb.tt('dve', t0.v, t0.v, t1.v, ALU.add)
            kb.tt('dve', cfre.v, t0.v, rden.v, ALU.mult)
            kb.tt('dve', t0.v, abim.v, lr.v, ALU.mult)
            kb.tt('dve', t1.v, am1.v, lim.v, ALU.mult)
            kb.tt('dve', t0.v, t0.v, t1.v, ALU.subtract)
            kb.tt('dve', cfim.v, t0.v, rden.v, ALU.mult)
            cfre_b = cfre.v.re("p (j o) -> p j o", o=1).bc([128, 8, 16])
            cfim_b = cfim.v.re("p (j o) -> p j o", o=1).bc([128, 8, 16])
            kb.tt('dve', bbre.v, cfre_b, bre.v, ALU.mult)
            kb.tt('dve', tmpb.v, cfim_b, bim.v, ALU.mult)
            kb.tt('dve', bbre.v, bbre.v, tmpb.v, ALU.subtract)
            kb.tt('dve', bbim.v, cfre_b, bim.v, ALU.mult)
            kb.tt('dve', tmpb.v, cfim_b, bre.v, ALU.mult)
            kb.tt('dve', bbim.v, bbim.v, tmpb.v, ALU.add)
            Xf = V(B2[0], B2[0].ap0.rearrange("p (j c) -> p j c", c=128))
            for (bb, BBT) in ((bbre, BBTre), (bbim, BBTim)):
                kb.memset('dve', Xf.v, 0.0)
                Xf4 = Xf.v.re("p (a b) c -> p a b c", b=4)
                bb4 = bb.v.re("p (a b) q -> p a b q", b=4)
                for j4 in range(4):
                    for half in range(2):
                        rows = slice(half * 64, (half + 1) * 64)
                        c0 = 32 * j4 + 16 * half
                        kb.cp('dve', Xf4[rows, :, j4, c0:c0 + 16], bb4[rows, :, j4, :])
                for g in range(2):
                    for k4 in range(4):
                        kb.tr(ptr[g][:, k4 * 128:(k4 + 1) * 128], Xf[:, g * 4 + k4, :], identb.v)
                    kb.cp('dve', BBT[:, g * 4:(g + 1) * 4, :], ptr[g].v.re("p (k t) -> p k t", t=128))
            ph = F1[10]
            th_b = th.v.re("p (j o) -> p j o", o=1).bc([128, 8, 64])
            ramp_b = cst['ramp'].v.re("p (o t) -> p o t", o=1).bc([128, 8, 64])
            kb.tt('dve', ph.v.re("p (j t) -> p j t", t=64), th_b, ramp_b, ALU.mult)
            sincos(ph.v, stab.v.re("p j t -> p (j t)"), ctab.v.re("p j t -> p (j t)"), 512)
            kb.cp('dve', rhotab.v, mag.v.re("p (j o) -> p j o", o=1).bc([128, 8, 64]))
            kb.memset('dve', rhotab[:, :, 0:1], 0.0)
            kb.cp('dve', rho.v, mag.v)
            cstt = V(F1[5], F1[5].ap0[:, 0:128].rearrange("p (t n) -> p t n", n=64))
            cst2 = V(B2[1], B2[1].ap0[:, 0:256].rearrange("p (t n) -> p t n", n=128))
            for (cname, Cm, sign) in (('s5_c_re', Cre, 1.0), ('s5_c_im', Cnim, -1.0)):
                kb.dma('sp', cstt.v, DV(dr[cname][l].rearrange("(t g) p n -> (g p) t n", t=2)))
                for half in range(2):
                    kb.ts('dve', cst2[:, :, half * 64:(half + 1) * 64], cstt.v, cst['rowmask'][:, half:half + 1], ALU.mult, sign, ALU.mult)
                kb.memset('dve', Cm.v, 0.0)
                for ti in range(2):
                    kb.tr(ptr[1][:, ti * 128:(ti + 1) * 128], cst2[:, ti, :], identb.v)
                for j in range(8):
                    c0 = 32 * (j % 4)
                    kb.cp('dve', Cm[:, j, c0:c0 + 32], ptr[1][:, (j // 4) * 128 + c0:(j // 4) * 128 + c0 + 32])

        PTt = [sb("PT%d" % i, [128, 128], BF16) for i in range(8)]
        Sbt = [sb("Sb%d" % i, [128, 64], BF16) for i in range(4)]
        Cbt = [sb("Cb%d" % i, [128, 65], BF16) for i in range(4)]

        def c3(v):
            return v.re("p (c t) -> p c t", t=64)

        def k3(v):
            return v.re("p (k t) -> p k t", t=T)

        def norm_to_aT(gpre):
            kb.memset('dve', ss.v, 0.0)
            for b in range(NB):
                kb.act(junk.v, h[:, b, :], AF.Square, accum=ss[:, b:b + 1])
            kb.act(rt.v, ss.v, AF.Sqrt, bias=EPS, scale=1.0 / D)
            kb.recip(rr.v, rt.v)
            for b in range(NB):
                kb.ts1('dve', xn[:, b, :], h[:, b, :], rr[:, b:b + 1], ALU.mult)
                for half in range(2):
                    pt = ptr[half]
                    for k4 in range(4):
                        kc = half * 4 + k4
                        kb.tr(pt[:, k4 * 128:(k4 + 1) * 128], xn[:, b, kc * 128:(kc + 1) * 128], identb.v)
                    kb.tt('dve', aT[:, half * 4:(half + 1) * 4, b * 128:(b + 1) * 128],
                          pt.v.re("p (k t) -> p k t", t=128),
                          gpre[:, half * 4:(half + 1) * 4].re("p (k o) -> p k o", o=1).bc([128, 4, 128]), ALU.mult)

        def gates(ti):
            pgA = big()
            pgB = big()
            for (pg, wg) in ((pgA, wgA), (pgB, wgB)):
                for kc in range(8):
                    kb.mm(pg[0:8, :], wg[:, kc, :], aT[:, kc, :], start=(kc == 0), stop=(kc == 7))
            eA, l1, cumA, gS, G, e2, clv, tmp = [F1[i][0:8, :] for i in range(8)]
            kb.act(eA, pgA[0:8, :], AF.Exp, bias=ngA.v, scale=-1.0)
            kb.act(l1, eA, AF.Ln, bias=1.0)
            ones8 = F1[9][0:8, :]
            kb.memset('dve', ones8, 1.0)
            kb.scan(cumA, ones8, l1, cumcar.v, ALU.mult, ALU.subtract)
            kb.cp('dve', cumcar.v, cumA[:, 511:512])
            for b in range(NB):
                kb.tr(pm[2][:, b * 8:(b + 1) * 8], cumA[:, b * 128:(b + 1) * 128], identf[0:8, 0:8])
            kb.cp('dve', cftok[:, ti * NB:(ti + 1) * NB, :], pm[2][:, 0:32].re("p (b g) -> p b g", g=8))
            kb.stt('dve', gS, pgB[0:8, :], gbias[:, 1:2], cumA, ALU.add, ALU.subtract)
            negb8 = F1[10][0:8, :]
            kb.memset('dve', negb8, -1.0e30)
            kb.scan(G, negb8, gS, Gcar.v, ALU.max, ALU.max)
            Gend_b = c3(G)[:, :, 63:64].bc([8, 8, 64])
            kb.tt('dve', c3(tmp), c3(gS), Gend_b, ALU.subtract)
            kb.act(e2, tmp, AF.Exp)
            kb.tt('dve', c3(tmp), c3(cumA), Gend_b, ALU.add)
            kb.act(clv, tmp, AF.Exp, scale=-1.0)
            Gpv = sm[22][0:8, 0:8]
            dd = sm[23][0:8, 0:8]
            dec = sm[19][0:8, 0:8]
            kb.cp('dve', Gpv[:, 0:1], Gcar.v)
            kb.cp('dve', Gpv[:, 1:8], c3(G)[:, 0:7, 63])
            kb.tt('dve', dd, Gpv, c3(G)[:, :, 63], ALU.subtract)
            kb.act(dec, dd, AF.Exp)
            kb.cp('dve', Gcar.v, c3(G)[:, 7, 63:64])
            for b in range(NB):
                kb.tr(pm[3][:, b * 16:b * 16 + 8], e2[:, b * 128:(b + 1) * 128], identf[0:8, 0:8])
                kb.tr(pm[3][:, b * 16 + 8:b * 16 + 16], clv[:, b * 128:(b + 1) * 128], identf[0:8, 0:8])
            kb.cp('dve', gtok.v, pm[3][:, 0:64].re("p (b g) -> p b g", g=16))
            for pr in range(2):
                kb.mm(pm[1][:, pr * 8:(pr + 1) * 8], cst['esel'][:, pr, :], dec)
            kb.cp('dve', decb.v, pm[1][:, 0:16].re("p (a c) -> p a c", c=8))

        def finish_tm(blk, num3, d2eps, gain2d, gate2d, c0, scr=None):
            if scr is None:
                scr = (F1[10], F1[11], sm[16], sm[17], ptr[1])
            sq = scr[0][:, 0:256].re("p (h v) -> p h v", v=64)
            s4 = scr[2][:, 0:4]
            r4 = scr[3][:, 0:4]
            y2d = scr[1][:, 0:256]
            ptrx = scr[4]
            y3d = y2d.re("p (h v) -> p h v", v=64)
            kb.tt('dve', sq, num3, num3, ALU.mult)
            kb.red(s4, sq)
            if d2eps is None:
                kb.act(r4, s4, AF.Sqrt, bias=EPS, scale=1.0 / 64)
            else:
                kb.stt('dve', s4, s4, 1.0 / 64, d2eps, ALU.mult, ALU.add)
                kb.act(r4, s4, AF.Sqrt)
            kb.recip(r4, r4)
            kb.tt('dve', y3d, num3, r4.re("p (h o) -> p h o", o=1).bc([128, 4, 64]), ALU.mult)
            if gate2d is None:
                kb.tt('dve', ytok[:, blk, c0:c0 + 256], y2d, gain2d, ALU.mult)
            else:
                kb.tt('dve', y2d, y2d, gain2d, ALU.mult)
                kb.tt('dve', ytok[:, blk, c0:c0 + 256], y2d, gate2d, ALU.mult)
            kc0 = 2 + c0 // 128
            for k in range(2):
                kb.tr(ptrx[:, k * 128:(k + 1) * 128], ytok[:, blk, c0 + k * 128:c0 + (k + 1) * 128], identb.v)
            kb.cp('act', yT[:, kc0:kc0 + 2, blk * 128:(blk + 1) * 128], ptrx[:, 0:256].re("p (k t) -> p k t", t=128))

        def s5_proj(slabA):
            uT = k3(F2[0].v)
            uTb = k3(XA[0].v)
            for kc in range(2):
                p = big()
                proj_fm(p.v, slabA, kc * 128, 128)
                kb.cp('act', uT[:, kc, :], p.v)
                kb.cp('dve', uTb[:, kc, :], p.v)

        def s5_main():
            uT = k3(F2[0].v)
            uTb = k3(XA[0].v)
            yv = k3(F2[1].v)
            prpi = [(s5pr, s5pi), (bigs[1], s5pi2)]
            xbs = [B2[1].v, XA[2].v]
            t1, t2, t3, t4, bmr, bmi, zr, zi = [F1[i].v for i in range(8)]
            ct = ctab.v.re("p j t -> p (j t)")
            st = stab.v.re("p j t -> p (j t)")
            rtb = rhotab.v.re("p j t -> p (j t)")

            def emit_bu(c):
                pr_, pi_ = prpi[c % 2]
                tok = slice(c * 64, (c + 1) * 64)
                for j in range(8):
                    kb.mm(c3(pr_.v)[:, j, :], BBTre[:, j, :], uTb[:, j // 4, tok], inc=False)
                    kb.mm(c3(pi_.v)[:, j, :], BBTim[:, j, :], uTb[:, j // 4, tok], inc=(j == 7))

            def emit_y(c):
                tok = slice(c * 64, (c + 1) * 64)
                xb = xbs[c % 2]
                xrb = c3(xb[:, 0:512])
                xib = c3(xb[:, 512:1024])
                for t2i in range(2):
                    for j4 in range(4):
                        j = t2i * 4 + j4
                        kb.mm(py_c[:, t2i * 64:(t2i + 1) * 64], Cre[:, j, :], xrb[:, j, :], start=(j4 == 0), stop=False, inc=False)
                        kb.mm(py_c[:, t2i * 64:(t2i + 1) * 64], Cnim[:, j, :], xib[:, j, :], start=False, stop=(j4 == 3), inc=(j4 == 3))
                for t2i in range(2):
                    kb.stt('dve', yv[:, t2i, tok], uT[:, t2i, tok], d5[:, t2i:t2i + 1], py_c[:, t2i * 64:(t2i + 1) * 64], ALU.mult, ALU.add)

            emit_bu(0)
            for c in range(8):
                if c + 1 < 8:
                    emit_bu(c + 1)
                pr_, pi_ = prpi[c % 2]
                xb = xbs[c % 2]
                kb.tt('dve', t1, ct, pr_.v, ALU.mult)
                kb.tt('dve', t2, st, pi_.v, ALU.mult)
                kb.tt('dve', bmr, t1, t2, ALU.add)
                kb.tt('dve', t3, ct, pi_.v, ALU.mult)
                kb.tt('dve', t4, st, pr_.v, ALU.mult)
                kb.tt('dve', bmi, t3, t4, ALU.subtract)
                kb.tt('dve', sm[18].v, rho.v, xrp.v, ALU.mult)
                kb.tt('dve', c3(bmr)[:, :, 0], c3(bmr)[:, :, 0], sm[18].v, ALU.add)
                kb.tt('dve', sm[13].v, rho.v, xip.v, ALU.mult)
                kb.tt('dve', c3(bmi)[:, :, 0], c3(bmi)[:, :, 0], sm[13].v, ALU.add)
                kb.scan(zr, rtb, bmr, 0.0, ALU.mult, ALU.add)
                kb.scan(zi, rtb, bmi, 0.0, ALU.mult, ALU.add)
                kb.tt('dve', t1, ct, zr, ALU.mult)
                kb.tt('dve', t2, st, zi, ALU.mult)
                kb.tt('dve', xb[:, 0:512], t1, t2, ALU.subtract)
                kb.tt('dve', t3, st, zr, ALU.mult)
                kb.tt('dve', t4, ct, zi, ALU.mult)
                kb.tt('dve', xb[:, 512:1024], t3, t4, ALU.add)
                kb.tt('dve', xrp.v, c3(t1)[:, :, 63], c3(t2)[:, :, 63], ALU.subtract)
                kb.tt('dve', xip.v, c3(t3)[:, :, 63], c3(t4)[:, :, 63], ALU.add)
                if c >= 1:
                    emit_y(c - 1)
            emit_y(7)
            gel = k3(F2[2].v)
            y3 = k3(F2[3].v)
            gelb = k3(B2[2].v)
            sq = k3(B2[3].v)
            for kc in range(2):
                y = yv[:, kc, :]
                a = F1[0].v
                b = F1[1].v
                kb.tt('dve', a, y, y, ALU.mult)
                kb.ts('dve', a, a, 0.044715, ALU.mult, 1.0, ALU.add)
                kb.tt('dve', a, a, y, ALU.mult)
                kb.act(b, a, AF.Sigmoid, scale=1.5957691216057308)
                kb.tt('dve', gel[:, kc, :], y, b, ALU.mult)
                kb.cp('dve', gelb[:, kc, :], gel[:, kc, :])
            for co in range(2):
                p = bigs[1]
                for kc in range(2):
                    kb.mm(p.v, wglu[:, kc, co * 128:(co + 1) * 128], gelb[:, kc, :], start=(kc == 0), stop=(kc == 1))
                b = F1[1].v
                kb.act(b, p.v, AF.Sigmoid)
                kb.tt('dve', y3[:, co, :], gel[:, co, :], b, ALU.mult)
                kb.act(sq[:, co, :], y3[:, co, :], AF.Square)
            pn = bigs[1]
            for kc in range(2):
                kb.mm(pn.v, cst['onesb'].v, sq[:, kc, :], start=(kc == 0), stop=(kc == 1))
            r = F1[2].v
            kb.act(r, pn.v, AF.Sqrt, bias=EPS, scale=1.0 / 256)
            kb.recip(r, r)
            for kc in range(2):
                kb.stt('dve', yT[:, kc, :], y3[:, kc, :], gainA[:, kc:kc + 1], r, ALU.mult, ALU.mult)

        def hgrn_prep(slabA, slabB, slabC, ti):
            qhT = k3(B2[0].v)
            khT = k3(B2[1].v)
            for pr in range(2):
                pq = big()
                proj_fm(pq.v, slabA, 256 + pr * 128, 128)
                pz = big()
                proj_fm(pz.v, slabB, pr * 128, 128)
                e, A, t1_, f, kk, b, d1, E1 = [F1[i].v for i in range(8)]
                kb.act(e, pz.v, AF.Exp, scale=-1.0)
                kb.ts1('dve', A, e, 1.0, ALU.add)
                kb.recip(A, A)
                kb.ts1('dve', t1_, e, E60, ALU.min)
                kb.ts1('dve', t1_, t1_, lb[:, pr:pr + 1], ALU.mult)
                kb.stt('dve', f, t1_, 1.0, A, ALU.add, ALU.mult)
                kb.act(f, f, AF.Ln)
                kb.stt('dve', kk, e, oml[:, pr:pr + 1], A, ALU.mult, ALU.mult)
                kb.scan(b, cst['resetmask'].v, f, 0.0, ALU.mult, ALU.add)
                b3 = c3(b)
                kb.tt('dve', c3(d1), b3, b3[:, :, 31:32].bc([128, 8, 64]), ALU.subtract)
                kb.act(E1, d1, AF.Exp)
                kb.tt('dve', qhT[:, pr, :], pq.v, E1, ALU.mult)
                kb.act(E1, d1, AF.Exp, scale=-1.0)
                kb.tt('dve', khT[:, pr, :], kk, E1, ALU.mult)
                kb.act(sdec[:, pr, :], b3[:, :, 63], AF.Exp)
                kb.tt('dve', sm[18].v, b3[:, :, 63], b3[:, :, 31], ALU.subtract)
                kb.act(sinj[:, pr, :], sm[18].v, AF.Exp)
                kb.act(ser[:, pr, :], b3[:, :, 31], AF.Exp)
            vb = B2[2].v.re("p (b c) -> p b c", c=256)
            sgate = F2[1].v.re("p (b c) -> p b c", c=256)
            for blk in range(NB):
                p = big()
                proj_tm(p[:, 0:256], slabB, 256, 256, blk)
                kb.cp('act', vb[:, blk, :], p[:, 0:256])
                p = big()
                proj_tm(p[:, 0:256], slabC, 0, 256, blk)
                kb.act(sgate[:, blk, :], p[:, 0:256], AF.Silu)
            khtok = B2[3].v.re("p (b r c) -> p b r c", r=2, c=128)
            for blk in range(NB):
                for pr in range(2):
                    kb.tr(ptr[0][:, pr * 128:(pr + 1) * 128], khT[:, pr, blk * 128:(blk + 1) * 128], identb.v)
                kb.cp('dve', khtok[:, blk, :, :], ptr[0][:, 0:256].re("p (r c) -> p r c", c=128))

        def hgrn_attn(ti):
            qhT = k3(B2[0].v)
            khT = k3(B2[1].v)
            vb = B2[2].v.re("p (b c) -> p b c", c=256)
            sgate = F2[1].v.re("p (b c) -> p b c", c=256)
            khtok = B2[3].v.re("p (b r c) -> p b r c", r=2, c=128)
            hpsT = [psT[0], psT[2]]
            for blk in range(NB):
                pob = hpo
                tk = slice(blk * 128, (blk + 1) * 128)
                for cc in range(2):
                    c = 2 * blk + cc
                    rows = slice(cc * 64, (cc + 1) * 64)
                    for pr in range(2):
                        kb.ts1('dve', Sbt[cc * 2 + pr].v, S32[:, pr, :], ser[:, pr, c:c + 1], ALU.mult)
                        pd = hpd[pr]
                        kb.mm(pd[:, 0:128], khtok[rows, blk, pr, :], vb[rows, blk, pr * 128:(pr + 1) * 128])
                        for half in range(2):
                            hs = slice(half * 64, (half + 1) * 64)
                            kb.ts1('dve', S32[hs, pr, :], S32[hs, pr, :], sdec[hs, pr, c:c + 1], ALU.mult)
                            kb.stt('dve', S32[hs, pr, :], pd[hs, half * 64:(half + 1) * 64], sinj[hs, pr, c:c + 1],
                                   S32[hs, pr, :], ALU.mult, ALU.add)
                for hh in range(4):
                    pr, half = hh // 2, hh % 2
                    hs = slice(half * 64, (half + 1) * 64)
                    ps = hpsT[hh % 2]
                    PT = PTt[hh]
                    kb.mm(ps.v, khT[hs, pr, tk], qhT[hs, pr, tk])
                    kb.tt('dve', PT.v, ps.v, cst['maskbd'].v, ALU.mult)
                    oc = slice(hh * 64, (hh + 1) * 64)
                    kb.mm(pob[:, oc], PT.v, vb[:, blk, oc], start=True, stop=False, inc=False)
                    kb.mm(pob[0:64, oc], qhT[hs, pr, blk * 128:blk * 128 + 64], Sbt[0 + pr][hs, :], start=False, stop=True, inc=False)
                    kb.mm(pob[64:128, oc], qhT[hs, pr, blk * 128 + 64:(blk + 1) * 128], Sbt[2 + pr][hs, :], start=False, stop=True)
                ob = F1[9][:, 0:256]
                kb.cp('act', ob, pob[:, 0:256])
                finish_tm(blk, ob.re("p (h v) -> p h v", v=64), None, gainBCD[:, 0:256], sgate[:, blk, :], 0,
                          scr=(F1[10], F1[11], sm[16], sm[17], ptr3))

        def fox_proj(slabC, slabD, ti):
            qT = k3(XA[1].v)
            for pr in range(2):
                p = big()
                proj_fm(p.v, slabC, 256 + pr * 128, 128)
                kb.cp('act', qT[:, pr, :], p.v)
                p = big()
                proj_fm(p.v, slabD, pr * 128, 128)
                kb.cp('act', kTc[:, pr, ti * T:(ti + 1) * T], p.v)
            for blk in range(NB):
                p = big()
                proj_tm(p[:, 0:256], slabD, 256, 256, blk)
                kb.cp('dve', vaug[:, ti * NB + blk, :, 0:64], p[:, 0:256].re("p (h v) -> p h v", v=64))

        def fox_attn(ti):
            qT = k3(XA[1].v)
            for qb in range(NB):
                gq = ti * NB + qb
                nk = gq + 1
                kb.mm(pc0_f.v, cst['sel127'].v, cftok[:, gq, :])
                kb.tt('dve', biasq[:, 0:nk, :], pc0_f[:, 0:4].re("p (o h) -> p o h", o=1).bc([128, nk, 4]),
                      cftok[:, 0:nk, 0:4], ALU.subtract)
                pob = po_f
                ngrp = (nk + 3) // 4
                glist = [(hh, g) for hh in range(4) for g in range(ngrp)]

                def scores(idx):
                    hh, g = glist[idx]
                    pr, half = hh // 2, hh % 2
                    hs = slice(half * 64, (half + 1) * 64)
                    qv = qT[hs, pr, qb * 128:(qb + 1) * 128]
                    for i in range(g * 4, min(nk, g * 4 + 4)):
                        pso = psG[idx % 2][:, (i % 4) * 128:(i % 4 + 1) * 128]
                        if i == gq:
                            kb.mm(pso, kTc[hs, pr, i * 128:(i + 1) * 128], qv, start=True, stop=False, inc=False)
                            kb.mm(pso, identb.v, cst['masknb'].v, start=False, stop=True)
                        else:
                            kb.mm(pso, kTc[hs, pr, i * 128:(i + 1) * 128], qv)
                scores(0)
                for idx in range(len(glist)):
                    if idx + 1 < len(glist):
                        scores(idx + 1)
                    hh, g = glist[idx]
                    blks = list(range(g * 4, min(nk, g * 4 + 4)))
                    for kbi in blks:
                        ps = psG[idx % 2][:, (kbi % 4) * 128:(kbi % 4 + 1) * 128]
                        PT = PTt[(idx % 2) * 4 + kbi % 4]
                        kb.act(PT.v, ps, AF.Exp, bias=biasq[:, kbi, hh:hh + 1], scale=0.125)
                    for kbi in blks:
                        PT = PTt[(idx % 2) * 4 + kbi % 4]
                        kb.mm(pob[:, hh * 65:(hh + 1) * 65], PT.v, vaug[:, kbi, hh, :], start=(kbi == 0), stop=(kbi == nk - 1), inc=True)
                ob = F1[9][:, 0:260]
                kb.cp('act', ob, pob[:, 0:260])
                ob3 = ob.re("p (h v) -> p h v", v=65)
                d2 = sm[15][:, 0:4]
                kb.tt('dve', d2, ob3[:, :, 64], ob3[:, :, 64], ALU.mult)
                kb.ts1('dve', d2, d2, EPS, ALU.mult)
                finish_tm(qb, ob3[:, :, 0:64], d2, gainBCD[:, 256:512], None, 256)

        def mlstm_prep(slabE, slabF, ti):
            for ctt in range(4):
                p = big()
                proj_fm(p.v, slabE, ctt * 128, 128)
                kb.cp('act', qkraw[:, ctt, 3:3 + T], p.v)
            qkc = [k3(XA[2].v), k3(XA[3].v)]
            for ctt in range(4):
                acc = F1[ctt].v
                kb.ts1('dve', acc, qkraw[:, ctt, 3:3 + T], convw[:, ctt, 3:4], ALU.mult)
                for j in range(3):
                    kb.stt('dve', acc, qkraw[:, ctt, j:j + T], convw[:, ctt, j:j + 1], acc, ALU.mult, ALU.add)
                kb.cp('pool', qkraw[:, ctt, 0:3], qkraw[:, ctt, T:T + 3])
                kb.act(qkc[ctt // 2][:, ctt % 2, :], acc, AF.Silu)
            sig_o = F2[2].v.re("p (b c) -> p b c", c=256)
            for blk in range(NB):
                p = big()
                proj_tm(p.v, slabF, 0, 512, blk)
                e2h = gtok[:, blk, 4:8]
                kb.tt('dve', vhat[:, blk, :, 0:64], p[:, 0:256].re("p (h v) -> p h v", v=64),
                      e2h.re("p (h o) -> p h o", o=1).bc([128, 4, 64]), ALU.mult)
                kb.cp('dve', vhat[:, blk, :, 64], e2h)
                kb.act(sig_o[:, blk, :], p[:, 256:512], AF.Sigmoid)
            ktok = junk.v.re("p (b r c) -> p b r c", r=2, c=128)
            for blk in range(NB):
                for pr in range(2):
                    kb.tr(ptr[0][:, pr * 128:(pr + 1) * 128], qkc[1][:, pr, blk * 128:(blk + 1) * 128], identb.v)
                kb.ts1('dve', ktok[:, blk, :, :], ptr[0][:, 0:256].re("p (r c) -> p r c", c=128), 0.125, ALU.mult)

        def mlstm_attn(ti):
            qkc = [k3(XA[2].v), k3(XA[3].v)]
            sig_o = F2[2].v.re("p (b c) -> p b c", c=256)
            ktok = junk.v.re("p (b r c) -> p b r c", r=2, c=128)
            mpsT = [psT[1], psT[3]]
            for blk in range(NB):
                pob = po[1]
                tk = slice(blk * 128, (blk + 1) * 128)
                for cc in range(2):
                    c = 2 * blk + cc
                    rows = slice(cc * 64, (cc + 1) * 64)
                    for pr in range(2):
                        kb.ts1('dve', C32[:, pr, :], C32[:, pr, :], decb[:, pr, c:c + 1], ALU.mult)
                        kb.cp('dve', Cbt[cc * 2 + pr].v, C32[:, pr, :])
                        pd = pm[2]
                        kb.mm(pd[:, 0:130], ktok[rows, blk, pr, :], vhat[rows, blk, 2 * pr:2 * pr + 2, :].re("p h v -> p (h v)"))
                        for half in range(2):
                            hs = slice(half * 64, (half + 1) * 64)
                            kb.tt('dve', C32[hs, pr, :], C32[hs, pr, :], pd[hs, half * 65:(half + 1) * 65], ALU.add)
                for hh in range(4):
                    pr, half = hh // 2, hh % 2
                    hs = slice(half * 64, (half + 1) * 64)
                    ps = mpsT[hh % 2]
                    PT = PTt[4 + hh]
                    kb.mm(ps.v, qkc[1][hs, pr, tk], qkc[0][hs, pr, tk])
                    kb.tt('dve', PT.v, ps.v, cst['maskbd8'].v, ALU.mult)
                    oc = slice(hh * 65, (hh + 1) * 65)
                    kb.mm(pob[:, oc], PT.v, vhat[:, blk, hh, :], start=True, stop=False, inc=False)
                    kb.mm(pob[0:64, oc], qkc[0][hs, pr, blk * 128:blk * 128 + 64], Cbt[0 + pr][hs, :], start=False, stop=True, inc=False)
                    kb.mm(pob[64:128, oc], qkc[0][hs, pr, blk * 128 + 64:(blk + 1) * 128], Cbt[2 + pr][hs, :], start=False, stop=True)
                ob = F1[4][:, 0:260]
                kb.cp('act', ob, pob[:, 0:260])
                ob3 = ob.re("p (h v) -> p h v", v=65)
                den = ob3[:, :, 64]
                Dn = sm[14][:, 0:4]
                d2 = sm[15][:, 0:4]
                kb.stt('dve', Dn, den, -1.0, den, ALU.mult, ALU.max)
                kb.tt('dve', Dn, Dn, gtok[:, blk, 12:16], ALU.max)
                kb.tt('dve', d2, Dn, Dn, ALU.mult)
                kb.ts1('dve', d2, d2, EPS, ALU.mult)
                finish_tm(blk, ob3[:, :, 0:64], d2, gainBCD[:, 512:768], sig_o[:, blk, :], 512,
                          scr=(F1[5], F1[6], sm[10], sm[11], ptr[1]))

        def post_norm(name, l):
            kb.dma('sp', gainb.v, DV(dr[name][l].partition_broadcast(128)))
            kb.memset('dve', ss.v, 0.0)
            for b in range(NB):
                kb.act(junk.v, ffo[:, b, :], AF.Square, accum=ss[:, b:b + 1])
            kb.act(rt.v, ss.v, AF.Sqrt, bias=EPS, scale=1.0 / D)
            kb.recip(rr.v, rt.v)
            for b in range(NB):
                kb.stt('dve', ffo[:, b, :], ffo[:, b, :], rr[:, b:b + 1], gainb.v, ALU.mult, ALU.mult)
                kb.tt('pool' if b % 2 == 0 else 'dve', h[:, b, :], h[:, b, :], ffo[:, b, :], ALU.add)

        def wout_stage(l):
            for dg in range(2):
                slab = get_slab()
                for blk in range(NB):
                    p = big()
                    for kc in range(8):
                        kb.mm(p.v, yT[:, kc, blk * 128:(blk + 1) * 128], slab[:, kc, 0:512], start=(kc == 0), stop=(kc == 7))
                    kb.cp('act', ffo[:, blk, dg * 512:(dg + 1) * 512], p.v)
            post_norm('ln_mix_post', l)

        def ffn_stage(l):
            norm_to_aT(gpre_ffn)
            for fg in range(6):
                sg_ = get_slab()
                su_ = get_slab()
                ncl = min(512, DFF - fg * 512)
                for j in range(ncl // 128):
                    fc = fg * 4 + j
                    pg = big()
                    proj_fm(pg.v, sg_, j * 128, 128)
                    pu = big()
                    proj_fm(pu.v, su_, j * 128, 128)
                    kb.act(silt.v, pg.v, AF.Silu)
                    kb.tt('dve', hid[:, fc, :], silt.v, pu.v, ALU.mult)
            for dg in range(2):
                s3 = [get_slab(), get_slab(), get_slab()]
                for blk in range(NB):
                    p = big()
                    for fc in range(NFC):
                        kb.mm(p.v, hid[:, fc, blk * 128:(blk + 1) * 128], s3[fc // 8][:, fc % 8, 0:512], start=(fc == 0), stop=(fc == NFC - 1))
                    kb.cp('act', ffo[:, blk, dg * 512:(dg + 1) * 512], p.v)
            post_norm('ln_ffn_post', l)

        hspT = Tl(hsp)
        outT = Tl(out_d)
        import os
        KSTOP = int(os.environ.get('KSTOP', '99'))
        for l in range(NL):
            load_params(l)
            kb.barrier()
            if KSTOP <= 1:
                break
            for ti in range(NT):
                rowsl = slice(ti * T, (ti + 1) * T)
                if l == 0:
                    kb.dma('sp', h.v, DV(dr['x'][rowsl, :].rearrange("(b p) d -> p b d", p=128)))
                else:
                    kb.dma('sp', h.v, hspT[rowsl, :].re("(b p) d -> p b d", p=128))
                norm_to_aT(gpre_mix)
                gates(ti)
                if KSTOP <= 2:
                    break
                sA = get_slab()
                s5_proj(sA)
                sB = get_slab()
                sC = get_slab()
                hgrn_prep(sA, sB, sC, ti)
                sD = get_slab()
                fox_proj(sC, sD, ti)
                sE = get_slab()
                sF = get_slab()
                mlstm_prep(sE, sF, ti)
                kb.parallel([lambda: hgrn_attn(ti), lambda: mlstm_attn(ti)])
                kb.parallel([s5_main, lambda: fox_attn(ti)])
                if dbg and l == 0 and ti == 0:
                    kb.dma('pool', DV(dbg_d.rearrange("p (k t) -> p k t", t=T)), yT.v)
                kb.barrier()
                wout_stage(l)
                ffn_stage(l)
                dst = hspT if l < NL - 1 else outT
                kb.dma('sp', dst[rowsl, :].re("(b p) d -> p b d", p=128), h.v)
                kb.barrier()
        kb.finish()
    return nc


def kernel(**inputs):
    NT = 8
    nc = build(NT, 2)
    consts = host_consts()
    params = {name: np.ascontiguousarray(np.asarray(inputs[name], dtype=np.float32)) for name, _ in PARAM_SPECS}
    x = np.asarray(inputs['x'], dtype=np.float32)
    in_maps = []
    for b in range(8):
        m = {'x': np.ascontiguousarray(x[b])}
        m.update(params)
        for name in consts:
            m['c_' + name] = consts[name]
        in_maps.append(m)
    res = run_bass_kernel_spmd(nc, in_maps, core_ids=list(range(8)))
    return np.stack([np.asarray(r['out'], dtype=np.float32) for r in res.results], 0)
```

```python
import contextlib
import math
import numpy as np
import ml_dtypes
import concourse.bass as bass
import concourse.mybir as mybir
from concourse.bass_utils import run_bass_kernel_spmd

F32 = mybir.dt.float32
BF16 = mybir.dt.bfloat16
I32 = mybir.dt.int32
ALU = mybir.AluOpType
AF = mybir.ActivationFunctionType
AX = mybir.AxisListType

D = 1024
T = 512
NB = 4
DFF = 2816
NFC = 22
EPS = 1e-6
DIN = 3084
TWO_PI = 2.0 * math.pi
E60 = 1.1420073898156842e26


class Tl:
    def __init__(self, ap, name=""):
        self.ap0 = ap
        self.name = name
        self.lw = None
        self.rd = {}

    def __getitem__(self, k):
        return V(self, self.ap0[k])

    @property
    def v(self):
        return V(self, self.ap0)


class V:
    def __init__(self, tl, ap):
        self.tl = tl
        self.ap = ap

    def __getitem__(self, k):
        return V(self.tl, self.ap[k])

    def re(self, pat, **kw):
        return V(self.tl, self.ap.rearrange(pat, **kw))

    def bc(self, shape):
        return V(self.tl, self.ap.to_broadcast(list(shape)))

    @property
    def v(self):
        return self


def _tl(x):
    return x.tl if isinstance(x, V) else None


def _ap(x):
    return x.ap if isinstance(x, V) else x


class KB:
    def __init__(self, nc, es):
        self.nc = nc
        self.es = es
        self.eng = {'pe': nc.tensor, 'act': nc.scalar, 'dve': nc.vector, 'pool': nc.gpsimd, 'sp': nc.sync}
        self.sem = {}
        self.cnt = {}
        self.known = {}
        for e in self.eng:
            self.sem[e] = es.enter_context(nc.semaphore('s_' + e))
            self.cnt[e] = 0
            self.known[e] = {}
        import os
        self.limit = int(os.environ.get('KOPS', '100000000'))
        self.rings = {'sp': [], 'pool': []}
        self.rpos = {'sp': 0, 'pool': 0}
        for q, n in (('sp', 8), ('pool', 6)):
            for i in range(n):
                k = 'd_%s%d' % (q, i)
                self.sem[k] = es.enter_context(nc.semaphore('s_' + k))
                self.cnt[k] = 0
                self.rings[q].append(k)

    def sb(self, name, shape, dt):
        t = self.es.enter_context(self.nc.sbuf_tensor(name, list(shape), dt))
        return Tl(t[:], name)

    def _mult(self, e):
        return 16 if e.startswith('d_') else 1

    def _waits(self, eng, reads, writes):
        deps = {}
        for tl in reads:
            if tl is not None and tl.lw:
                e, q = tl.lw
                deps[e] = max(deps.get(e, 0), q)
        for tl in writes:
            if tl is None:
                continue
            if tl.lw:
                e, q = tl.lw
                deps[e] = max(deps.get(e, 0), q)
            for e, q in tl.rd.items():
                deps[e] = max(deps.get(e, 0), q)
        E = self.eng[eng]
        for e, q in deps.items():
            if e == 'pe' and eng == 'pe':
                continue
            if self.known[eng].get(e, 0) < q:
                E.wait_ge(self.sem[e], q * self._mult(e))
                self.known[eng][e] = q

    def count_ops(self, fn):
        self._counting = True
        self._ccount = 0
        try:
            fn()
        finally:
            self._counting = False
        return self._ccount

    def parallel(self, fns):
        import threading
        counts = [max(1, self.count_ops(f)) for f in fns]
        n = len(fns)
        st = {'turn': 0, 'alive': [True] * n, 'done': [0] * n, 'err': None}
        cv = threading.Condition()
        self._par = (st, cv, counts, threading.local())

        def pick_next():
            best, bf = None, None
            for i in range(n):
                if st['alive'][i]:
                    fr = st['done'][i] / counts[i]
                    if bf is None or fr < bf:
                        best, bf = i, fr
            st['turn'] = best

        def runner(i):
            self._par[3].idx = i
            with cv:
                while st['turn'] != i:
                    cv.wait()
            try:
                fns[i]()
            except BaseException as e:
                st['err'] = e
            finally:
                with cv:
                    st['alive'][i] = False
                    pick_next()
                    cv.notify_all()
        ths = [threading.Thread(target=runner, args=(i,)) for i in range(n)]
        for t in ths:
            t.start()
        for t in ths:
            t.join()
        self._par = None
        if st['err'] is not None:
            raise st['err']

    def _yield_point(self):
        par = getattr(self, '_par', None)
        if par is None:
            return
        st, cv, counts, tls = par
        i = getattr(tls, 'idx', None)
        if i is None:
            return
        st['done'][i] += 1
        if st['done'][i] % 6 == 0:
            with cv:
                best, bf = None, None
                for j in range(len(counts)):
                    if st['alive'][j]:
                        fr = st['done'][j] / counts[j]
                        if bf is None or fr < bf:
                            best, bf = j, fr
                if best != i:
                    st['turn'] = best
                    cv.notify_all()
                    while st['turn'] != i:
                        cv.wait()

    def op(self, eng, fn, reads, writes, inc=True):
        if getattr(self, '_counting', False):
            self._ccount += 1
            return
        self._yield_point()
        if getattr(self, '_par', None) is not None:
            inc = True
        self.n = getattr(self, 'n', 0) + 1
        if self.n > self.limit:
            return
        banks = []
        for tl in list(reads) + list(writes):
            bkk = getattr(tl, 'bank', None) if tl is not None else None
            if bkk is not None and bkk not in banks:
                banks.append(bkk)
        writes = list(writes) + banks
        self._waits(eng, reads, writes)
        ins = fn(self.eng[eng])
        if inc:
            self.cnt[eng] += 1
            ins.then_inc(self.sem[eng], 1)
            q = self.cnt[eng]
        else:
            q = self.cnt[eng] + 1
        for tl in writes:
            if tl is not None:
                tl.lw = (eng, q)
                tl.rd = {}
        for tl in reads:
            if tl is not None:
                tl.rd[eng] = max(tl.rd.get(eng, 0), q)

    def dma(self, q, out, in_, **kw):
        if getattr(self, '_counting', False):
            self._ccount += 1
            return
        self._yield_point()
        self.n = getattr(self, 'n', 0) + 1
        if self.n > self.limit:
            return
        ring = self.rings[q]
        k = ring[self.rpos[q] % len(ring)]
        self.rpos[q] += 1
        E = self.eng[q]
        if self.cnt[k] > 0 and self.known[q].get(k, 0) < self.cnt[k]:
            E.wait_ge(self.sem[k], 16 * self.cnt[k])
            self.known[q][k] = self.cnt[k]
        self._waits(q, [_tl(in_)], [_tl(out)])
        ins = E.dma_start(out=_ap(out), in_=_ap(in_), **kw)
        self.cnt[k] += 1
        ins.then_inc(self.sem[k], 16)
        qn = self.cnt[k]
        if _tl(out) is not None:
            out.tl.lw = (k, qn)
            out.tl.rd = {}
        if _tl(in_) is not None:
            in_.tl.rd[k] = qn

    def barrier(self):
        ce = ['pe', 'act', 'dve', 'pool']
        for e in ce:
            for f in ce:
                if e == f:
                    continue
                if self.known[e].get(f, 0) < self.cnt[f]:
                    self.eng[e].wait_ge(self.sem[f], self.cnt[f])
                    self.known[e][f] = self.cnt[f]
        for f in ce:
            if self.known['sp'].get(f, 0) < self.cnt[f]:
                self.eng['sp'].wait_ge(self.sem[f], self.cnt[f])
                self.known['sp'][f] = self.cnt[f]

    def finish(self):
        E = self.eng['sp']
        print("KB ops emitted:", getattr(self, 'n', 0), {e: self.cnt[e] for e in self.cnt})
        for f in ['pe', 'act', 'dve', 'pool']:
            if self.cnt[f] > 0:
                E.wait_ge(self.sem[f], self.cnt[f])
        for q in self.rings:
            for k in self.rings[q]:
                if self.cnt[k] > 0:
                    E.wait_ge(self.sem[k], 16 * self.cnt[k])

    def mm(self, out, lhsT, rhs, start=True, stop=True, inc=None):
        if inc is None:
            inc = stop
        self.op('pe', lambda e: e.matmul(out.ap, lhsT=lhsT.ap, rhs=rhs.ap, start=start, stop=stop),
                [lhsT.tl, rhs.tl], [out.tl], inc=inc)

    def tr(self, out, in_, ident):
        self.op('pe', lambda e: e.transpose(out.ap, in_.ap, ident.ap), [in_.tl, ident.tl], [out.tl])

    def act(self, out, in_, func, bias=None, scale=1.0, accum=None):
        reads = [in_.tl]
        kw = {}
        if bias is not None:
            kw['bias'] = _ap(bias)
            reads.append(_tl(bias))
        kw['scale'] = _ap(scale)
        reads.append(_tl(scale))
        writes = [out.tl]
        if accum is not None:
            kw['accum_out'] = accum.ap
            writes.append(accum.tl)
        self.op('act', lambda e: e.activation(out=out.ap, in_=in_.ap, func=func, **kw), reads, writes)

    def tt(self, eng, out, a, b, op):
        self.op(eng, lambda e: e.tensor_tensor(out=out.ap, in0=a.ap, in1=b.ap, op=op), [a.tl, b.tl], [out.tl])

    def ts(self, eng, out, a, s1, op0, s2, op1):
        self.op(eng, lambda e: e.tensor_scalar(out=out.ap, in0=a.ap, scalar1=_ap(s1), scalar2=_ap(s2), op0=op0, op1=op1),
                [a.tl, _tl(s1), _tl(s2)], [out.tl])

    def ts1(self, eng, out, a, s1, op):
        self.op(eng, lambda e: e.tensor_single_scalar(out=out.ap, in_=a.ap, scalar=_ap(s1), op=op),
                [a.tl, _tl(s1)], [out.tl])

    def stt(self, eng, out, a, sc, b, op0, op1):
        self.op(eng, lambda e: e.scalar_tensor_tensor(out=out.ap, in0=a.ap, scalar=_ap(sc), in1=b.ap, op0=op0, op1=op1),
                [a.tl, _tl(sc), b.tl], [out.tl])

    def cp(self, eng, out, a):
        if eng == 'act':
            self.op('act', lambda e: e.copy(out=out.ap, in_=a.ap), [a.tl], [out.tl])
        else:
            self.op(eng, lambda e: e.tensor_copy(out=out.ap, in_=a.ap), [a.tl], [out.tl])

    def recip(self, out, a):
        self.op('dve', lambda e: e.reciprocal(out=out.ap, in_=a.ap), [a.tl], [out.tl])

    def red(self, out, a, op=ALU.add):
        self.op('dve', lambda e: e.tensor_reduce(out=out.ap, in_=a.ap, axis=AX.X, op=op), [a.tl], [out.tl])

    def scan(self, out, d0, d1, init, op0, op1):
        self.op('dve', lambda e: e.tensor_tensor_scan(out=out.ap, data0=d0.ap, data1=d1.ap, initial=_ap(init), op0=op0, op1=op1),
                [d0.tl, d1.tl, _tl(init)], [out.tl])

    def memset(self, eng, out, val):
        self.op(eng, lambda e: e.memset(out.ap, val), [], [out.tl])


def host_consts():
    c = {}
    c['identf'] = np.eye(128, dtype=np.float32)
    c['identb'] = np.eye(128, dtype=np.float32).astype(ml_dtypes.bfloat16)
    s = np.arange(128)[:, None]
    t = np.arange(128)[None, :]
    bd = ((s // 64 == t // 64) & (s <= t)).astype(np.float32)
    c['maskbd'] = bd
    c['maskbd8'] = bd * 0.125
    c['maskneg'] = np.where(s <= t, 0.0, -1.0e5).astype(np.float32)
    c['masknb'] = np.where(s <= t, 0.0, -1.0e5).astype(np.float32).astype(ml_dtypes.bfloat16)
    rm = np.ones((128, 512), np.float32)
    rm[:, ::64] = 0.0
    c['resetmask'] = rm
    c['ramp'] = np.tile(np.arange(1, 65, dtype=np.float32)[None, :], (128, 1))
    r = np.arange(128)
    c['rowmask'] = np.stack([((r % 32) // 16 == 0), ((r % 32) // 16 == 1)], 1).astype(np.float32)
    sel = np.zeros((128, 128), np.float32)
    sel[127, :] = 1.0
    c['sel127'] = sel
    es = np.zeros((8, 2, 128), np.float32)
    for pr in range(2):
        for m in range(128):
            es[4 + 2 * pr + m // 64, pr, m] = 1.0
    c['esel'] = es
    c['onesb'] = np.ones((128, 128), np.float32).astype(ml_dtypes.bfloat16)
    return c


CONST_SPECS = [('identf', [128, 128], F32), ('identb', [128, 128], BF16), ('maskbd', [128, 128], F32),
               ('maskbd8', [128, 128], F32), ('maskneg', [128, 128], F32), ('masknb', [128, 128], BF16), ('resetmask', [128, 512], F32),
               ('ramp', [128, 64], F32), ('rowmask', [128, 2], F32), ('sel127', [128, 128], F32),
               ('esel', [8, 2, 128], F32), ('onesb', [128, 128], BF16)]

PARAM_SPECS = [('w_in', [2, 1024, DIN]), ('gate_bias', [2, 12]), ('s5_lambda_re', [2, 16, 64]),
               ('s5_lambda_im', [2, 16, 64]), ('s5_b_re', [2, 16, 64, 16]), ('s5_b_im', [2, 16, 64, 16]),
               ('s5_c_re', [2, 16, 16, 64]), ('s5_c_im', [2, 16, 16, 64]), ('s5_d', [2, 256]),
               ('s5_log_dt', [2, 16]), ('s5_w_glu', [2, 256, 256]), ('hgrn_lb_logits', [2, 256]),
               ('mlstm_conv_w', [2, 4, 512]), ('mix_gain', [2, 1024]), ('w_out', [2, 1024, 1024]),
               ('ln_mix_pre', [2, 1024]), ('ln_mix_post', [2, 1024]), ('ln_ffn_pre', [2, 1024]),
               ('ln_ffn_post', [2, 1024]), ('w_ffn_gate', [2, 1024, DFF]), ('w_ffn_up', [2, 1024, DFF]),
               ('w_ffn_down', [2, DFF, 1024])]


def DV(ap):
    return V(None, ap)


def build(NT, NL=2, dbg=False):
    nc = bass.Bass("TRN2", target_bir_lowering=False)
    SL = NT * T
    NBT = NT * NB
    dr = {}
    dr['x'] = nc.dram_tensor("x", [SL, D], F32, kind="ExternalInput").ap()
    for name, shp in PARAM_SPECS:
        dr[name] = nc.dram_tensor(name, shp, F32, kind="ExternalInput").ap()
    for name, shp, dt in CONST_SPECS:
        dr[name] = nc.dram_tensor("c_" + name, shp, dt, kind="ExternalInput").ap()
    out_d = nc.dram_tensor("out", [SL, D], F32, kind="ExternalOutput").ap()
    hsp = nc.dram_tensor("hspill", [SL, D], F32).ap()
    dbg_d = nc.dram_tensor("dbg", [128, 4096], F32, kind="ExternalOutput").ap() if dbg else None

    with contextlib.ExitStack() as es:
        es.enter_context(nc.allow_non_contiguous_dma(reason="small strided parameter loads"))
        kb = KB(nc, es)
        sb = kb.sb
        cst = {}
        for name, shp, dt in CONST_SPECS:
            cst[name] = sb("k_" + name, shp, dt)
            kb.dma('sp', cst[name].v, DV(dr[name]))
        identf, identb = cst['identf'], cst['identb']

        h = sb("h", [128, NB, D], F32)
        aT = sb("aT", [128, 8, T], BF16)
        NS = 4
        wslab = [sb("wslab%d" % i, [128, 8, 512], BF16) for i in range(NS)]
        gainb = sb("gainb", [128, D], F32)
        gainBCD = sb("gainBCD", [128, 768], F32)
        kTc = sb("kTc", [128, 2, SL], BF16)
        vaug = sb("vaug", [128, NBT, 4, 65], BF16)
        cftok = sb("cftok", [128, NBT, 8], F32)
        biasq = sb("biasq", [128, NBT, 4], F32)
        ctab = sb("ctab", [128, 8, 64], F32)
        stab = sb("stab", [128, 8, 64], F32)
        rhotab = sb("rhotab", [128, 8, 64], F32)
        BBTre = sb("BBTre", [128, 8, 128], BF16)
        BBTim = sb("BBTim", [128, 8, 128], BF16)
        Cre = sb("Cre", [128, 8, 128], BF16)
        Cnim = sb("Cnim", [128, 8, 128], BF16)
        rho = sb("rho", [128, 8], F32)
        d5 = sb("d5", [128, 2], F32)
        gainA = sb("gainA", [128, 2], F32)
        wglu = sb("wglu", [128, 2, 256], BF16)
        S32 = sb("S32", [128, 2, 64], F32)
        C32 = sb("C32", [128, 2, 65], F32)
        xrp = sb("xrp", [128, 8], F32)
        xip = sb("xip", [128, 8], F32)
        cumcar = sb("cumcar", [8, 1], F32)
        Gcar = sb("Gcar", [8, 1], F32)
        gpre_mix = sb("gpre_mix", [128, 8], F32)
        gpre_ffn = sb("gpre_ffn", [128, 8], F32)
        lb = sb("lb", [128, 2], F32)
        oml = sb("oml", [128, 2], F32)
        convw = sb("convw", [128, 4, 4], F32)
        gbias = sb("gbias", [8, 2], F32)
        ngA = sb("ngA", [8, 1], F32)
        wgA = sb("wgA", [128, 8, 8], BF16)
        wgB = sb("wgB", [128, 8, 8], BF16)
        gtok = sb("gtok", [128, NB, 16], F32)
        decb = sb("decb", [128, 2, 8], F32)
        vhat = sb("vhat", [128, NB, 4, 65], BF16)
        qkraw = sb("qkraw", [128, 4, 3 + T], F32)
        junk = sb("junk", [128, D], BF16)
        itile = V(junk, junk.ap0.bitcast(I32))
        ss = sb("ss", [128, 4], F32)
        rt = sb("rt", [128, 4], F32)
        rr = sb("rr", [128, 4], F32)
        sm = [sb("sm%d" % i, [128, 8], F32) for i in range(24)]
        sdec = sb("sdec", [128, 2, 8], F32)
        sinj = sb("sinj", [128, 2, 8], F32)
        ser = sb("ser", [128, 2, 8], F32)

        fa_t = es.enter_context(nc.sbuf_tensor("fa", [128, 10240], F32))
        ba_t = es.enter_context(nc.sbuf_tensor("ba", [128, 15360], BF16))
        F2 = [Tl(fa_t[:, i * 1024:(i + 1) * 1024]) for i in range(4)]
        F1 = [Tl(fa_t[:, 4096 + i * 512:4096 + (i + 1) * 512]) for i in range(12)]
        ffo = Tl(fa_t[:, 0:4096].rearrange("p (b d) -> p b d", d=D))
        silt = Tl(fa_t[:, 4096:4608])
        silt2 = Tl(fa_t[:, 4608:5120])
        yT = Tl(ba_t[:, 0:4096].rearrange("p (k t) -> p k t", t=T))
        ytok = Tl(ba_t[:, 4096:7168].rearrange("p (b c) -> p b c", c=768))
        B2 = [Tl(ba_t[:, 7168 + i * 1024:7168 + (i + 1) * 1024]) for i in range(4)]
        xn = Tl(ba_t[:, 11264:15360].rearrange("p (b d) -> p b d", d=D))
        hid = Tl(ba_t[:, 0:11264].rearrange("p (f t) -> p f t", t=T))
        XA = [Tl(ba_t[:, 11264 + i * 1024:11264 + (i + 1) * 1024]) for i in range(4)]

        pb = [es.enter_context(nc.psum_tensor("pb%d" % i, [128, 512], F32)) for i in range(7)]
        pbfA = es.enter_context(nc.psum_tensor("pbfA", [128, 1024], BF16))
        bigs = [Tl(pb[i][:]) for i in range(3)]
        bigpos = [0]

        def big():
            t = bigs[bigpos[0] % 3]
            bigpos[0] += 1
            return t
        psT = [Tl(pb[3][:, 0:128]), Tl(pb[4][:, 0:128]), Tl(pb[3][:, 128:256]), Tl(pb[4][:, 128:256])]
        po = [Tl(pb[5][:, 0:260]), Tl(pb[6][:, 0:260])]
        pm = [Tl(pb[5][:, 260:390]), Tl(pb[5][:, 390:512]), Tl(pb[6][:, 260:390]), Tl(pb[6][:, 390:512])]
        ptr = [Tl(pbfA[:, 0:512]), Tl(pb[4][:].bitcast(BF16)[:, 0:512])]
        po_f = Tl(pb[0][:, 0:260])
        pc0_f = Tl(pb[0][:, 260:268])
        py_c = Tl(pb[2][:, 0:128])
        psG = [Tl(pb[3][:]), Tl(pb[4][:])]
        psG[0].bank = None
        psG[1].bank = None
        s5pr = Tl(pb[5][:])
        s5pi = Tl(pb[6][:])
        s5pi2 = Tl(pbfA[:].bitcast(F32))
        ptr3 = Tl(pb[3][:].bitcast(BF16)[:, 0:512])
        hpo = Tl(pb[5][:, 0:256])
        hpd = [Tl(pb[5][:, 256:384]), Tl(pb[5][:, 384:512])]
        bk = [Tl(None, "bank%d" % i) for i in range(8)]
        for i in range(3):
            bigs[i].bank = bk[i]
        psT[0].bank = bk[3]
        psT[2].bank = bk[3]
        psT[1].bank = bk[4]
        psT[3].bank = bk[4]
        ptr[1].bank = bk[4]
        for t_ in (po[0], pm[0], pm[1], s5pr):
            t_.bank = bk[5]
        for t_ in (po[1], pm[2], pm[3], s5pi):
            t_.bank = bk[6]
        ptr[0].bank = bk[7]
        s5pi2.bank = bk[7]
        ptr3.bank = bk[3]
        hpo.bank = bk[5]
        hpd[0].bank = bk[5]
        hpd[1].bank = bk[5]
        psG[0].bank = bk[3]
        psG[1].bank = bk[4]
        po_f.bank = bk[0]
        pc0_f.bank = bk[0]
        py_c.bank = bk[2]

        plan = []
        for l in range(NL):
            for ti in range(NT):
                W = dr['w_in'][l]
                for (c0, c1) in [(0, 512), (512, 1024), (1024, 1536), (1536, 2048), (2052, 2564), (2564, 3076)]:
                    plan.append((W[:, c0:c1], 8, c1 - c0))
                for dg in range(2):
                    plan.append((dr['w_out'][l][:, dg * 512:(dg + 1) * 512], 8, 512))
                for fg in range(6):
                    c0 = fg * 512
                    ncl = min(512, DFF - c0)
                    plan.append((dr['w_ffn_gate'][l][:, c0:c0 + ncl], 8, ncl))
                    plan.append((dr['w_ffn_up'][l][:, c0:c0 + ncl], 8, ncl))
                for dg in range(2):
                    for (f0, nf) in ((0, 8), (8, 8), (16, 6)):
                        plan.append((dr['w_ffn_down'][l][f0 * 128:(f0 + nf) * 128, dg * 512:(dg + 1) * 512], nf, 512))
        sstate = {'ptr': 0, 'issued': 0}

        def slab_issue(i):
            ap, nk, ncl = plan[i]
            sl = wslab[i % NS]
            kb.dma('pool', sl[:, 0:nk, 0:ncl], DV(ap.rearrange("(kc p) c -> p kc c", p=128)))

        def get_slab():
            idx = sstate['ptr']
            lim = min(len(plan), idx + 2)
            while sstate['issued'] < lim:
                slab_issue(sstate['issued'])
                sstate['issued'] += 1
            sstate['ptr'] += 1
            return wslab[idx % NS]

        def proj_fm(outv, slab, c0, ncl, t0=0, nt=T):
            for kc in range(8):
                kb.mm(outv, slab[:, kc, c0:c0 + ncl], aT[:, kc, t0:t0 + nt], start=(kc == 0), stop=(kc == 7))

        def proj_tm(outv, slab, c0, ncl, blk):
            for kc in range(8):
                kb.mm(outv, aT[:, kc, blk * 128:(blk + 1) * 128], slab[:, kc, c0:c0 + ncl], start=(kc == 0), stop=(kc == 7))

        def sincos(src, osin, ocos, N):
            a = F1[8][:, 0:N]
            b = F1[9][:, 0:N]
            ii = itile[:, 0:N]
            for (shift, outv) in ((0.0, osin), (math.pi / 2, ocos)):
                kb.ts('dve', a, src, shift, ALU.add, 1.0 / TWO_PI, ALU.mult)
                kb.cp('dve', ii, a)
                kb.cp('dve', b, ii)
                kb.stt('dve', a, b, -TWO_PI, src, ALU.mult, ALU.add)
                kb.ts('dve', a, a, shift, ALU.add, 3.1415925, ALU.min)
                kb.ts1('dve', a, a, -3.1415925, ALU.max)
                kb.act(outv, a, AF.Sin)

        def colsplit(ap_1d):
            return DV(ap_1d.rearrange("(kc p) -> p kc", p=128))

        def load_params(l):
            kb.dma('sp', gpre_mix.v, colsplit(dr['ln_mix_pre'][l]))
            kb.dma('sp', gpre_ffn.v, colsplit(dr['ln_ffn_pre'][l]))
            kb.dma('sp', d5.v, colsplit(dr['s5_d'][l]))
            kb.dma('sp', gainA.v, colsplit(dr['mix_gain'][l][0:256]))
            kb.dma('sp', gainBCD.v, DV(dr['mix_gain'][l][256:1024].partition_broadcast(128)))
            kb.dma('pool', wglu.v, DV(dr['s5_w_glu'][l].rearrange("(kc p) c -> p kc c", p=128)))
            for ctt in range(4):
                kb.dma('sp', convw[:, ctt, :], DV(dr['mlstm_conv_w'][l][:, ctt * 128:(ctt + 1) * 128].rearrange("j p -> p j")))
            gb = dr['gate_bias'][l]

            def col(a):
                return DV(a.rearrange("(p o) -> p o", o=1))
            kb.dma('sp', gbias[0:4, 0:1], col(gb[0:4]))
            kb.dma('sp', gbias[4:8, 0:1], col(gb[8:12]))
            kb.dma('sp', gbias[0:4, 1:2], col(gb[0:4]))
            kb.dma('sp', gbias[4:8, 1:2], col(gb[4:8]))
            kb.ts1('dve', ngA.v, gbias[:, 0:1], -1.0, ALU.mult)
            W = dr['w_in'][l]

            def gcols(c0):
                return DV(W[:, c0:c0 + 4].rearrange("(kc p) c -> p kc c", p=128))
            kb.dma('pool', wgA[:, :, 0:4], gcols(2048))
            kb.dma('pool', wgA[:, :, 4:8], gcols(3080))
            kb.dma('pool', wgB[:, :, 0:4], gcols(2048))
            kb.dma('pool', wgB[:, :, 4:8], gcols(3076))
            if l == 0:
                kb.memset('dve', lb.v, 0.0)
                kb.memset('dve', oml.v, 1.0)
            else:
                x0, x1 = sm[20], sm[21]
                kb.dma('sp', x0[:, 0:2], colsplit(dr['hgrn_lb_logits'][0]))
                kb.dma('sp', x1[:, 0:2], colsplit(dr['hgrn_lb_logits'][1]))
                kb.tt('dve', x0[:, 0:2], x0[:, 0:2], x1[:, 0:2], ALU.subtract)
                kb.act(x0[:, 0:2], x0[:, 0:2], AF.Exp)
                kb.ts1('dve', x0[:, 0:2], x0[:, 0:2], 1.0, ALU.add)
                kb.recip(lb.v, x0[:, 0:2])
                kb.ts('dve', oml.v, lb.v, -1.0, ALU.mult, 1.0, ALU.add)
            kb.memset('dve', S32.v, 0.0)
            kb.memset('dve', C32.v, 0.0)
            kb.memset('dve', xrp.v, 0.0)
            kb.memset('dve', xip.v, 0.0)
            kb.memset('dve', cumcar.v, 0.0)
            kb.memset('dve', Gcar.v, 0.0)
            kb.memset('dve', qkraw[:, :, 0:3], 0.0)
            if l == 0:
                kb.memset('dve', vaug[:, :, :, 64:65], 1.0)
            s5_prep(l)

        def s5_prep(l):
            ldt, lre, lim, dtt, lr, mag, th, sn, cs = sm[0:9]
            abre, abim, den, rden, am1, cfre, cfim, t0, t1 = sm[9:18]
            for half in range(2):
                rows = slice(half * 64, (half + 1) * 64)
                kb.dma('sp', ldt[rows, :], DV(dr['s5_log_dt'][l].rearrange("(j h) -> h j", h=2)[half].partition_broadcast(64)))
                kb.dma('sp', lre[rows, :], DV(dr['s5_lambda_re'][l].rearrange("(j h) n -> h n j", h=2)[half]))
                kb.dma('sp', lim[rows, :], DV(dr['s5_lambda_im'][l].rearrange("(j h) n -> h n j", h=2)[half]))
            bre = V(F1[0], F1[0].ap0[:, 0:128].rearrange("p (j q) -> p j q", q=16))
            bim = V(F1[1], F1[1].ap0[:, 0:128].rearrange("p (j q) -> p j q", q=16))
            bbre = V(F1[2], F1[2].ap0[:, 0:128].rearrange("p (j q) -> p j q", q=16))
            bbim = V(F1[3], F1[3].ap0[:, 0:128].rearrange("p (j q) -> p j q", q=16))
            tmpb = V(F1[4], F1[4].ap0[:, 0:128].rearrange("p (j q) -> p j q", q=16))
            for half in range(2):
                rows = slice(half * 64, (half + 1) * 64)
                kb.dma('sp', bre[rows, :, :], DV(dr['s5_b_re'][l].rearrange("(j h) n q -> h n j q", h=2)[half]))
                kb.dma('sp', bim[rows, :, :], DV(dr['s5_b_im'][l].rearrange("(j h) n q -> h n j q", h=2)[half]))
            kb.act(dtt.v, ldt.v, AF.Exp)
            kb.ts1('dve', lr.v, lre.v, -1e-4, ALU.min)
            kb.tt('dve', t0.v, lr.v, dtt.v, ALU.mult)
            kb.act(mag.v, t0.v, AF.Exp)
            kb.tt('dve', th.v, lim.v, dtt.v, ALU.mult)
            sincos(th.v, sn.v, cs.v, 8)
            kb.tt('dve', abre.v, mag.v, cs.v, ALU.mult)
            kb.tt('dve', abim.v, mag.v, sn.v, ALU.mult)
            kb.tt('dve', t0.v, lr.v, lr.v, ALU.mult)
            kb.tt('dve', t1.v, lim.v, lim.v, ALU.mult)
            kb.tt('dve', den.v, t0.v, t1.v, ALU.add)
            kb.recip(rden.v, den.v)
            kb.ts1('dve', am1.v, abre.v, -1.0, ALU.add)
            kb.tt('dve', t0.v, am1.v, lr.v, ALU.mult)
            kb.tt('dve', t1.v, abim.v, lim.v, ALU.mult)
            kb.tt('dve', t0.v, t0.v, t1.v, ALU.add)
            kb.tt('dve', cfre.v, t0.v, rden.v, ALU.mult)
            kb.tt('dve', t0.v, abim.v, lr.v, ALU.mult)
            kb.tt('dve', t1.v, am1.v, lim.v, ALU.mult)
            kb.tt('dve', t0.v, t0.v, t1.v, ALU.subtract)
            kb.tt('dve', cfim.v, t0.v, rden.v, ALU.mult)
            cfre_b = cfre.v.re("p (j o) -> p j o", o=1).bc([128, 8, 16])
            cfim_b = cfim.v.re("p (j o) -> p j o", o=1).bc([128, 8, 16])
            kb.tt('dve', bbre.v, cfre_b, bre.v, ALU.mult)
            kb.tt('dve', tmpb.v, cfim_b, bim.v, ALU.mult)
            kb.tt('dve', bbre.v, bbre.v, tmpb.v, ALU.subtract)
            kb.tt('dve', bbim.v, cfre_b, bim.v, ALU.mult)
            kb.tt('dve', tmpb.v, cfim_b, bre.v, ALU.mult)
            kb.tt('dve', bbim.v, bbim.v, tmpb.v, ALU.add)
            Xf = V(B2[0], B2[0].ap0.rearrange("p (j c) -> p j c", c=128))
            for (bb, BBT) in ((bbre, BBTre), (bbim, BBTim)):
                kb.memset('dve', Xf.v, 0.0)
                Xf4 = Xf.v.re("p (a b) c -> p a b c", b=4)
                bb4 = bb.v.re("p (a b) q -> p a b q", b=4)
                for j4 in range(4):
                    for half in range(2):
                        rows = slice(half * 64, (half + 1) * 64)
                        c0 = 32 * j4 + 16 * half
                        kb.cp('dve', Xf4[rows, :, j4, c0:c0 + 16], bb4[rows, :, j4, :])
                for g in range(2):
                    for k4 in range(4):
                        kb.tr(ptr[g][:, k4 * 128:(k4 + 1) * 128], Xf[:, g * 4 + k4, :], identb.v)
                    kb.cp('dve', BBT[:, g * 4:(g + 1) * 4, :], ptr[g].v.re("p (k t) -> p k t", t=128))
            ph = F1[10]
            th_b = th.v.re("p (j o) -> p j o", o=1).bc([128, 8, 64])
            ramp_b = cst['ramp'].v.re("p (o t) -> p o t", o=1).bc([128, 8, 64])
            kb.tt('dve', ph.v.re("p (j t) -> p j t", t=64), th_b, ramp_b, ALU.mult)
            sincos(ph.v, stab.v.re("p j t -> p (j t)"), ctab.v.re("p j t -> p (j t)"), 512)
            kb.cp('dve', rhotab.v, mag.v.re("p (j o) -> p j o", o=1).bc([128, 8, 64]))
            kb.memset('dve', rhotab[:, :, 0:1], 0.0)
            kb.cp('dve', rho.v, mag.v)
            cstt = V(F1[5], F1[5].ap0[:, 0:128].rearrange("p (t n) -> p t n", n=64))
            cst2 = V(B2[1], B2[1].ap0[:, 0:256].rearrange("p (t n) -> p t n", n=128))
            for (cname, Cm, sign) in (('s5_c_re', Cre, 1.0), ('s5_c_im', Cnim, -1.0)):
                kb.dma('sp', cstt.v, DV(dr[cname][l].rearrange("(t g) p n -> (g p) t n", t=2)))
                for half in range(2):
                    kb.ts('dve', cst2[:, :, half * 64:(half + 1) * 64], cstt.v, cst['rowmask'][:, half:half + 1], ALU.mult, sign, ALU.mult)
                kb.memset('dve', Cm.v, 0.0)
                for ti in range(2):
                    kb.tr(ptr[1][:, ti * 128:(ti + 1) * 128], cst2[:, ti, :], identb.v)
                for j in range(8):
                    c0 = 32 * (j % 4)
                    kb.cp('dve', Cm[:, j, c0:c0 + 32], ptr[1][:, (j // 4) * 128 + c0:(j // 4) * 128 + c0 + 32])

        PTt = [sb("PT%d" % i, [128, 128], BF16) for i in range(8)]
        Sbt = [sb("Sb%d" % i, [128, 64], BF16) for i in range(4)]
        Cbt = [sb("Cb%d" % i, [128, 65], BF16) for i in range(4)]

        def c3(v):
            return v.re("p (c t) -> p c t", t=64)

        def k3(v):
            return v.re("p (k t) -> p k t", t=T)

        def norm_to_aT(gpre):
            kb.memset('dve', ss.v, 0.0)
            for b in range(NB):
                kb.act(junk.v, h[:, b, :], AF.Square, accum=ss[:, b:b + 1])
            kb.act(rt.v, ss.v, AF.Sqrt, bias=EPS, scale=1.0 / D)
            kb.recip(rr.v, rt.v)
            for b in range(NB):
                kb.ts1('dve', xn[:, b, :], h[:, b, :], rr[:, b:b + 1], ALU.mult)
                for half in range(2):
                    pt = ptr[half]
                    for k4 in range(4):
                        kc = half * 4 + k4
                        kb.tr(pt[:, k4 * 128:(k4 + 1) * 128], xn[:, b, kc * 128:(kc + 1) * 128], identb.v)
                    kb.tt('dve', aT[:, half * 4:(half + 1) * 4, b * 128:(b + 1) * 128],
                          pt.v.re("p (k t) -> p k t", t=128),
                          gpre[:, half * 4:(half + 1) * 4].re("p (k o) -> p k o", o=1).bc([128, 4, 128]), ALU.mult)

        def gates(ti):
            pgA = big()
            pgB = big()
            for (pg, wg) in ((pgA, wgA), (pgB, wgB)):
                for kc in range(8):
                    kb.mm(pg[0:8, :], wg[:, kc, :], aT[:, kc, :], start=(kc == 0), stop=(kc == 7))
            eA, l1, cumA, gS, G, e2, clv, tmp = [F1[i][0:8, :] for i in range(8)]
            kb.act(eA, pgA[0:8, :], AF.Exp, bias=ngA.v, scale=-1.0)
            kb.act(l1, eA, AF.Ln, bias=1.0)
            ones8 = F1[9][0:8, :]
            kb.memset('dve', ones8, 1.0)
            kb.scan(cumA, ones8, l1, cumcar.v, ALU.mult, ALU.subtract)
            kb.cp('dve', cumcar.v, cumA[:, 511:512])
            for b in range(NB):
                kb.tr(pm[2][:, b * 8:(b + 1) * 8], cumA[:, b * 128:(b + 1) * 128], identf[0:8, 0:8])
            kb.cp('dve', cftok[:, ti * NB:(ti + 1) * NB, :], pm[2][:, 0:32].re("p (b g) -> p b g", g=8))
            kb.stt('dve', gS, pgB[0:8, :], gbias[:, 1:2], cumA, ALU.add, ALU.subtract)
            negb8 = F1[10][0:8, :]
            kb.memset('dve', negb8, -1.0e30)
            kb.scan(G, negb8, gS, Gcar.v, ALU.max, ALU.max)
            Gend_b = c3(G)[:, :, 63:64].bc([8, 8, 64])
            kb.tt('dve', c3(tmp), c3(gS), Gend_b, ALU.subtract)
            kb.act(e2, tmp, AF.Exp)
            kb.tt('dve', c3(tmp), c3(cumA), Gend_b, ALU.add)
            kb.act(clv, tmp, AF.Exp, scale=-1.0)
            Gpv = sm[22][0:8, 0:8]
            dd = sm[23][0:8, 0:8]
            dec = sm[19][0:8, 0:8]
            kb.cp('dve', Gpv[:, 0:1], Gcar.v)
            kb.cp('dve', Gpv[:, 1:8], c3(G)[:, 0:7, 63])
            kb.tt('dve', dd, Gpv, c3(G)[:, :, 63], ALU.subtract)
            kb.act(dec, dd, AF.Exp)
            kb.cp('dve', Gcar.v, c3(G)[:, 7, 63:64])
            for b in range(NB):
                kb.tr(pm[3][:, b * 16:b * 16 + 8], e2[:, b * 128:(b + 1) * 128], identf[0:8, 0:8])
                kb.tr(pm[3][:, b * 16 + 8:b * 16 + 16], clv[:, b * 128:(b + 1) * 128], identf[0:8, 0:8])
            kb.cp('dve', gtok.v, pm[3][:, 0:64].re("p (b g) -> p b g", g=16))
            for pr in range(2):
                kb.mm(pm[1][:, pr * 8:(pr + 1) * 8], cst['esel'][:, pr, :], dec)
            kb.cp('dve', decb.v, pm[1][:, 0:16].re("p (a c) -> p a c", c=8))

        def finish_tm(blk, num3, d2eps, gain2d, gate2d, c0, scr=None):
            if scr is None:
                scr = (F1[10], F1[11], sm[16], sm[17], ptr[1])
            sq = scr[0][:, 0:256].re("p (h v) -> p h v", v=64)
            s4 = scr[2][:, 0:4]
            r4 = scr[3][:, 0:4]
            y2d = scr[1][:, 0:256]
            ptrx = scr[4]
            y3d = y2d.re("p (h v) -> p h v", v=64)
            kb.tt('dve', sq, num3, num3, ALU.mult)
            kb.red(s4, sq)
            if d2eps is None:
                kb.act(r4, s4, AF.Sqrt, bias=EPS, scale=1.0 / 64)
            else:
                kb.stt('dve', s4, s4, 1.0 / 64, d2eps, ALU.mult, ALU.add)
                kb.act(r4, s4, AF.Sqrt)
            kb.recip(r4, r4)
            kb.tt('dve', y3d, num3, r4.re("p (h o) -> p h o", o=1).bc([128, 4, 64]), ALU.mult)
            if gate2d is None:
                kb.tt('dve', ytok[:, blk, c0:c0 + 256], y2d, gain2d, ALU.mult)
            else:
                kb.tt('dve', y2d, y2d, gain2d, ALU.mult)
                kb.tt('dve', ytok[:, blk, c0:c0 + 256], y2d, gate2d, ALU.mult)
            kc0 = 2 + c0 // 128
            for k in range(2):
                kb.tr(ptrx[:, k * 128:(k + 1) * 128], ytok[:, blk, c0 + k * 128:c0 + (k + 1) * 128], identb.v)
            kb.cp('act', yT[:, kc0:kc0 + 2, blk * 128:(blk + 1) * 128], ptrx[:, 0:256].re("p (k t) -> p k t", t=128))

        def s5_proj(slabA):
            uT = k3(F2[0].v)
            uTb = k3(XA[0].v)
            for kc in range(2):
                p = big()
                proj_fm(p.v, slabA, kc * 128, 128)
                kb.cp('act', uT[:, kc, :], p.v)
                kb.cp('dve', uTb[:, kc, :], p.v)

        def s5_main():
            uT = k3(F2[0].v)
            uTb = k3(XA[0].v)
            yv = k3(F2[1].v)
            prpi = [(s5pr, s5pi), (bigs[1], s5pi2)]
            xbs = [B2[1].v, XA[2].v]
            t1, t2, t3, t4, bmr, bmi, zr, zi = [F1[i].v for i in range(8)]
            ct = ctab.v.re("p j t -> p (j t)")
            st = stab.v.re("p j t -> p (j t)")
            rtb = rhotab.v.re("p j t -> p (j t)")

            def emit_bu(c):
                pr_, pi_ = prpi[c % 2]
                tok = slice(c * 64, (c + 1) * 64)
                for j in range(8):
                    kb.mm(c3(pr_.v)[:, j, :], BBTre[:, j, :], uTb[:, j // 4, tok], inc=False)
                    kb.mm(c3(pi_.v)[:, j, :], BBTim[:, j, :], uTb[:, j // 4, tok], inc=(j == 7))

            def emit_y(c):
                tok = slice(c * 64, (c + 1) * 64)
                xb = xbs[c % 2]
                xrb = c3(xb[:, 0:512])
                xib = c3(xb[:, 512:1024])
                for t2i in range(2):
                    for j4 in range(4):
                        j = t2i * 4 + j4
                        kb.mm(py_c[:, t2i * 64:(t2i + 1) * 64], Cre[:, j, :], xrb[:, j, :], start=(j4 == 0), stop=False, inc=False)
                        kb.mm(py_c[:, t2i * 64:(t2i + 1) * 64], Cnim[:, j, :], xib[:, j, :], start=False, stop=(j4 == 3), inc=(j4 == 3))
                for t2i in range(2):
                    kb.stt('dve', yv[:, t2i, tok], uT[:, t2i, tok], d5[:, t2i:t2i + 1], py_c[:, t2i * 64:(t2i + 1) * 64], ALU.mult, ALU.add)

            emit_bu(0)
            for c in range(8):
                if c + 1 < 8:
                    emit_bu(c + 1)
                pr_, pi_ = prpi[c % 2]
                xb = xbs[c % 2]
                kb.tt('dve', t1, ct, pr_.v, ALU.mult)
                kb.tt('dve', t2, st, pi_.v, ALU.mult)
                kb.tt('dve', bmr, t1, t2, ALU.add)
                kb.tt('dve', t3, ct, pi_.v, ALU.mult)
                kb.tt('dve', t4, st, pr_.v, ALU.mult)
                kb.tt('dve', bmi, t3, t4, ALU.subtract)
                kb.tt('dve', sm[18].v, rho.v, xrp.v, ALU.mult)
                kb.tt('dve', c3(bmr)[:, :, 0], c3(bmr)[:, :, 0], sm[18].v, ALU.add)
                kb.tt('dve', sm[13].v, rho.v, xip.v, ALU.mult)
                kb.tt('dve', c3(bmi)[:, :, 0], c3(bmi)[:, :, 0], sm[13].v, ALU.add)
                kb.scan(zr, rtb, bmr, 0.0, ALU.mult, ALU.add)
                kb.scan(zi, rtb, bmi, 0.0, ALU.mult, ALU.add)
                kb.tt('dve', t1, ct, zr, ALU.mult)
                kb.tt('dve', t2, st, zi, ALU.mult)
                kb.tt('dve', xb[:, 0:512], t1, t2, ALU.subtract)
                kb.tt('dve', t3, st, zr, ALU.mult)
                kb.tt('dve', t4, ct, zi, ALU.mult)
                kb.tt('dve', xb[:, 512:1024], t3, t4, ALU.add)
                kb.tt('dve', xrp.v, c3(t1)[:, :, 63], c3(t2)[:, :, 63], ALU.subtract)
                kb.tt('dve', xip.v, c3(t3)[:, :, 63], c3(t4)[:, :, 63], ALU.add)
                if c >= 1:
                    emit_y(c - 1)
            emit_y(7)
            gel = k3(F2[2].v)
            y3 = k3(F2[3].v)
            gelb = k3(B2[2].v)
            sq = k3(B2[3].v)
            for kc in range(2):
                y = yv[:, kc, :]
                a = F1[0].v
                b = F1[1].v
                kb.tt('dve', a, y, y, ALU.mult)
                kb.ts('dve', a, a, 0.044715, ALU.mult, 1.0, ALU.add)
                kb.tt('dve', a, a, y, ALU.mult)
                kb.act(b, a, AF.Sigmoid, scale=1.5957691216057308)
                kb.tt('dve', gel[:, kc, :], y, b, ALU.mult)
                kb.cp('dve', gelb[:, kc, :], gel[:, kc, :])
            for co in range(2):
                p = bigs[1]
                for kc in range(2):
                    kb.mm(p.v, wglu[:, kc, co * 128:(co + 1) * 128], gelb[:, kc, :], start=(kc == 0), stop=(kc == 1))
                b = F1[1].v
                kb.act(b, p.v, AF.Sigmoid)
                kb.tt('dve', y3[:, co, :], gel[:, co, :], b, ALU.mult)
                kb.act(sq[:, co, :], y3[:, co, :], AF.Square)
            pn = bigs[1]
            for kc in range(2):
                kb.mm(pn.v, cst['onesb'].v, sq[:, kc, :], start=(kc == 0), stop=(kc == 1))
            r = F1[2].v
            kb.act(r, pn.v, AF.Sqrt, bias=EPS, scale=1.0 / 256)
            kb.recip(r, r)
            for kc in range(2):
                kb.stt('dve', yT[:, kc, :], y3[:, kc, :], gainA[:, kc:kc + 1], r, ALU.mult, ALU.mult)

        def hgrn_prep(slabA, slabB, slabC, ti):
            qhT = k3(B2[0].v)
            khT = k3(B2[1].v)
            for pr in range(2):
                pq = big()
                proj_fm(pq.v, slabA, 256 + pr * 128, 128)
                pz = big()
                proj_fm(pz.v, slabB, pr * 128, 128)
                e, A, t1_, f, kk, b, d1, E1 = [F1[i].v for i in range(8)]
                kb.act(e, pz.v, AF.Exp, scale=-1.0)
                kb.ts1('dve', A, e, 1.0, ALU.add)
                kb.recip(A, A)
                kb.ts1('dve', t1_, e, E60, ALU.min)
                kb.ts1('dve', t1_, t1_, lb[:, pr:pr + 1], ALU.mult)
                kb.stt('dve', f, t1_, 1.0, A, ALU.add, ALU.mult)
                kb.act(f, f, AF.Ln)
                kb.stt('dve', kk, e, oml[:, pr:pr + 1], A, ALU.mult, ALU.mult)
                kb.scan(b, cst['resetmask'].v, f, 0.0, ALU.mult, ALU.add)
                b3 = c3(b)
                kb.tt('dve', c3(d1), b3, b3[:, :, 31:32].bc([128, 8, 64]), ALU.subtract)
                kb.act(E1, d1, AF.Exp)
                kb.tt('dve', qhT[:, pr, :], pq.v, E1, ALU.mult)
                kb.act(E1, d1, AF.Exp, scale=-1.0)
                kb.tt('dve', khT[:, pr, :], kk, E1, ALU.mult)
                kb.act(sdec[:, pr, :], b3[:, :, 63], AF.Exp)
                kb.tt('dve', sm[18].v, b3[:, :, 63], b3[:, :, 31], ALU.subtract)
                kb.act(sinj[:, pr, :], sm[18].v, AF.Exp)
                kb.act(ser[:, pr, :], b3[:, :, 31], AF.Exp)
            vb = B2[2].v.re("p (b c) -> p b c", c=256)
            sgate = F2[1].v.re("p (b c) -> p b c", c=256)
            for blk in range(NB):
                p = big()
                proj_tm(p[:, 0:256], slabB, 256, 256, blk)
                kb.cp('act', vb[:, blk, :], p[:, 0:256])
                p = big()
                proj_tm(p[:, 0:256], slabC, 0, 256, blk)
                kb.act(sgate[:, blk, :], p[:, 0:256], AF.Silu)
            khtok = B2[3].v.re("p (b r c) -> p b r c", r=2, c=128)
            for blk in range(NB):
                for pr in range(2):
                    kb.tr(ptr[0][:, pr * 128:(pr + 1) * 128], khT[:, pr, blk * 128:(blk + 1) * 128], identb.v)
                kb.cp('dve', khtok[:, blk, :, :], ptr[0][:, 0:256].re("p (r c) -> p r c", c=128))

        def hgrn_attn(ti):
            qhT = k3(B2[0].v)
            khT = k3(B2[1].v)
            vb = B2[2].v.re("p (b c) -> p b c", c=256)
            sgate = F2[1].v.re("p (b c) -> p b c", c=256)
            khtok = B2[3].v.re("p (b r c) -> p b r c", r=2, c=128)
            hpsT = [psT[0], psT[2]]
            for blk in range(NB):
                pob = hpo
                tk = slice(blk * 128, (blk + 1) * 128)
                for cc in range(2):
                    c = 2 * blk + cc
                    rows = slice(cc * 64, (cc + 1) * 64)
                    for pr in range(2):
                        kb.ts1('dve', Sbt[cc * 2 + pr].v, S32[:, pr, :], ser[:, pr, c:c + 1], ALU.mult)
                        pd = hpd[pr]
                        kb.mm(pd[:, 0:128], khtok[rows, blk, pr, :], vb[rows, blk, pr * 128:(pr + 1) * 128])
                        for half in range(2):
                            hs = slice(half * 64, (half + 1) * 64)
                            kb.ts1('dve', S32[hs, pr, :], S32[hs, pr, :], sdec[hs, pr, c:c + 1], ALU.mult)
                            kb.stt('dve', S32[hs, pr, :], pd[hs, half * 64:(half + 1) * 64], sinj[hs, pr, c:c + 1],
                                   S32[hs, pr, :], ALU.mult, ALU.add)
                for hh in range(4):
                    pr, half = hh // 2, hh % 2
                    hs = slice(half * 64, (half + 1) * 64)
                    ps = hpsT[hh % 2]
                    PT = PTt[hh]
                    kb.mm(ps.v, khT[hs, pr, tk], qhT[hs, pr, tk])
                    kb.tt('dve', PT.v, ps.v, cst['maskbd'].v, ALU.mult)
                    oc = slice(hh * 64, (hh + 1) * 64)
                    kb.mm(pob[:, oc], PT.v, vb[:, blk, oc], start=True, stop=False, inc=False)
                    kb.mm(pob[0:64, oc], qhT[hs, pr, blk * 128:blk * 128 + 64], Sbt[0 + pr][hs, :], start=False, stop=True, inc=False)
                    kb.mm(pob[64:128, oc], qhT[hs, pr, blk * 128 + 64:(blk + 1) * 128], Sbt[2 + pr][hs, :], start=False, stop=True)
                ob = F1[9][:, 0:256]
                kb.cp('act', ob, pob[:, 0:256])
                finish_tm(blk, ob.re("p (h v) -> p h v", v=64), None, gainBCD[:, 0:256], sgate[:, blk, :], 0,
                          scr=(F1[10], F1[11], sm[16], sm[17], ptr3))

        def fox_proj(slabC, slabD, ti):
            qT = k3(XA[1].v)
            for pr in range(2):
                p = big()
                proj_fm(p.v, slabC, 256 + pr * 128, 128)
                kb.cp('act', qT[:, pr, :], p.v)
                p = big()
                proj_fm(p.v, slabD, pr * 128, 128)
                kb.cp('act', kTc[:, pr, ti * T:(ti + 1) * T], p.v)
            for blk in range(NB):
                p = big()
                proj_tm(p[:, 0:256], slabD, 256, 256, blk)
                kb.cp('dve', vaug[:, ti * NB + blk, :, 0:64], p[:, 0:256].re("p (h v) -> p h v", v=64))

        def fox_attn(ti):
            qT = k3(XA[1].v)
            for qb in range(NB):
                gq = ti * NB + qb
                nk = gq + 1
                kb.mm(pc0_f.v, cst['sel127'].v, cftok[:, gq, :])
                kb.tt('dve', biasq[:, 0:nk, :], pc0_f[:, 0:4].re("p (o h) -> p o h", o=1).bc([128, nk, 4]),
                      cftok[:, 0:nk, 0:4], ALU.subtract)
                pob = po_f
                ngrp = (nk + 3) // 4
                glist = [(hh, g) for hh in range(4) for g in range(ngrp)]

                def scores(idx):
                    hh, g = glist[idx]
                    pr, half = hh // 2, hh % 2
                    hs = slice(half * 64, (half + 1) * 64)
                    qv = qT[hs, pr, qb * 128:(qb + 1) * 128]
                    for i in range(g * 4, min(nk, g * 4 + 4)):
                        pso = psG[idx % 2][:, (i % 4) * 128:(i % 4 + 1) * 128]
                        if i == gq:
                            kb.mm(pso, kTc[hs, pr, i * 128:(i + 1) * 128], qv, start=True, stop=False, inc=False)
                            kb.mm(pso, identb.v, cst['masknb'].v, start=False, stop=True)
                        else:
                            kb.mm(pso, kTc[hs, pr, i * 128:(i + 1) * 128], qv)
                scores(0)
                for idx in range(len(glist)):
                    if idx + 1 < len(glist):
                        scores(idx + 1)
                    hh, g = glist[idx]
                    blks = list(range(g * 4, min(nk, g * 4 + 4)))
                    for kbi in blks:
                        ps = psG[idx % 2][:, (kbi % 4) * 128:(kbi % 4 + 1) * 128]
                        PT = PTt[(idx % 2) * 4 + kbi % 4]
                        kb.act(PT.v, ps, AF.Exp, bias=biasq[:, kbi, hh:hh + 1], scale=0.125)
                    for kbi in blks:
                        PT = PTt[(idx % 2) * 4 + kbi % 4]
                        kb.mm(pob[:, hh * 65:(hh + 1) * 65], PT.v, vaug[:, kbi, hh, :], start=(kbi == 0), stop=(kbi == nk - 1), inc=True)
                ob = F1[9][:, 0:260]
                kb.cp('act', ob, pob[:, 0:260])
                ob3 = ob.re("p (h v) -> p h v", v=65)
                d2 = sm[15][:, 0:4]
                kb.tt('dve', d2, ob3[:, :, 64], ob3[:, :, 64], ALU.mult)
                kb.ts1('dve', d2, d2, EPS, ALU.mult)
                finish_tm(qb, ob3[:, :, 0:64], d2, gainBCD[:, 256:512], None, 256)

        def mlstm_prep(slabE, slabF, ti):
            for ctt in range(4):
                p = big()
                proj_fm(p.v, slabE, ctt * 128, 128)
                kb.cp('act', qkraw[:, ctt, 3:3 + T], p.v)
            qkc = [k3(XA[2].v), k3(XA[3].v)]
            for ctt in range(4):
                acc = F1[ctt].v
                kb.ts1('dve', acc, qkraw[:, ctt, 3:3 + T], convw[:, ctt, 3:4], ALU.mult)
                for j in range(3):
                    kb.stt('dve', acc, qkraw[:, ctt, j:j + T], convw[:, ctt, j:j + 1], acc, ALU.mult, ALU.add)
                kb.cp('pool', qkraw[:, ctt, 0:3], qkraw[:, ctt, T:T + 3])
                kb.act(qkc[ctt // 2][:, ctt % 2, :], acc, AF.Silu)
            sig_o = F2[2].v.re("p (b c) -> p b c", c=256)
            for blk in range(NB):
                p = big()
                proj_tm(p.v, slabF, 0, 512, blk)
                e2h = gtok[:, blk, 4:8]
                kb.tt('dve', vhat[:, blk, :, 0:64], p[:, 0:256].re("p (h v) -> p h v", v=64),
                      e2h.re("p (h o) -> p h o", o=1).bc([128, 4, 64]), ALU.mult)
                kb.cp('dve', vhat[:, blk, :, 64], e2h)
                kb.act(sig_o[:, blk, :], p[:, 256:512], AF.Sigmoid)
            ktok = junk.v.re("p (b r c) -> p b r c", r=2, c=128)
            for blk in range(NB):
                for pr in range(2):
                    kb.tr(ptr[0][:, pr * 128:(pr + 1) * 128], qkc[1][:, pr, blk * 128:(blk + 1) * 128], identb.v)
                kb.ts1('dve', ktok[:, blk, :, :], ptr[0][:, 0:256].re("p (r c) -> p r c", c=128), 0.125, ALU.mult)

        def mlstm_attn(ti):
            qkc = [k3(XA[2].v), k3(XA[3].v)]
            sig_o = F2[2].v.re("p (b c) -> p b c", c=256)
            ktok = junk.v.re("p (b r c) -> p b r c", r=2, c=128)
            mpsT = [psT[1], psT[3]]
            for blk in range(NB):
                pob = po[1]
                tk = slice(blk * 128, (blk + 1) * 128)
                for cc in range(2):
                    c = 2 * blk + cc
                    rows = slice(cc * 64, (cc + 1) * 64)
                    for pr in range(2):
                        kb.ts1('dve', C32[:, pr, :], C32[:, pr, :], decb[:, pr, c:c + 1], ALU.mult)
                        kb.cp('dve', Cbt[cc * 2 + pr].v, C32[:, pr, :])
                        pd = pm[2]
                        kb.mm(pd[:, 0:130], ktok[rows, blk, pr, :], vhat[rows, blk, 2 * pr:2 * pr + 2, :].re("p h v -> p (h v)"))
                        for half in range(2):
                            hs = slice(half * 64, (half + 1) * 64)
                            kb.tt('dve', C32[hs, pr, :], C32[hs, pr, :], pd[hs, half * 65:(half + 1) * 65], ALU.add)
                for hh in range(4):
                    pr, half = hh // 2, hh % 2
                    hs = slice(half * 64, (half + 1) * 64)
                    ps = mpsT[hh % 2]
                    PT = PTt[4 + hh]
                    kb.mm(ps.v, qkc[1][hs, pr, tk], qkc[0][hs, pr, tk])
                    kb.tt('dve', PT.v, ps.v, cst['maskbd8'].v, ALU.mult)
                    oc = slice(hh * 65, (hh + 1) * 65)
                    kb.mm(pob[:, oc], PT.v, vhat[:, blk, hh, :], start=True, stop=False, inc=False)
                    kb.mm(pob[0:64, oc], qkc[0][hs, pr, blk * 128:blk * 128 + 64], Cbt[0 + pr][hs, :], start=False, stop=True, inc=False)
                    kb.mm(pob[64:128, oc], qkc[0][hs, pr, blk * 128 + 64:(blk + 1) * 128], Cbt[2 + pr][hs, :], start=False, stop=True)
                ob = F1[4][:, 0:260]
                kb.cp('act', ob, pob[:, 0:260])
                ob3 = ob.re("p (h v) -> p h v", v=65)
                den = ob3[:, :, 64]
                Dn = sm[14][:, 0:4]
                d2 = sm[15][:, 0:4]
                kb.stt('dve', Dn, den, -1.0, den, ALU.mult, ALU.max)
                kb.tt('dve', Dn, Dn, gtok[:, blk, 12:16], ALU.max)
                kb.tt('dve', d2, Dn, Dn, ALU.mult)
                kb.ts1('dve', d2, d2, EPS, ALU.mult)
                finish_tm(blk, ob3[:, :, 0:64], d2, gainBCD[:, 512:768], sig_o[:, blk, :], 512,
                          scr=(F1[5], F1[6], sm[10], sm[11], ptr[1]))

        def post_norm(name, l):
            kb.dma('sp', gainb.v, DV(dr[name][l].partition_broadcast(128)))
            kb.memset('dve', ss.v, 0.0)
            for b in range(NB):
                kb.act(junk.v, ffo[:, b, :], AF.Square, accum=ss[:, b:b + 1])
            kb.act(rt.v, ss.v, AF.Sqrt, bias=EPS, scale=1.0 / D)
            kb.recip(rr.v, rt.v)
            for b in range(NB):
                kb.stt('dve', ffo[:, b, :], ffo[:, b, :], rr[:, b:b + 1], gainb.v, ALU.mult, ALU.mult)
                kb.tt('pool' if b % 2 == 0 else 'dve', h[:, b, :], h[:, b, :], ffo[:, b, :], ALU.add)

        def wout_stage(l):
            for dg in range(2):
                slab = get_slab()
                for blk in range(NB):
                    p = big()
                    for kc in range(8):
                        kb.mm(p.v, yT[:, kc, blk * 128:(blk + 1) * 128], slab[:, kc, 0:512], start=(kc == 0), stop=(kc == 7))
                    kb.cp('act' if blk % 2 == 0 else 'dve', ffo[:, blk, dg * 512:(dg + 1) * 512], p.v)
            post_norm('ln_mix_post', l)

        def ffn_stage(l):
            norm_to_aT(gpre_ffn)
            for fg in range(6):
                sg_ = get_slab()
                su_ = get_slab()
                ncl = min(512, DFF - fg * 512)
                for j in range(ncl // 128):
                    fc = fg * 4 + j
                    pg = big()
                    proj_fm(pg.v, sg_, j * 128, 128)
                    pu = big()
                    proj_fm(pu.v, su_, j * 128, 128)
                    sl_ = silt if fc % 2 == 0 else silt2
                    kb.act(sl_.v, pg.v, AF.Silu)
                    kb.tt('dve', hid[:, fc, :], sl_.v, pu.v, ALU.mult)
            for dg in range(2):
                s3 = [get_slab(), get_slab(), get_slab()]
                for blk in range(NB):
                    p = big()
                    for fc in range(NFC):
                        kb.mm(p.v, hid[:, fc, blk * 128:(blk + 1) * 128], s3[fc // 8][:, fc % 8, 0:512], start=(fc == 0), stop=(fc == NFC - 1))
                    kb.cp('act' if blk % 2 == 0 else 'dve', ffo[:, blk, dg * 512:(dg + 1) * 512], p.v)
            post_norm('ln_ffn_post', l)

        hspT = Tl(hsp)
        outT = Tl(out_d)
        import os
        KSTOP = int(os.environ.get('KSTOP', '99'))
        for l in range(NL):
            load_params(l)
            kb.barrier()
            if KSTOP <= 1:
                break
            for ti in range(NT):
                rowsl = slice(ti * T, (ti + 1) * T)
                if l == 0:
                    kb.dma('sp', h.v, DV(dr['x'][rowsl, :].rearrange("(b p) d -> p b d", p=128)))
                else:
                    kb.dma('sp', h.v, hspT[rowsl, :].re("(b p) d -> p b d", p=128))
                norm_to_aT(gpre_mix)
                gates(ti)
                if KSTOP <= 2:
                    break
                sA = get_slab()
                s5_proj(sA)
                sB = get_slab()
                sC = get_slab()
                hgrn_prep(sA, sB, sC, ti)
                sD = get_slab()
                fox_proj(sC, sD, ti)
                sE = get_slab()
                sF = get_slab()
                mlstm_prep(sE, sF, ti)
                kb.parallel([lambda: hgrn_attn(ti), lambda: mlstm_attn(ti)])
                kb.parallel([s5_main, lambda: fox_attn(ti)])
                if dbg and l == 0 and ti == 0:
                    kb.dma('pool', DV(dbg_d.rearrange("p (k t) -> p k t", t=T)), yT.v)
                kb.barrier()
                wout_stage(l)
                ffn_stage(l)
                dst = hspT if l < NL - 1 else outT
                kb.dma('sp', dst[rowsl, :].re("(b p) d -> p b d", p=128), h.v)
                kb.barrier()
        kb.finish()
    return nc


def kernel(**inputs):
    NT = 8
    nc = build(NT, 2)
    consts = host_consts()
    params = {name: np.ascontiguousarray(np.asarray(inputs[name], dtype=np.float32)) for name, _ in PARAM_SPECS}
    x = np.asarray(inputs['x'], dtype=np.float32)
    in_maps = []
    for b in range(8):
        m = {'x': np.ascontiguousarray(x[b])}
        m.update(params)
        for name in consts:
            m['c_' + name] = consts[name]
        in_maps.append(m)
    res = run_bass_kernel_spmd(nc, in_maps, core_ids=list(range(8)))
    return np.stack([np.asarray(r['out'], dtype=np.float32) for r in res.results], 0)
```
